# Optimizing a Trainium2 kernel written in Bass

```python
import math
import jax, jax.numpy as jnp
from jax import lax
import numpy as np

D_MODEL = 4096
BATCH = 4
SEQ = 4096
DEPTH = 1

CHUNK = 64
EPS = 1e-6
D_MIX = D_MODEL
D_S5 = D_MIX // 2
S5_GROUP = 16
S5_GROUPS = D_S5 // S5_GROUP
S5_STATE = 64
DT_MIN = 1e-3
DT_MAX = 1e-1
D_GM = D_MIX - D_S5
GM_HEADS = 8
GM_HEAD_DIM = D_GM // GM_HEADS
GM_BLOCK = 128
PEER_HEADS = 8
PEER_KEYS = 128
PEER_EXPERTS = PEER_KEYS * PEER_KEYS
PEER_QDIM = 256
PEER_TOPK = 16
PEER_TOKEN_BLOCK = 128

kernel_name = "hymba_s5_gmlp_peer_block"


def _rmsnorm(x, g):
    xf = x.astype(jnp.float32)
    y = xf * lax.rsqrt(jnp.mean(xf * xf, axis=-1, keepdims=True) + EPS)
    return (y * g.astype(jnp.float32)).astype(x.dtype)


def _layernorm(x, g, b):
    xf = x.astype(jnp.float32)
    mu = jnp.mean(xf, axis=-1, keepdims=True)
    xc = xf - mu
    y = xc * lax.rsqrt(jnp.mean(xc * xc, axis=-1, keepdims=True) + EPS)
    return (y * g.astype(jnp.float32) + b.astype(jnp.float32)).astype(x.dtype)


def _complex_affine_combine(left, right):
    a1r, a1i, b1r, b1i = left
    a2r, a2i, b2r, b2i = right
    ar = a2r * a1r - a2i * a1i
    ai = a2r * a1i + a2i * a1r
    br = a2r * b1r - a2i * b1i + b2r
    bi = a2r * b1i + a2i * b1r + b2i
    return (ar, ai, br, bi)


def _s5_mixer(u, a_re, a_im, log_dt, b_re, b_im, c_re, c_im, d_skip, w_glu):
    bsz, seq, _ = u.shape
    uf = u.astype(jnp.float32).reshape(bsz, seq, S5_GROUPS, S5_GROUP)
    dt = jnp.exp(log_dt.astype(jnp.float32))[:, None]
    lr = a_re.astype(jnp.float32)
    li = a_im.astype(jnp.float32)
    mag = jnp.exp(lr * dt)
    ang = li * dt
    abar_r = mag * jnp.cos(ang)
    abar_i = mag * jnp.sin(ang)
    nr = abar_r - 1.0
    ni = abar_i
    den = lr * lr + li * li
    coef_r = (nr * lr + ni * li) / den
    coef_i = (ni * lr - nr * li) / den
    br = b_re.astype(jnp.float32)
    bi = b_im.astype(jnp.float32)
    bbar_r = coef_r[..., None] * br - coef_i[..., None] * bi
    bbar_i = coef_r[..., None] * bi + coef_i[..., None] * br
    bu_r = jnp.einsum('blgc,gpc->lbgp', uf, bbar_r)
    bu_i = jnp.einsum('blgc,gpc->lbgp', uf, bbar_i)
    a_r = jnp.broadcast_to(abar_r[None, None], (seq, 1, S5_GROUPS, S5_STATE))
    a_i = jnp.broadcast_to(abar_i[None, None], (seq, 1, S5_GROUPS, S5_STATE))
    _, _, s_r, s_i = lax.associative_scan(_complex_affine_combine, (a_r, a_i, bu_r, bu_i), axis=0)
    y = (jnp.einsum('lbgp,gcp->blgc', s_r, c_re.astype(jnp.float32))
         - jnp.einsum('lbgp,gcp->blgc', s_i, c_im.astype(jnp.float32)))
    y = (y + d_skip.astype(jnp.float32).reshape(S5_GROUPS, S5_GROUP) * uf).reshape(bsz, seq, D_S5)
    y = jax.nn.gelu(y).astype(u.dtype)
    return y * jax.nn.sigmoid(y @ w_glu)


def _gmlp_mixer(z, ln_g, ln_b, w_s, b_s):
    bsz, seq, _ = z.shape
    z = jax.nn.gelu(z)
    u, v = jnp.split(z, 2, axis=-1)
    v = _layernorm(v, ln_g, ln_b)
    nblk = seq // GM_BLOCK
    v = v.reshape(bsz, nblk, GM_BLOCK, GM_HEADS, GM_HEAD_DIM)
    pos = jnp.arange(GM_BLOCK)
    mask = (pos[None, :] // CHUNK) <= (pos[:, None] // CHUNK)
    w = jnp.where(mask[None], w_s, 0.0)
    gate = jnp.einsum('hij,bnjhd->bnihd', w, v) + jnp.transpose(b_s)[None, None, :, :, None]
    return u * gate.reshape(bsz, seq, D_GM)


def _peer(x, w_q, keys_1, keys_2, expert_down, expert_up):
    bsz, seq, dim = x.shape
    q = (x @ w_q).reshape(bsz, seq, PEER_HEADS, 2, PEER_QDIM // 2)
    s1 = jnp.einsum('blhd,nd->blhn', q[..., 0, :], keys_1).astype(jnp.float32)
    s2 = jnp.einsum('blhd,nd->blhn', q[..., 1, :], keys_2).astype(jnp.float32)
    v1, i1 = lax.top_k(s1, PEER_TOPK)
    v2, i2 = lax.top_k(s2, PEER_TOPK)
    cand = (v1[..., :, None] + v2[..., None, :]).reshape(bsz, seq, PEER_HEADS, PEER_TOPK * PEER_TOPK)
    sc, ci = lax.top_k(cand, PEER_TOPK)
    e1 = jnp.take_along_axis(i1, ci // PEER_TOPK, axis=-1)
    e2 = jnp.take_along_axis(i2, ci % PEER_TOPK, axis=-1)
    expert = e1 * PEER_KEYS + e2
    gates = jax.nn.softmax(sc, axis=-1).astype(x.dtype)
    nblk = (bsz * seq) // PEER_TOKEN_BLOCK
    hk = PEER_HEADS * PEER_TOPK
    xb = x.reshape(nblk, PEER_TOKEN_BLOCK, dim)
    eb = expert.reshape(nblk, PEER_TOKEN_BLOCK, hk)
    gb = gates.reshape(nblk, PEER_TOKEN_BLOCK, hk)

    def block(args):
        xt, et, gt = args
        u = jnp.take(expert_down, et, axis=0)
        act = jax.nn.gelu(jnp.einsum('td,ted->te', xt, u)) * gt
        v = jnp.take(expert_up, et, axis=0)
        return jnp.einsum('te,ted->td', act, v)

    out = lax.map(block, (xb, eb, gb))
    return out.reshape(bsz, seq, dim)


def setup_inputs(seed: int = 0) -> dict:
    key = jax.random.key(seed)
    ks = jax.random.split(key, 27)
    f32 = jnp.float32
    nrm = lambda k, shape, s: jax.random.normal(k, shape, f32) * s
    gain = lambda k, shape: 1.0 + 0.02 * jax.random.normal(k, shape, f32)
    n_idx = jnp.arange(S5_STATE, dtype=f32)
    return {
        "x": nrm(ks[0], (BATCH, SEQ, D_MODEL), 1.0),
        "norm_mix_g": gain(ks[1], (DEPTH, D_MODEL)),
        "w_in": nrm(ks[2], (DEPTH, D_MODEL, D_S5 + 2 * D_GM), D_MODEL ** -0.5),
        "s5_a_re": -0.5 + 0.01 * jax.random.normal(ks[3], (DEPTH, S5_GROUPS, S5_STATE), f32),
        "s5_a_im": math.pi * n_idx + 0.01 * jax.random.normal(ks[4], (DEPTH, S5_GROUPS, S5_STATE), f32),
        "s5_log_dt": jax.random.uniform(ks[5], (DEPTH, S5_GROUPS), f32, math.log(DT_MIN), math.log(DT_MAX)),
        "s5_b_re": nrm(ks[6], (DEPTH, S5_GROUPS, S5_STATE, S5_GROUP), (2 * S5_GROUP) ** -0.5),
        "s5_b_im": nrm(ks[7], (DEPTH, S5_GROUPS, S5_STATE, S5_GROUP), (2 * S5_GROUP) ** -0.5),
        "s5_c_re": nrm(ks[8], (DEPTH, S5_GROUPS, S5_GROUP, S5_STATE), S5_STATE ** -0.5),
        "s5_c_im": nrm(ks[9], (DEPTH, S5_GROUPS, S5_GROUP, S5_STATE), S5_STATE ** -0.5),
        "s5_d": nrm(ks[10], (DEPTH, D_S5), 0.5),
        "s5_w_glu": nrm(ks[11], (DEPTH, D_S5, D_S5), D_S5 ** -0.5),
        "gm_ln_g": gain(ks[12], (DEPTH, D_GM)),
        "gm_ln_b": nrm(ks[13], (DEPTH, D_GM), 0.02),
        "gm_w_s": nrm(ks[14], (DEPTH, GM_HEADS, GM_BLOCK, GM_BLOCK), GM_BLOCK ** -0.5),
        "gm_b_s": gain(ks[15], (DEPTH, GM_HEADS, GM_BLOCK)),
        "norm_s5_out_g": gain(ks[16], (DEPTH, D_S5)),
        "norm_gm_out_g": gain(ks[17], (DEPTH, D_GM)),
        "w_out": nrm(ks[18], (DEPTH, D_MIX, D_MODEL), D_MIX ** -0.5),
        "norm_ffn_g": gain(ks[19], (DEPTH, D_MODEL)),
        "peer_w_q": nrm(ks[20], (DEPTH, D_MODEL, PEER_HEADS * PEER_QDIM), D_MODEL ** -0.5),
        "peer_keys_1": nrm(ks[21], (DEPTH, PEER_KEYS, PEER_QDIM // 2), (PEER_QDIM // 2) ** -0.5),
        "peer_keys_2": nrm(ks[22], (DEPTH, PEER_KEYS, PEER_QDIM // 2), (PEER_QDIM // 2) ** -0.5),
        "peer_down": nrm(ks[23], (DEPTH, PEER_EXPERTS, D_MODEL), D_MODEL ** -0.5),
        "peer_up": nrm(ks[24], (DEPTH, PEER_EXPERTS, D_MODEL), PEER_HEADS ** -0.5),
        "norm_final_g": gain(ks[25], (D_MODEL,)),
    }


def reference(x, norm_mix_g, w_in, s5_a_re, s5_a_im, s5_log_dt, s5_b_re, s5_b_im, s5_c_re, s5_c_im,
              s5_d, s5_w_glu, gm_ln_g, gm_ln_b, gm_w_s, gm_b_s, norm_s5_out_g, norm_gm_out_g, w_out,
              norm_ffn_g, peer_w_q, peer_keys_1, peer_keys_2, peer_down, peer_up, norm_final_g):
    h = x
    for i in range(DEPTH):
        a = _rmsnorm(h, norm_mix_g[i])
        z = a @ w_in[i]
        y_s5 = _s5_mixer(z[..., :D_S5], s5_a_re[i], s5_a_im[i], s5_log_dt[i], s5_b_re[i], s5_b_im[i],
                         s5_c_re[i], s5_c_im[i], s5_d[i], s5_w_glu[i])
        y_gm = _gmlp_mixer(z[..., D_S5:], gm_ln_g[i], gm_ln_b[i], gm_w_s[i], gm_b_s[i])
        mixed = jnp.concatenate([_rmsnorm(y_s5, norm_s5_out_g[i]), _rmsnorm(y_gm, norm_gm_out_g[i])], axis=-1)
        h = h + mixed @ w_out[i]
        h = h + _peer(_rmsnorm(h, norm_ffn_g[i]), peer_w_q[i], peer_keys_1[i], peer_keys_2[i],
                      peer_down[i], peer_up[i])
    return _rmsnorm(h, norm_final_g)
```

```python
import contextlib
import math

import ml_dtypes
import numpy as np

import concourse.bass as bass
import concourse.mybir as mybir
from concourse.bass_utils import run_bass_kernel_spmd

F32 = mybir.dt.float32
BF16 = mybir.dt.bfloat16
U32 = mybir.dt.uint32
ALU = mybir.AluOpType
AF = mybir.ActivationFunctionType
AX = mybir.AxisListType

D = 4096
NTOK = 2048
TS = 512
NST = NTOK // TS
EPS = 1e-6
TWO_PI = 2.0 * math.pi
GELU = AF.Gelu_apprx_tanh
CONV_INTERLEAVE = True


class Buf:
    __slots__ = ("name", "w", "r")

    def __init__(self, name=""):
        self.name = name
        self.w = None
        self.r = []


class Sched:
    EPOCH = 3500

    def __init__(self, nc, stack):
        self.nc = nc
        self.stack = stack
        self.eng = {"pe": nc.tensor, "dve": nc.vector, "act": nc.scalar, "pool": nc.gpsimd, "sp": nc.sync}
        self.sem = {}
        self.cnt = {}
        self.pending = {}
        self.seen = {k: {} for k in self.eng}
        self.nsem = 0
        self.last_old = {}
        for k in self.eng:
            self._new_epoch(k)
        self.dpool = {}
        for k, n in (("sp", 24), ("pool", 24), ("act", 8)):
            self.dpool[k] = [[self._mksem(f"d{k}{i}"), 0] for i in range(n)]
        self.dnext = {k: 0 for k in self.dpool}
        self.out_events = []
        self.nins = 0

    def _mksem(self, name):
        self.nsem += 1
        return self.stack.enter_context(self.nc.semaphore(f"{name}_{self.nsem}"))

    def _new_epoch(self, k):
        if k in self.sem and self.cnt[k] > 0:
            self.last_old[k] = (self.sem[k], self.cnt[k])
        self.sem[k] = self._mksem(f"e{k}")
        self.cnt[k] = 0
        self.pending[k] = False

    def _wait(self, k, evs):
        need = {}
        for (s, v) in evs:
            if self.seen[k].get(id(s), (None, 0))[1] >= v:
                continue
            if id(s) not in need or need[id(s)][1] < v:
                need[id(s)] = (s, v)
        for s, v in need.values():
            self.eng[k].wait_ge(s, v)
            self.seen[k][id(s)] = (s, v)

    def _deps(self, k, r, w):
        evs = []
        for b in r:
            if b.w is not None:
                evs.append(b.w)
        for b in w:
            if b.w is not None:
                evs.append(b.w)
            evs.extend(b.r)
        if k == "pe":
            evs = [e for e in evs if e[0] is not self.sem["pe"]]
        return evs

    def _mark(self, ev, r, w):
        for b in r:
            b.r.append(ev)
            if len(b.r) > 48:
                best = {}
                for (s, v) in b.r:
                    if id(s) not in best or best[id(s)][1] < v:
                        best[id(s)] = (s, v)
                b.r = list(best.values())
        for b in w:
            b.w = ev
            b.r = []

    def op(self, k, fn, r=(), w=(), signal=True):
        self._wait(k, self._deps(k, r, w))
        ins = fn(self.eng[k])
        self.nins += 1
        if signal:
            ins.then_inc(self.sem[k], 1)
            self.cnt[k] += 1
            self.pending[k] = False
            ev = (self.sem[k], self.cnt[k])
        else:
            self.pending[k] = True
            ev = (self.sem[k], self.cnt[k] + 1)
        self._mark(ev, r, w)
        if signal and self.cnt[k] >= self.EPOCH:
            self._new_epoch(k)
        return ev

    def dma(self, k, out, in_, r=(), w=(), is_output=False, **kw):
        pool = self.dpool[k]
        i = self.dnext[k]
        self.dnext[k] = (i + 1) % len(pool)
        slot = pool[i]
        evs = self._deps(k, r, w)
        if slot[1] > 0:
            evs.append((slot[0], slot[1]))
        self._wait(k, evs)
        ins = self.eng[k].dma_start(out=out, in_=in_, **kw)
        self.nins += 1
        slot[1] += 16
        ins.then_inc(slot[0], 16)
        ev = (slot[0], slot[1])
        self._mark(ev, r, w)
        if is_output:
            self.out_events.append(ev)
        return ev

    def barrier(self):
        evs = []
        for k in self.eng:
            assert not self.pending[k], k
            if self.cnt[k] > 0:
                evs.append((self.sem[k], self.cnt[k]))
            elif k in self.last_old:
                evs.append(self.last_old[k])
        for k in self.dpool:
            for s, v in self.dpool[k]:
                if v > 0:
                    evs.append((s, v))
        for k in self.eng:
            self._wait(k, evs)

    def barrier_dma(self):
        evs = []
        for k in self.dpool:
            for s_, v in self.dpool[k]:
                if v > 0:
                    evs.append((s_, v))
        for k in ("pool", "sp"):
            self._wait(k, evs)

    def finish(self):
        self._wait("sp", self.out_events)
        self.barrier()


class Ctx:
    pass


_UID = [0]


def _uname(name):
    _UID[0] += 1
    return f"{name}_{_UID[0]}"


def _tile(nc, stack, name, shape, dt):
    return stack.enter_context(nc.sbuf_tensor(_uname("sb_" + name), list(shape), dt)), Buf(name)


def phase_a(C, st_list):
    nc, S, T = C.nc, C.S, C.T
    with contextlib.ExitStack() as ph:
        tl = lambda name, shape, dt: _tile(nc, ph, name, shape, dt)
        gmix, bgmix = tl("gmix", [128, D], F32)
        S.dma("sp", gmix[:], T["gmix_r"][:], w=[bgmix])
        xt = [tl(f"xt{i}", [128, D], F32) for i in range(2)]
        abf = [tl(f"abf{i}", [128, D], BF16) for i in range(4)]
        aT, baT = tl("aT", [128, 32, TS], BF16)
        wb = [tl(f"wb{i}", [128, 32, 256], BF16) for i in range(2)]
        zs = [tl(f"zs{i}", [128, TS], BF16) for i in range(3)]
        V = [tl(f"V{i}", [128, 2048], F32) for i in range(4)]
        vn = [tl(f"vn{i}", [128, 2048], BF16) for i in range(2)]
        junk, bjunk = tl("junkA", [128, 2048], BF16)
        st8 = [tl(f"st8{i}", [128, 8], F32) for i in range(2)]
        w_in_v = T["w_in"].rearrange("(c p) n -> p c n", p=128)
        nx = 0
        nw = 0
        nz = 0
        nb = 0
        def norm_part(st):
            own = st >= NST
            xsrc = T["x_own"] if own else T["x_prev"]
            t0 = (st - NST if own else st) * TS
            for tt in range(4):
                (x_t, bx), (a_t, ba), (s8, bs8) = xt[tt % 2], abf[tt], st8[tt % 2]
                S.dma("sp", x_t[:], xsrc[t0 + tt * 128:t0 + (tt + 1) * 128, :], w=[bx])
                S.op("dve", lambda e: e.memset(s8[:], 0.0), w=[bs8])
                S.op("act", lambda e: e.activation(out=a_t[:], in_=x_t[:], func=AF.Square, accum_out=s8[:, 0:1]),
                     r=[bx], w=[ba, bs8])
                S.op("dve", lambda e: e.tensor_scalar(out=s8[:, 1:2], in0=s8[:, 0:1], scalar1=1.0 / D, scalar2=EPS,
                                                      op0=ALU.mult, op1=ALU.add), r=[bs8], w=[bs8])
                S.op("act", lambda e: e.activation(out=s8[:, 3:4], in_=s8[:, 1:2], func=AF.Sqrt), r=[bs8], w=[bs8])
                S.op("dve", lambda e: e.reciprocal(out=s8[:, 2:3], in_=s8[:, 3:4]), r=[bs8], w=[bs8])
                S.op("dve", lambda e: e.scalar_tensor_tensor(out=a_t[:], in0=x_t[:], scalar=s8[:, 2:3], in1=gmix[:],
                                                             op0=ALU.mult, op1=ALU.mult), r=[bx, bs8, bgmix], w=[ba])

        def transpose_part():
            nonlocal_nb = cntA["nb"]
            for tt in range(4):
                (a_t, ba) = abf[tt]
                for dcg in range(4):
                    pb, bpb = C.ps[nonlocal_nb % 8], C.bps[nonlocal_nb % 8]
                    nonlocal_nb += 1
                    pbb = pb[:].bitcast(BF16)
                    for j in range(8):
                        dc = dcg * 8 + j
                        S.op("pe", lambda e: e.transpose(out=pbb[:, j * 128:(j + 1) * 128],
                                                         in_=a_t[:, dc * 128:(dc + 1) * 128], identity=C.identb[:]),
                             r=[ba, C.bidentb], w=[bpb], signal=(j == 7))
                    src = pbb[:, 0:1024].rearrange("p (j t) -> p j t", j=8)
                    dst = aT[:, dcg * 8:(dcg + 1) * 8, tt * 128:(tt + 1) * 128]
                    if dcg % 2 == 0:
                        S.op("act", lambda e: e.activation(out=dst, in_=src, func=AF.Copy), r=[bpb], w=[baT])
                    else:
                        S.op("dve", lambda e: e.tensor_copy(out=dst, in_=src), r=[bpb], w=[baT])
            cntA["nb"] = nonlocal_nb

        cntA = {"nb": 0}
        norm_part(st_list[0])
        for si, st in enumerate(st_list):
            own = st >= NST
            t0 = (st - NST if own else st) * TS
            transpose_part()
            if si + 1 < len(st_list):
                norm_part(st_list[si + 1])
            nb = cntA["nb"]
            ncol = 24 if own else 8
            for ct in range(ncol):
                (w_t, bw) = wb[nw % 2]
                nw += 1
                if ct not in C.win_cached:
                    S.dma("pool", w_t[:], w_in_v[:, :, ct * 256:(ct + 1) * 256], w=[bw])
                    S.dma("sp", T["WinS"][ct], w_t[:], r=[bw])
                    C.win_cached.add(ct)
                    C.win_fresh.add(ct)
                else:
                    if ct in C.win_fresh:
                        S.barrier_dma()
                        C.win_fresh.clear()
                    S.dma("pool", w_t[:], T["WinS"][ct], w=[bw])
                if ct < 16:
                    for sub in range(2):
                        pb, bpb = C.ps[nb % 8], C.bps[nb % 8]
                        nb += 1
                        for dc in range(32):
                            S.op("pe", lambda e: e.matmul(pb[:], w_t[:, dc, sub * 128:(sub + 1) * 128], aT[:, dc, :],
                                                          start=(dc == 0), stop=(dc == 31)),
                                 r=[bw, baT], w=[bpb], signal=(dc == 31))
                        (z_t, bz) = zs[nz % 3]
                        nz += 1
                        if ct < 8:
                            S.op("act", lambda e: e.activation(out=z_t[:], in_=pb[:], func=AF.Copy), r=[bpb], w=[bz])
                            row = (ct * 2 + sub) * 128
                            S.dma("sp", T["ZT"][row:row + 128, st * TS:(st + 1) * TS], z_t[:], r=[bz])
                        else:
                            S.op("act", lambda e: e.activation(out=z_t[:], in_=pb[:], func=GELU), r=[bpb], w=[bz])
                            row = ((ct - 8) * 2 + sub) * 128
                            S.dma("sp", T["UT"][row:row + 128, t0:t0 + TS], z_t[:], r=[bz])
                else:
                    vc = ct - 16
                    for tt in range(4):
                        pb, bpb = C.ps[nb % 8], C.bps[nb % 8]
                        nb += 1
                        for dc in range(32):
                            S.op("pe", lambda e: e.matmul(pb[:, 0:256], aT[:, dc, tt * 128:(tt + 1) * 128], w_t[:, dc, :],
                                                          start=(dc == 0), stop=(dc == 31)),
                                 r=[bw, baT], w=[bpb], signal=(dc == 31))
                        S.op("act", lambda e: e.activation(out=V[tt][0][:, vc * 256:(vc + 1) * 256], in_=pb[:, 0:256],
                                                           func=GELU), r=[bpb], w=[V[tt][1]])
            cntA["nb"] = nb
            if own:
                for tt in range(4):
                    v_t, bv = V[tt]
                    (s8, bs8) = st8[nx % 2]
                    (vn_t, bvn) = vn[nx % 2]
                    nx += 1
                    S.op("dve", lambda e: e.memset(s8[:], 0.0), w=[bs8])
                    S.op("act", lambda e: e.activation(out=vn_t[:], in_=v_t[:], func=AF.Square,
                                                       accum_out=s8[:, 0:1]), r=[bv], w=[bvn, bs8])
                    S.op("dve", lambda e: e.tensor_scalar(out=junk[:, 0:2048], in0=v_t[:], scalar1=1.0, scalar2=0.0,
                                                          op0=ALU.mult, op1=ALU.add, accum_out=s8[:, 1:2]),
                         r=[bv], w=[bjunk, bs8])
                    S.op("dve", lambda e: e.tensor_scalar(out=s8[:, 2:3], in0=s8[:, 1:2], scalar1=1.0 / 2048, scalar2=None,
                                                          op0=ALU.mult), r=[bs8], w=[bs8])
                    S.op("dve", lambda e: e.tensor_tensor(out=s8[:, 3:4], in0=s8[:, 2:3], in1=s8[:, 2:3], op=ALU.mult),
                         r=[bs8], w=[bs8])
                    S.op("dve", lambda e: e.scalar_tensor_tensor(out=s8[:, 4:5], in0=s8[:, 0:1], scalar=1.0 / 2048,
                                                                 in1=s8[:, 3:4], op0=ALU.mult, op1=ALU.subtract),
                         r=[bs8], w=[bs8])
                    S.op("dve", lambda e: e.tensor_scalar(out=s8[:, 6:7], in0=s8[:, 4:5], scalar1=EPS, scalar2=None,
                                                          op0=ALU.add), r=[bs8], w=[bs8])
                    S.op("act", lambda e: e.activation(out=s8[:, 7:8], in_=s8[:, 6:7], func=AF.Sqrt), r=[bs8], w=[bs8])
                    S.op("dve", lambda e: e.reciprocal(out=s8[:, 5:6], in_=s8[:, 7:8]), r=[bs8], w=[bs8])
                    S.op("dve", lambda e: e.tensor_scalar(out=vn_t[:], in0=v_t[:], scalar1=s8[:, 2:3], scalar2=s8[:, 5:6],
                                                          op0=ALU.subtract, op1=ALU.mult), r=[bv, bs8], w=[bvn])
                    S.dma("sp", T["VN"][t0 + tt * 128:t0 + (tt + 1) * 128, :], vn_t[:], r=[bvn])
        S.barrier()


def phase_s5(C, gb_list):
    nc, S, T = C.nc, C.S, C.T
    PI = math.pi
    with contextlib.ExitStack() as ph:
        tl = lambda name, shape, dt: _tile(nc, ph, name, shape, dt)
        bset = Buf("s5setup")
        raw = lambda name, shape, dt: ph.enter_context(nc.sbuf_tensor(_uname("sr_" + name), list(shape), dt))
        so = lambda k, fn: S.op(k, fn, r=[bset], w=[bset])
        aq, iq, lq = raw("aq", [128, 128], F32), raw("iq", [128, 128], F32), raw("lq", [128, 128], F32)
        theta, mag, tq = raw("theta", [128, 128], F32), raw("mag", [128, 128], F32), raw("tq", [128, 128], F32)
        offs = raw("offs", [128, 8, 128], F32)
        S.dma("sp", aq[:], T["a_re_q"][:], w=[bset])
        S.dma("sp", iq[:], T["a_im_q"][:], w=[bset])
        S.dma("sp", lq[:], T["ldt_q"][:], w=[bset])
        so("act", lambda e: e.activation(out=lq[:], in_=lq[:], func=AF.Exp))
        so("dve", lambda e: e.tensor_tensor(out=tq[:], in0=aq[:], in1=lq[:], op=ALU.mult))
        so("act", lambda e: e.activation(out=mag[:], in_=tq[:], func=AF.Exp))
        so("dve", lambda e: e.tensor_tensor(out=theta[:], in0=iq[:], in1=lq[:], op=ALU.mult))
        kq = raw("kq", [128, 128], mybir.dt.int32)
        so("dve", lambda e: e.tensor_scalar(out=tq[:], in0=theta[:], scalar1=1.0 / TWO_PI, scalar2=None, op0=ALU.mult))
        so("dve", lambda e: e.tensor_copy(out=kq[:], in_=tq[:]))
        so("dve", lambda e: e.tensor_tensor(out=theta[:], in0=tq[:], in1=kq[:], op=ALU.subtract))
        for seg in range(8):
            so("dve", lambda e: e.tensor_scalar(out=tq[:], in0=theta[:], scalar1=float(512 * seg), scalar2=None, op0=ALU.mult))
            so("dve", lambda e: e.tensor_copy(out=kq[:], in_=tq[:]))
            so("dve", lambda e: e.tensor_tensor(out=offs[:, seg, :], in0=tq[:], in1=kq[:], op=ALU.subtract))
        hp = raw("hp", [128, 1], F32)
        BST1, BST2 = raw("BST1", [128, 16, 128], BF16), raw("BST2", [128, 16, 128], BF16)
        sg = raw("sg", [128, 1], F32)
        CST1, CST2 = raw("CST1", [128, 16, 128], BF16), raw("CST2", [128, 16, 128], BF16)
        dcol, DST = raw("dcol", [128, 16], F32), raw("DST", [128, 16, 128], BF16)
        rmask, cmask = raw("rmask", [128, 8], F32), raw("cmask", [128, 8, 128], BF16)
        iota = raw("iota512", [128, 512], F32)
        riota = raw("riota512", [128, 512], F32)
        c512, s512 = raw("c512", [128, 128], F32), raw("s512", [128, 128], F32)
        swapI = raw("swapI", [128, 128], F32)
        eq_, mag512 = raw("eq", [128, 128], F32), raw("mag512", [128, 128], F32)
        tmpst = contextlib.ExitStack()
        rawt = lambda name, shape, dt: tmpst.enter_context(nc.sbuf_tensor(_uname("st_" + name), list(shape), dt))
        names = ["arb", "aib", "ldb", "brb", "bib", "magb", "ang", "nsin", "ncos", "nr", "ni", "u1", "u2", "cr", "ci"]
        B = {n: rawt("s5" + n, [128, 1024], F32) for n in names}
        for n, src in (("arb", "a_re_b"), ("aib", "a_im_b"), ("ldb", "ldt_b"), ("brb", "b_re_b"), ("bib", "b_im_b")):
            S.dma("sp", B[n][:], T[src][:], w=[bset])
        tt_ = lambda o, a, b, op: so("dve", lambda e: e.tensor_tensor(out=B[o][:], in0=B[a][:], in1=B[b][:], op=op))
        so("act", lambda e: e.activation(out=B["ldb"][:], in_=B["ldb"][:], func=AF.Exp))
        tt_("u1", "arb", "ldb", ALU.mult)
        so("act", lambda e: e.activation(out=B["magb"][:], in_=B["u1"][:], func=AF.Exp))
        tt_("ang", "aib", "ldb", ALU.mult)
        kb = rawt("kb", [128, 1024], mybir.dt.int32)
        so("dve", lambda e: e.memset(hp[:], PI / 2))
        so("dve", lambda e: e.tensor_scalar(out=B["u1"][:], in0=B["ang"][:], scalar1=1.0 / TWO_PI, scalar2=None, op0=ALU.mult))
        so("dve", lambda e: e.tensor_copy(out=kb[:], in_=B["u1"][:]))
        so("dve", lambda e: e.tensor_tensor(out=B["u1"][:], in0=B["u1"][:], in1=kb[:], op=ALU.subtract))
        so("dve", lambda e: e.scalar_tensor_tensor(out=B["u2"][:], in0=B["u1"][:], scalar=-1.0, in1=B["u1"][:],
                                                   op0=ALU.mult, op1=ALU.max))
        so("act", lambda e: e.activation(out=B["nsin"][:], in_=B["u1"][:], func=AF.Sin, scale=TWO_PI))
        so("act", lambda e: e.activation(out=B["ncos"][:], in_=B["u2"][:], func=AF.Sin, scale=-TWO_PI, bias=hp[:, 0:1]))
        tt_("u1", "magb", "ncos", ALU.mult)
        so("dve", lambda e: e.tensor_scalar(out=B["nr"][:], in0=B["u1"][:], scalar1=-1.0, scalar2=None, op0=ALU.add))
        tt_("ni", "magb", "nsin", ALU.mult)
        tt_("u1", "arb", "arb", ALU.mult)
        tt_("u2", "aib", "aib", ALU.mult)
        tt_("u1", "u1", "u2", ALU.add)
        so("dve", lambda e: e.reciprocal(out=B["u2"][:], in_=B["u1"][:]))
        tt_("cr", "nr", "arb", ALU.mult)
        tt_("u1", "ni", "aib", ALU.mult)
        tt_("cr", "cr", "u1", ALU.add)
        tt_("cr", "cr", "u2", ALU.mult)
        tt_("ci", "ni", "arb", ALU.mult)
        tt_("u1", "nr", "aib", ALU.mult)
        tt_("ci", "ci", "u1", ALU.subtract)
        tt_("ci", "ci", "u2", ALU.mult)
        tt_("nr", "cr", "brb", ALU.mult)
        tt_("u1", "ci", "bib", ALU.mult)
        tt_("nr", "nr", "u1", ALU.subtract)
        tt_("ni", "cr", "bib", ALU.mult)
        tt_("u1", "ci", "brb", ALU.mult)
        tt_("ni", "ni", "u1", ALU.add)
        v3 = lambda n: B[n][:].rearrange("p (g q) -> p g q", g=16)
        so("dve", lambda e: e.tensor_copy(out=BST1[:, :, 0:64], in_=v3("nr")))
        so("dve", lambda e: e.tensor_copy(out=BST1[:, :, 64:128], in_=v3("ni")))
        so("dve", lambda e: e.tensor_copy(out=BST2[:, :, 0:64], in_=v3("ni")))
        so("dve", lambda e: e.tensor_scalar(out=BST2[:, :, 64:128], in0=v3("nr"), scalar1=-1.0, scalar2=None, op0=ALU.mult))
        cm1 = B["arb"]
        cmA = rawt("cmA", [128, 2048], F32)
        S.dma("sp", cmA[:], T["cmix1"][:], w=[bset])
        S.dma("sp", sg[:], T["sgn1"][:], w=[bset])
        cmB = rawt("cmB", [128, 2048], F32)
        S.dma("sp", cmB[:], T["cmix2"][:], w=[bset])
        so("dve", lambda e: e.tensor_scalar(out=CST2[:].rearrange("p g q -> p (g q)"), in0=cmB[:], scalar1=-1.0,
                                            scalar2=None, op0=ALU.mult))
        so("dve", lambda e: e.tensor_scalar(out=CST1[:].rearrange("p g q -> p (g q)"), in0=cmA[:], scalar1=sg[:, 0:1],
                                            scalar2=None, op0=ALU.mult))
        S.dma("sp", dcol[:], T["d_col"][:], w=[bset])
        for gb in range(16):
            so("dve", lambda e: e.tensor_scalar(out=DST[:, gb, :], in0=C.identf[:], scalar1=dcol[:, gb:gb + 1],
                                                scalar2=None, op0=ALU.mult))
        S.dma("sp", rmask[:], T["rowmask"][:], w=[bset])
        S.dma("pool", cmask[:], T["colmask"][:], w=[bset])
        S.dma("sp", iota[:], T["iota512"][:], w=[bset])

        S.dma("sp", swapI[:], T["swapI"][:], w=[bset])
        S.dma("sp", riota[:], T["riota512"][:], w=[bset])
        so("dve", lambda e: e.tensor_tensor(out=eq_[:], in0=aq[:], in1=lq[:], op=ALU.mult))
        so("act", lambda e: e.activation(out=mag512[:], in_=eq_[:], func=AF.Exp, scale=512.0))
        so("dve", lambda e: e.tensor_scalar(out=tq[:], in0=theta[:], scalar1=512.0, scalar2=None, op0=ALU.mult))
        so("dve", lambda e: e.tensor_copy(out=kq[:], in_=tq[:]))
        so("dve", lambda e: e.tensor_tensor(out=tq[:], in0=tq[:], in1=kq[:], op=ALU.subtract))
        so("dve", lambda e: e.scalar_tensor_tensor(out=c512[:], in0=tq[:], scalar=-1.0, in1=tq[:], op0=ALU.mult, op1=ALU.max))
        so("act", lambda e: e.activation(out=s512[:], in_=tq[:], func=AF.Sin, scale=TWO_PI))
        so("act", lambda e: e.activation(out=c512[:], in_=c512[:], func=AF.Sin, scale=-TWO_PI, bias=hp[:, 0:1]))
        so("dve", lambda e: e.tensor_scalar(out=s512[:], in0=s512[:], scalar1=sg[:, 0:1], scalar2=None, op0=ALU.mult))

        S.barrier()
        tmpst.close()
        NSET = 4
        zt = [tl(f"zt{i}", [128, 4096], BF16) for i in range(2)]
        bm = [[tl(f"bm{k}{i}", [128, 128], BF16) for i in range(4)] for k in range(NSET)]
        ROT = [tl(f"ROT{k}", [128, 128], F32) for k in range(NSET)]
        rtm = [tl(f"rtm{k}", [128, 128], F32) for k in range(2)]
        SNt = [tl(f"SN{k}", [128, 512], F32) for k in range(NSET)]
        CSt = [tl(f"CS{k}", [128, 512], F32) for k in range(NSET)]
        kph = [tl(f"kph{k}", [128, 512], mybir.dt.int32) for k in range(2)]
        t1 = [tl(f"t1{k}", [128, 512], F32) for k in range(4)]
        t2 = [tl(f"t2{k}", [128, 512], F32) for k in range(4)]
        vv = [tl(f"vv{k}", [128, 512], F32) for k in range(4)]
        q1 = [tl(f"q1{k}", [128, 512], BF16) for k in range(4)]
        q2 = [tl(f"q2{k}", [128, 512], BF16) for k in range(4)]
        ini = [tl(f"ini{k}", [128, 1], F32) for k in range(4)]
        accs = [tl(f"accs{k}", [128, 4], F32) for k in range(4)]
        CSb = [tl(f"CSb{k}", [128, 512], BF16) for k in range(NSET)]
        SNb = [tl(f"SNb{k}", [128, 512], BF16) for k in range(NSET)]
        vb = [tl(f"vb{k}", [128, 512], BF16) for k in range(4)]
        junkS = [tl(f"junkS{k}", [128, 512], F32) for k in range(2)]
        DEC = [tl(f"DEC{k}", [128, 512], F32) for k in range(2)]
        MCS = [tl(f"MCS{k}", [128, 512], F32) for k in range(NSET)]
        MSN = [tl(f"MSN{k}", [128, 512], F32) for k in range(NSET)]
        conv_jobs = []
        wout_v = T["w_out"].rearrange("(c p) n -> p c n", p=128)
        wq_v = T["w_q"].rearrange("(c p) n -> p c n", p=128)
        dn_v = T["downT"].rearrange("(c p) e -> p c e", p=128)
        up_v = T["up"].rearrange("(c p) d -> p c d", p=128)
        for ds in range(8):
            conv_jobs.append((T["WoS"][ds], wout_v[:, :, ds * 512:(ds + 1) * 512]))
        for qc in range(8):
            conv_jobs.append((T["WqS"][qc], wq_v[:, :, qc * 256:(qc + 1) * 256]))
        for cg in range(16):
            for cp in range(4):
                i_ = cg * 4 + cp
                conv_jobs.append((T["DnS"][i_], dn_v[:, :, i_ * 256:(i_ + 1) * 256]))
            for ds in range(8):
                conv_jobs.append((T["UpS"][cg * 8 + ds], up_v[:, cg * 8:(cg + 1) * 8, ds * 512:(ds + 1) * 512]))
        ysb = [tl(f"ysb{k}", [128, 512], BF16) for k in range(2)]
        P1, bP1, P2, bP2, RB, bRB = C.ps[0], C.bps[0], C.ps[1], C.bps[1], C.ps[2], C.bps[2]
        cnt = {"prep": 0, "it": 0, "y": 0}

        def prep_group(g):
            k = g % NSET
            gb, g8 = divmod(g, 8)
            S.op("act", lambda e: e.activation(out=bm[k][0][0][:], in_=BST1[:, gb, :], func=AF.Copy, scale=rmask[:, g8:g8 + 1]),
                 r=[bset], w=[bm[k][0][1]])
            S.op("act", lambda e: e.activation(out=bm[k][1][0][:], in_=BST2[:, gb, :], func=AF.Copy, scale=rmask[:, g8:g8 + 1]),
                 r=[bset], w=[bm[k][1][1]])
            S.op("dve", lambda e: e.tensor_tensor(out=bm[k][2][0][:], in0=CST1[:, gb, :], in1=cmask[:, g8, :],
                                                  op=ALU.mult), r=[bset], w=[bm[k][2][1]])
            S.op("dve", lambda e: e.tensor_tensor(out=bm[k][3][0][:], in0=CST2[:, gb, :], in1=cmask[:, g8, :],
                                                  op=ALU.mult), r=[bset], w=[bm[k][3][1]])
            (rt, brt), (rm_, brm) = ROT[k], rtm[cnt["prep"] % 2]
            S.op("act", lambda e: e.activation(out=rt[:], in_=C.identf[:], func=AF.Copy, scale=c512[:, g:g + 1]),
                 r=[bset, C.bidentf], w=[brt])
            S.op("act", lambda e: e.activation(out=rm_[:], in_=swapI[:], func=AF.Copy, scale=s512[:, g:g + 1]), r=[bset], w=[brm])
            S.op("dve", lambda e: e.tensor_tensor(out=rt[:], in0=rt[:], in1=rm_[:], op=ALU.add), r=[brt, brm], w=[brt])
            (sn, bsn), (cs, bcs), (ki_, bki) = SNt[k], CSt[k], kph[cnt["prep"] % 2]
            cnt["prep"] += 1
            S.op("act", lambda e: e.activation(out=sn[:], in_=iota[:], func=AF.Copy, scale=theta[:, g:g + 1]), r=[bset], w=[bsn])
            S.op("dve", lambda e: e.tensor_copy(out=ki_[:], in_=sn[:]), r=[bsn], w=[bki])
            S.op("dve", lambda e: e.tensor_tensor(out=sn[:], in0=sn[:], in1=ki_[:], op=ALU.subtract), r=[bki, bsn], w=[bsn])
            S.op("dve", lambda e: e.scalar_tensor_tensor(out=cs[:], in0=sn[:], scalar=-1.0, in1=sn[:], op0=ALU.mult, op1=ALU.max),
                 r=[bsn], w=[bcs])
            S.op("act", lambda e: e.activation(out=sn[:], in_=sn[:], func=AF.Sin, scale=TWO_PI), r=[bsn], w=[bsn])
            S.op("act", lambda e: e.activation(out=cs[:], in_=cs[:], func=AF.Sin, scale=-TWO_PI, bias=hp[:, 0:1]), r=[bcs, bset], w=[bcs])
            S.op("act", lambda e: e.activation(out=CSb[k][0][:], in_=cs[:], func=AF.Copy), r=[bcs], w=[CSb[k][1]])
            S.op("act", lambda e: e.activation(out=SNb[k][0][:], in_=sn[:], func=AF.Copy), r=[bsn], w=[SNb[k][1]])
            (dc_, bdc) = DEC[cnt["prep"] % 2]
            S.op("act", lambda e: e.activation(out=dc_[:], in_=riota[:], func=AF.Exp, scale=eq_[:, g:g + 1]), r=[bset], w=[bdc])
            S.op("dve", lambda e: e.tensor_tensor(out=MCS[k][0][:], in0=dc_[:], in1=cs[:], op=ALU.mult), r=[bdc, bcs], w=[MCS[k][1]])
            S.op("dve", lambda e: e.tensor_tensor(out=MSN[k][0][:], in0=dc_[:], in1=sn[:], op=ALU.mult), r=[bdc, bsn], w=[MSN[k][1]])

        PB3 = (0, 1, 3)

        def stage_a(itn, g, seg, z_t, bz):
            k, ix = g % NSET, itn % 4
            i1_, i2_ = PB3[(2 * itn) % 3], PB3[(2 * itn + 1) % 3]
            P1, bP1, P2, bP2 = C.ps[i1_], C.bps[i1_], C.ps[i2_], C.bps[i2_]
            zseg = z_t[:, seg * 512:(seg + 1) * 512]
            S.op("pe", lambda e: e.matmul(P1[:], bm[k][0][0][:], zseg, start=True, stop=True), r=[bm[k][0][1], bz], w=[bP1])
            S.op("pe", lambda e: e.matmul(P2[:], bm[k][1][0][:], zseg, start=True, stop=True), r=[bm[k][1][1], bz], w=[bP2])
            if seg < 4:
                (ac, bac), (jk, bjk) = accs[ix], junkS[itn % 2]
                S.op("dve", lambda e: e.memset(ac[:, 0:2], 0.0), w=[bac])
                S.op("dve", lambda e: e.scalar_tensor_tensor(out=jk[:], in0=P1[:], scalar=1.0, in1=MCS[k][0][:], op0=ALU.mult, op1=ALU.mult,
                                                             accum_out=ac[:, 0:1]), r=[bP1, MCS[k][1]], w=[bjk, bac])
                S.op("dve", lambda e: e.scalar_tensor_tensor(out=jk[:], in0=P2[:], scalar=1.0, in1=MSN[k][0][:], op0=ALU.mult, op1=ALU.mult,
                                                             accum_out=ac[:, 1:2]), r=[bP2, MSN[k][1]], w=[bjk, bac])
                return
            (a1, ba1), (a2, ba2) = t1[ix], t2[ix]
            S.op("dve", lambda e: e.tensor_tensor(out=a1[:], in0=P1[:], in1=CSt[k][0][:], op=ALU.mult), r=[bP1, CSt[k][1]], w=[ba1])
            S.op("dve", lambda e: e.tensor_tensor(out=a2[:], in0=P2[:], in1=SNt[k][0][:], op=ALU.mult), r=[bP2, SNt[k][1]], w=[ba2])
            S.op("dve", lambda e: e.tensor_tensor(out=a1[:], in0=a1[:], in1=a2[:], op=ALU.add), r=[ba1, ba2], w=[ba1])

        def stage_b(itn, g, seg, g8):
            k, ix = g % NSET, itn % 4
            if seg < 4:
                (ac, bac) = accs[ix]
                S.op("dve", lambda e: e.tensor_tensor(out=ac[:, 2:3], in0=ac[:, 0:1], in1=ac[:, 1:2], op=ALU.add), r=[bac], w=[bac])
                if seg > 0:
                    S.op("dve", lambda e: e.scalar_tensor_tensor(out=ac[:, 3:4], in0=ini[ix][0][:, 0:1], scalar=mag512[:, g:g + 1],
                                                                 in1=ac[:, 2:3], op0=ALU.mult, op1=ALU.add), r=[bac, ini[ix][1], bset], w=[bac])
                    vend = ac[:, 3:4]
                else:
                    vend = ac[:, 2:3]
                col = itn % 8
                S.op("pe", lambda e: e.matmul(RB[:, col:col + 1], ROT[k][0][:], vend, start=True, stop=True), r=[ROT[k][1], bac], w=[bRB])
                nx_ = ini[(itn + 2) % 4]
                S.op("act", lambda e: e.activation(out=nx_[0][:], in_=RB[:, col:col + 1], func=AF.Copy), r=[bRB], w=[nx_[1]])
                return
            (a1, ba1), (v_t, bv) = t1[ix], vv[ix]
            if seg == 0:
                init, rr = 0.0, [ba1, bset]
            else:
                init, rr = ini[ix][0][:, 0:1], [ba1, bset, ini[ix][1]]
            S.op("dve", lambda e: e.tensor_tensor_scan(out=v_t[:], data0=mag[:, g:g + 1].broadcast_to([128, 512]), data1=a1[:],
                                                       initial=init, op0=ALU.mult, op1=ALU.add), r=rr, w=[bv])
            if seg < 7:
                col = itn % 8
                S.op("pe", lambda e: e.matmul(RB[:, col:col + 1], ROT[k][0][:], v_t[:, 511:512], start=True, stop=True),
                     r=[ROT[k][1], bv], w=[bRB])
                nx_ = ini[(itn + 2) % 4]
                S.op("act", lambda e: e.activation(out=nx_[0][:], in_=RB[:, col:col + 1], func=AF.Copy), r=[bRB], w=[nx_[1]])
            if seg >= 4:
                (x1, bx1), (x2, bx2) = q1[ix], q2[ix]
                (vb_, bvb) = vb[ix]
                S.op("act", lambda e: e.activation(out=vb_[:], in_=v_t[:], func=AF.Copy), r=[bv], w=[bvb])
                S.op("dve", lambda e: e.tensor_tensor(out=x1[:], in0=vb_[:], in1=CSb[k][0][:], op=ALU.mult), r=[bvb, CSb[k][1]], w=[bx1])
                S.op("dve", lambda e: e.tensor_tensor(out=x2[:], in0=vb_[:], in1=SNb[k][0][:], op=ALU.mult), r=[bvb, SNb[k][1]], w=[bx2])

        def stage_c(itn, g, seg, g8):
            k, ix = g % NSET, itn % 4
            if seg >= 4:
                (x1, bx1), (x2, bx2) = q1[ix], q2[ix]
                Y, bY = C.ps[4 + seg - 4], C.bps[4 + seg - 4]
                S.op("pe", lambda e: e.matmul(Y[:], bm[k][2][0][:], x1[:], start=False, stop=False), r=[bm[k][2][1], bx1], w=[bY])
                S.op("pe", lambda e: e.matmul(Y[:], bm[k][3][0][:], x2[:], start=False, stop=(g8 == 7)), r=[bm[k][3][1], bx2], w=[bY])

        prep_group(gb_list[0] * 8)
        prep_group(gb_list[0] * 8 + 1)
        for gi, gb in enumerate(gb_list):
            z_t, bz = zt[gi % 2]
            S.dma("sp", z_t[:], T["ZT"][gb * 128:(gb + 1) * 128, :], w=[bz])
            for so_ in range(4):
                S.op("pe", lambda e: e.matmul(C.ps[4 + so_][:], DST[:, gb, :], z_t[:, 2048 + so_ * 512:2048 + (so_ + 1) * 512],
                                              start=True, stop=False), r=[bset, bz], w=[C.bps[4 + so_]])
            prev = None
            prev2 = None
            for pr in range(4):
                pair = (gb * 8 + pr * 2, gb * 8 + pr * 2 + 1)
                for seg in range(8):
                    for g in pair:
                        itn = cnt["it"]
                        cnt["it"] += 1
                        stage_a(itn, g, seg, z_t, bz)
                        if prev is not None:
                            stage_b(*prev)
                        if prev2 is not None:
                            stage_c(*prev2)
                        prev2 = prev
                        prev = (itn, g, seg, g % 8)
                    if seg == 3:
                        if pr < 3:
                            nxt = (pair[0] + 2, pair[1] + 2)
                        elif gi + 1 < len(gb_list):
                            nxt = (gb_list[gi + 1] * 8, gb_list[gi + 1] * 8 + 1)
                        else:
                            nxt = ()
                        for g_ in nxt:
                            prep_group(g_)
                    if CONV_INTERLEAVE and seg in (1, 3, 5, 7) and conv_jobs:
                        o_ap, i_ap = conv_jobs.pop(0)
                        S.dma("pool", o_ap, i_ap)
            stage_b(*prev)
            stage_c(*prev2)
            stage_c(*prev)
            for so_ in range(4):
                y_t, by = ysb[cnt["y"] % 2]
                cnt["y"] += 1
                S.op("act", lambda e: e.activation(out=y_t[:], in_=C.ps[4 + so_][:], func=GELU), r=[C.bps[4 + so_]], w=[by])
                S.dma("sp", T["YG"][gb * 128:(gb + 1) * 128, so_ * 512:(so_ + 1) * 512], y_t[:], r=[by])
        while conv_jobs:
            o_ap, i_ap = conv_jobs.pop(0)
            S.dma("pool", o_ap, i_ap)
        S.barrier()


def rstd_ops(S, src_ap, dst, bdst, tmp, n, scale, rsrc):
    S.op("dve", lambda e: e.tensor_scalar(out=tmp[:, 0:n], in0=src_ap, scalar1=scale, scalar2=EPS, op0=ALU.mult, op1=ALU.add),
         r=list(rsrc) + [bdst], w=[bdst])
    S.op("act", lambda e: e.activation(out=tmp[:, n:2 * n], in_=tmp[:, 0:n], func=AF.Sqrt), r=[bdst], w=[bdst])
    S.op("dve", lambda e: e.reciprocal(out=dst, in_=tmp[:, n:2 * n]), r=[bdst], w=[bdst])


def phase_b(C, st_list, stop_after=None):
    nc, S, T = C.nc, C.S, C.T
    with contextlib.ExitStack() as pbs:
        raw = lambda name, shape, dt: pbs.enter_context(nc.sbuf_tensor(_uname("sr_" + name), list(shape), dt))
        bK = Buf("bconst")
        so = lambda k, fn: S.op(k, fn, r=[bK], w=[bK])
        wmT = raw("wmT", [128, 8, 128], BF16)
        bsr = raw("bsr", [128, 8, 128], F32)
        BIAS = raw("BIAS", [128, 16, 128], F32)
        cols = raw("cols", [128, 4, 16], F32)
        ones = raw("ones", [128, 128], BF16)
        S.dma("pool", wmT[:], T["wmT"][:], w=[bK])
        S.dma("sp", bsr[:], T["bs_rep"][:], w=[bK])
        S.dma("sp", cols[:], T["gm_cols"][:], w=[bK])
        so("dve", lambda e: e.memset(ones[:], 1.0))
        so("dve", lambda e: e.memset(wmT[64:128, :, 0:64], 0.0))
        for hh in range(8):
            bank = C.ps[hh // 4]
            S.op("pe", lambda e: e.matmul(bank[:, (hh % 4) * 128:(hh % 4 + 1) * 128], ones[:], wmT[:, hh, :], start=True, stop=True),
                 r=[bK], w=[C.bps[hh // 4]])
        for ct in range(16):
            hh = ct // 2
            bank = C.ps[hh // 4]
            S.op("dve", lambda e: e.scalar_tensor_tensor(out=BIAS[:, ct, :], in0=bank[:, (hh % 4) * 128:(hh % 4 + 1) * 128],
                                                         scalar=cols[:, 1, ct:ct + 1], in1=bsr[:, hh, :], op0=ALU.mult, op1=ALU.add),
                 r=[bK, C.bps[hh // 4]], w=[bK])
        hT = [_tile(nc, pbs, f"h{i}", [128, D], F32) for i in range(4)]
        rstd, brstd = _tile(nc, pbs, "rstdAB", [128, 8], F32)
        rtmp = raw("rtmp", [128, 16], F32)
        rtmp2 = raw("rtmp2", [128, 16], F32)
        wglu_v = T["w_glu"].rearrange("(c p) n -> p c n", p=128)
        wout_v = T["w_out"].rearrange("(c p) n -> p c n", p=128)
        nb = 0
        for st in st_list:
            t0 = st * TS
            with contextlib.ExitStack() as b1:
                tl = lambda name, shape, dt: _tile(nc, b1, name, shape, dt)
                yT, byT = _tile(nc, b1, "yT", [128, 32, TS], BF16)
                with contextlib.ExitStack() as b1a:
                    tla = lambda name, shape, dt: _tile(nc, b1a, name, shape, dt)
                    ygT, bygT = tla("ygT", [128, 16, TS], BF16)
                    uT, buT = tla("uT", [128, 16, TS], BF16)
                    wg = [tla(f"wg{i}", [128, 16, 256], BF16) for i in range(2)]
                    sig = [tla(f"sig{i}", [128, TS], F32) for i in range(3)]
                    ypre = [tla(f"ypre{i}", [128, TS], BF16) for i in range(3)]
                    ysq = [tla(f"ysq{i}", [128, TS], BF16) for i in range(3)]
                    vnt = [tla(f"vnt{i}", [128, 2048], BF16) for i in range(2)]
                    S.dma("sp", ygT[:], T["YG"].rearrange("(c p) t -> p c t", p=128)[:, :, t0:t0 + TS], w=[bygT])
                    S.dma("sp", uT[:], T["UT"].rearrange("(c p) t -> p c t", p=128)[:, :, t0:t0 + TS], w=[buT])
                    SSQ, bSSQ = C.ps[7], C.bps[7]
                    n2 = 0
                    deferred = []
                    for oc2 in range(8):
                        (w_t, bw) = wg[oc2 % 2]
                        S.dma("pool", w_t[:], wglu_v[:, :, oc2 * 256:(oc2 + 1) * 256], w=[bw])
                        for sub in range(2):
                            oc = oc2 * 2 + sub
                            pb_, bpb = C.ps[nb % 6], C.bps[nb % 6]
                            nb += 1
                            for ci in range(16):
                                S.op("pe", lambda e: e.matmul(pb_[:], w_t[:, ci, sub * 128:(sub + 1) * 128], ygT[:, ci, :],
                                                              start=(ci == 0), stop=(ci == 15)), r=[bw, bygT], w=[bpb], signal=(ci == 15))
                            while len(deferred) > 1:
                                deferred.pop(0)()
                            (sg_, bsg), (yp, byp), (yq, byq) = sig[n2 % 3], ypre[n2 % 3], ysq[n2 % 3]
                            n2 += 1
                            S.op("act", lambda e: e.activation(out=sg_[:], in_=pb_[:], func=AF.Sigmoid), r=[bpb], w=[bsg])
                            S.op("dve", lambda e: e.tensor_tensor(out=yp[:], in0=ygT[:, oc, :], in1=sg_[:], op=ALU.mult), r=[bygT, bsg], w=[byp])
                            S.op("act", lambda e: e.activation(out=yq[:], in_=yp[:], func=AF.Square), r=[byp], w=[byq])
                            S.op("dve", lambda e: e.tensor_scalar(out=yT[:, oc, :], in0=yp[:], scalar1=cols[:, 2, oc:oc + 1], scalar2=None,
                                                                  op0=ALU.mult), r=[byp, bK], w=[byT])
                            def ssq_a(oc=oc, yq=yq, byq=byq):
                                for tt in range(4):
                                    S.op("pe", lambda e: e.matmul(SSQ[:, oc * 4 + tt:oc * 4 + tt + 1], yq[:, tt * 128:(tt + 1) * 128],
                                                                  ones[:, 0:1], start=True, stop=True), r=[byq, bK], w=[bSSQ])
                            deferred.append(ssq_a)
                    for tt in range(4):
                        (v_t, bv) = vnt[tt % 2]
                        S.dma("sp", v_t[:], T["VN"][t0 + tt * 128:t0 + (tt + 1) * 128, :], w=[bv])
                        for cq in range(4):
                            pb_, bpb = C.ps[nb % 6], C.bps[nb % 6]
                            nb += 1
                            for j in range(4):
                                ct = cq * 4 + j
                                S.op("pe", lambda e: e.matmul(pb_[:, j * 128:(j + 1) * 128], v_t[:, ct * 128:(ct + 1) * 128], wmT[:, ct // 2, :],
                                                              start=True, stop=True), r=[bv, bK], w=[bpb], signal=(j == 3))
                            while len(deferred) > 1:
                                deferred.pop(0)()
                            (sg_, bsg), (yp, byp), (yq, byq) = sig[n2 % 3], ypre[n2 % 3], ysq[n2 % 3]
                            n2 += 1
                            for j in range(4):
                                ct = cq * 4 + j
                                S.op("dve", lambda e: e.scalar_tensor_tensor(out=sg_[:, j * 128:(j + 1) * 128], in0=pb_[:, j * 128:(j + 1) * 128],
                                                                             scalar=cols[:, 0, ct:ct + 1], in1=BIAS[:, ct, :],
                                                                             op0=ALU.mult, op1=ALU.add), r=[bpb, bK], w=[bsg])
                            S.op("dve", lambda e: e.tensor_tensor(out=yp[:].rearrange("p (j t) -> p j t", j=4),
                                                                  in0=sg_[:].rearrange("p (j t) -> p j t", j=4),
                                                                  in1=uT[:, cq * 4:(cq + 1) * 4, tt * 128:(tt + 1) * 128], op=ALU.mult),
                                 r=[bsg, buT], w=[byp])
                            S.op("act", lambda e: e.activation(out=yq[:], in_=yp[:], func=AF.Square), r=[byp], w=[byq])
                            for j in range(4):
                                ct = cq * 4 + j
                                S.op("act", lambda e: e.activation(out=yT[:, 16 + ct, tt * 128:(tt + 1) * 128], in_=yp[:, j * 128:(j + 1) * 128],
                                                                   func=AF.Copy, scale=cols[:, 3, ct:ct + 1]), r=[byp, bK], w=[byT])

                            def ssq_b(cq=cq, tt=tt, yq=yq, byq=byq):
                                for j in range(4):
                                    ct = cq * 4 + j
                                    S.op("pe", lambda e: e.matmul(SSQ[:, 64 + ct * 4 + tt:64 + ct * 4 + tt + 1], yq[:, j * 128:(j + 1) * 128],
                                                                  ones[:, 0:1], start=True, stop=True), r=[byq, bK], w=[bSSQ])
                            deferred.append(ssq_b)
                    while deferred:
                        deferred.pop(0)()
                    S.op("dve", lambda e: e.reduce_sum(out=rtmp[:, 8:16].rearrange("p (a t) -> p a t", a=2),
                                                       in_=SSQ[:, 0:128].rearrange("p (a o t) -> p a t o", a=2, t=4),
                                                       axis=AX.X), r=[bSSQ, brstd], w=[brstd])
                    rstd_ops(S, rtmp[:, 8:16], rstd[:], brstd, rtmp2, 8, 1.0 / 2048, [])
                    if "YTd" in T:
                        S.dma("sp", T["YTd"].rearrange("(c p) t -> p c t", p=128), yT[:], r=[byT], is_output=True)
                        S.dma("sp", T["RSd"][:], rstd[:], r=[brstd], is_output=True)
                    S.barrier()
                with contextlib.ExitStack() as b2:
                    tlb = lambda name, shape, dt: _tile(nc, b2, name, shape, dt)
                    wo = [tlb(f"wo{i}", [128, 32, 512], BF16) for i in range(2)]
                    xs = [tlb(f"xs{i}", [128, 512], F32) for i in range(2)]
                    tm = [tlb(f"tm{i}", [128, 512], F32) for i in range(2)]
                    n3 = 0
                    for ds in range(8):
                        (w_t, bw) = wo[ds % 2]
                        S.dma("pool", w_t[:], T["WoS"][ds], w=[bw])
                        for tt in range(4):
                            PA, bPA = C.ps[nb % 8], C.bps[nb % 8]
                            PB, bPB = C.ps[(nb + 1) % 8], C.bps[(nb + 1) % 8]
                            nb += 2
                            for ci in range(16):
                                S.op("pe", lambda e: e.matmul(PA[:], yT[:, ci, tt * 128:(tt + 1) * 128], w_t[:, ci, :],
                                                              start=(ci == 0), stop=(ci == 15)), r=[byT, bw], w=[bPA], signal=(ci == 15))
                            for ci in range(16, 32):
                                S.op("pe", lambda e: e.matmul(PB[:], yT[:, ci, tt * 128:(tt + 1) * 128], w_t[:, ci, :],
                                                              start=(ci == 16), stop=(ci == 31)), r=[byT, bw], w=[bPB], signal=(ci == 31))
                            (x_t, bx), (t_t, bt) = xs[n3 % 2], tm[n3 % 2]
                            n3 += 1
                            S.dma("sp", x_t[:], T["x_own"][t0 + tt * 128:t0 + (tt + 1) * 128, ds * 512:(ds + 1) * 512], w=[bx])
                            S.op("dve", lambda e: e.scalar_tensor_tensor(out=t_t[:], in0=PA[:], scalar=rstd[:, tt:tt + 1], in1=x_t[:],
                                                                         op0=ALU.mult, op1=ALU.add), r=[bPA, brstd, bx], w=[bt])
                            S.op("dve", lambda e: e.scalar_tensor_tensor(out=hT[tt][0][:, ds * 512:(ds + 1) * 512], in0=PB[:],
                                                                         scalar=rstd[:, 4 + tt:5 + tt], in1=t_t[:],
                                                                         op0=ALU.mult, op1=ALU.add), r=[bPB, brstd, bt], w=[hT[tt][1]])
                    S.barrier()
            if stop_after == "B2":
                for tt in range(4):
                    S.dma("sp", T["out"][t0 + tt * 128:t0 + (tt + 1) * 128, :], hT[tt][0][:], r=[hT[tt][1]], is_output=True)
                S.barrier()
                continue
            with contextlib.ExitStack() as pk:
                xnT, bxnT = _tile(nc, pk, "xnT", [128, 32, TS], BF16)
                with contextlib.ExitStack() as b34:
                    qT, bqT = _tile(nc, b34, "qT", [128, 16, TS], BF16)
                    with contextlib.ExitStack() as b3:
                        tl = lambda name, shape, dt: _tile(nc, b3, name, shape, dt)
                        gffn, bgffn = tl("gffn", [128, D], F32)
                        S.dma("sp", gffn[:], T["gffn_r"][:], w=[bgffn])
                        xn = [tl(f"xn{i}", [128, D], BF16) for i in range(2)]
                        junk, bjunk = tl("junkB", [128, D], BF16)
                        s8 = [tl(f"s8b{i}", [128, 8], F32) for i in range(2)]
                        wq = [tl(f"wq{i}", [128, 32, 256], BF16) for i in range(2)]
                        for tt in range(4):
                            (x_t, bx), (s_, bs_) = xn[tt % 2], s8[tt % 2]
                            h_t, bh = hT[tt]
                            S.op("dve", lambda e: e.memset(s_[:], 0.0), w=[bs_])
                            S.op("act", lambda e: e.activation(out=junk[:], in_=h_t[:], func=AF.Square, accum_out=s_[:, 0:1]),
                                 r=[bh], w=[bjunk, bs_])
                            rstd_ops(S, s_[:, 0:1], s_[:, 1:2], bs_, s_[:, 2:4], 1, 1.0 / D, [])
                            S.op("dve", lambda e: e.scalar_tensor_tensor(out=x_t[:], in0=h_t[:], scalar=s_[:, 1:2], in1=gffn[:],
                                                                         op0=ALU.mult, op1=ALU.mult), r=[bh, bs_, bgffn], w=[bx])
                            for dcg in range(4):
                                pb_, bpb = C.ps[nb % 8], C.bps[nb % 8]
                                nb += 1
                                pbb = pb_[:].bitcast(BF16)
                                for j in range(8):
                                    dc = dcg * 8 + j
                                    S.op("pe", lambda e: e.transpose(out=pbb[:, j * 128:(j + 1) * 128], in_=x_t[:, dc * 128:(dc + 1) * 128],
                                                                     identity=C.identb[:]), r=[bx, C.bidentb], w=[bpb], signal=(j == 7))
                                src = pbb[:, 0:1024].rearrange("p (j t) -> p j t", j=8)
                                dst = xnT[:, dcg * 8:(dcg + 1) * 8, tt * 128:(tt + 1) * 128]
                                if dcg % 2 == 0:
                                    S.op("act", lambda e: e.activation(out=dst, in_=src, func=AF.Copy), r=[bpb], w=[bxnT])
                                else:
                                    S.op("dve", lambda e: e.tensor_copy(out=dst, in_=src), r=[bpb], w=[bxnT])
                        wq_v = T["w_q"].rearrange("(c p) n -> p c n", p=128)
                        for qc in range(8):
                            (w_t, bw) = wq[qc % 2]
                            S.dma("pool", w_t[:], T["WqS"][qc], w=[bw])
                            for sub in range(2):
                                pb_, bpb = C.ps[nb % 8], C.bps[nb % 8]
                                nb += 1
                                for dc in range(32):
                                    S.op("pe", lambda e: e.matmul(pb_[:], w_t[:, dc, sub * 128:(sub + 1) * 128], xnT[:, dc, :],
                                                                  start=(dc == 0), stop=(dc == 31)), r=[bw, bxnT], w=[bpb], signal=(dc == 31))
                                S.op("act", lambda e: e.activation(out=qT[:, qc * 2 + sub, :], in_=pb_[:], func=AF.Copy), r=[bpb], w=[bqT])
                        if "Qd" in T:
                            S.dma("sp", T["Qd"].rearrange("(c p) t -> p c t", p=128), qT[:], r=[bqT], is_output=True)
                        S.barrier()
                    with contextlib.ExitStack() as b4:
                        raw4 = lambda name, shape, dt: b4.enter_context(nc.sbuf_tensor(_uname("sr_" + name), list(shape), dt))
                        bR = Buf("route")
                        ro = lambda k, fn, extra_r=(), extra_w=(): S.op(k, fn, r=[bR] + list(extra_r), w=[bR] + list(extra_w))
                        S1 = [raw4(f"S{a}sb", [128, 8, 128], F32) for a in range(2)]
                        Vv = [raw4(f"V{a}", [128, 8, 16], F32) for a in range(2)]
                        Iu = [raw4(f"I{a}u", [128, 8, 16], U32) for a in range(2)]
                        If_ = [raw4(f"I{a}f", [128, 8, 16], F32) for a in range(2)]
                        tmp16 = raw4("tmp16", [128, 16, 128], F32)
                        tmpc = raw4("tmpc", [128, 8, 256], F32)
                        bAH = [[Buf(f"ah{a}{h}") for h in range(8)] for a in range(2)]
                        bH = [Buf(f"h{h}") for h in range(8)]
                        cand = raw4("cand", [128, 8, 256], F32)
                        SC = raw4("SC", [128, 8, 16], F32)
                        CIu = raw4("CIu", [128, 8, 16], U32)
                        CIf = raw4("CIf", [128, 8, 16], F32)
                        ii = raw4("ii", [128, 8, 16], mybir.dt.int32)
                        irf = raw4("irf", [128, 8, 16], F32)
                        jrf = raw4("jrf", [128, 8, 16], F32)
                        ex = raw4("ex", [128, 8, 16], F32)
                        zz = raw4("zz", [128, 16], F32)
                        gate = raw4("gate", [128, 8, 16], F32)
                        e12 = [raw4(f"e{a}r", [128, 8, 16], F32) for a in range(2)]
                        tr3 = raw4("tr3", [128, 3, 128], F32)
                        ne2 = raw4("ne2", [128, 128], F32)
                        onef = raw4("onef", [128, 1], F32)
                        sqt = [_tile(nc, b4, f"sqt{i}", [128, 128], F32) for i in range(2)]
                        ro("dve", lambda e: e.memset(onef[:], 1.0))
                        At = [_tile(nc, b4, f"At{i}", [128, 128], BF16) for i in range(4)]
                        Bt = [_tile(nc, b4, f"Bt{i}", [128, 128], BF16) for i in range(4)]
                        GTt = [_tile(nc, b4, f"GTt{i}", [128, 128, 128], BF16) for i in range(1)]
                        io16 = C.iota128[:, 0:16]
                        for tt in range(4):
                            tsl = slice(tt * 128, (tt + 1) * 128)
                            for hh in range(8):
                                for a in range(2):
                                    bk = a * 2 + hh // 4
                                    S.op("pe", lambda e: e.matmul(C.ps[bk][:, (hh % 4) * 128:(hh % 4 + 1) * 128], qT[:, 2 * hh + a, tsl],
                                                                  C.kT[a][:], start=True, stop=True), r=[bqT, C.bkT], w=[C.bps[bk]])
                            for a in range(2):
                                for hb in range(2):
                                    ro("act", lambda e: e.activation(out=S1[a][:, hb * 4:(hb + 1) * 4, :],
                                                                     in_=C.ps[a * 2 + hb][:].rearrange("p (h n) -> p h n", h=4), func=AF.Copy),
                                       extra_r=[C.bps[a * 2 + hb]])
                            ah = [(a, hh) for a in range(2) for hh in range(8)]
                            for (a, hh) in ah:
                                S.op("dve", lambda e: e.max(out=Vv[a][:, hh, 0:8], in_=S1[a][:, hh, :]), r=[bR], w=[bAH[a][hh]])
                            for (a, hh) in ah:
                                S.op("dve", lambda e: e.max_index(out=Iu[a][:, hh, 0:8], in_max=Vv[a][:, hh, 0:8], in_values=S1[a][:, hh, :]),
                                     r=[bR, bAH[a][hh]], w=[bAH[a][hh]])
                            for (a, hh) in ah:
                                S.op("dve", lambda e: e.match_replace(out=tmp16[:, a * 8 + hh, :], in_to_replace=Vv[a][:, hh, 0:8],
                                                                      in_values=S1[a][:, hh, :], imm_value=-1e30), r=[bR, bAH[a][hh]], w=[bAH[a][hh]])
                            for (a, hh) in ah:
                                S.op("dve", lambda e: e.max(out=Vv[a][:, hh, 8:16], in_=tmp16[:, a * 8 + hh, :]), r=[bAH[a][hh]], w=[bAH[a][hh]])
                            for (a, hh) in ah:
                                S.op("dve", lambda e: e.max_index(out=Iu[a][:, hh, 8:16], in_max=Vv[a][:, hh, 8:16], in_values=tmp16[:, a * 8 + hh, :]),
                                     r=[bAH[a][hh]], w=[bAH[a][hh]])
                            for a in range(2):
                                ro("dve", lambda e: e.tensor_copy(out=If_[a][:], in_=Iu[a][:]), extra_r=bAH[a], extra_w=bAH[a])
                            for hh in range(8):
                                S.op("dve", lambda e: e.tensor_tensor(out=cand[:, hh, :].rearrange("p (i j) -> p i j", i=16),
                                                                      in0=Vv[0][:, hh, :].unsqueeze(2).broadcast_to([128, 16, 16]),
                                                                      in1=Vv[1][:, hh, :].unsqueeze(1).broadcast_to([128, 16, 16]), op=ALU.add),
                                     r=[bAH[0][hh], bAH[1][hh], bR], w=[bH[hh]])
                            for hh in range(8):
                                S.op("dve", lambda e: e.max(out=SC[:, hh, 0:8], in_=cand[:, hh, :]), r=[bH[hh]], w=[bH[hh]])
                            for hh in range(8):
                                S.op("dve", lambda e: e.max_index(out=CIu[:, hh, 0:8], in_max=SC[:, hh, 0:8], in_values=cand[:, hh, :]), r=[bH[hh]], w=[bH[hh]])
                            for hh in range(8):
                                S.op("dve", lambda e: e.match_replace(out=tmpc[:, hh, :], in_to_replace=SC[:, hh, 0:8], in_values=cand[:, hh, :],
                                                                      imm_value=-1e30), r=[bH[hh], bR], w=[bH[hh]])
                            for hh in range(8):
                                S.op("dve", lambda e: e.max(out=SC[:, hh, 8:16], in_=tmpc[:, hh, :]), r=[bH[hh]], w=[bH[hh]])
                            for hh in range(8):
                                S.op("dve", lambda e: e.max_index(out=CIu[:, hh, 8:16], in_max=SC[:, hh, 8:16], in_values=tmpc[:, hh, :]), r=[bH[hh]], w=[bH[hh]])
                            ro("dve", lambda e: e.tensor_copy(out=CIf[:], in_=CIu[:]), extra_r=bH, extra_w=bH)
                            ro("dve", lambda e: e.tensor_tensor(out=ex[:], in0=SC[:], in1=SC[:, :, 0:1].broadcast_to([128, 8, 16]), op=ALU.subtract))
                            ro("act", lambda e: e.activation(out=ex[:], in_=ex[:], func=AF.Exp))
                            ro("dve", lambda e: e.reduce_sum(out=zz[:, 0:8], in_=ex[:], axis=AX.X))
                            ro("dve", lambda e: e.reciprocal(out=zz[:, 8:16], in_=zz[:, 0:8]))
                            ro("dve", lambda e: e.tensor_tensor(out=gate[:], in0=ex[:], in1=zz[:, 8:16].unsqueeze(2).broadcast_to([128, 8, 16]),
                                                                op=ALU.mult))
                            ro("dve", lambda e: e.tensor_scalar(out=ii[:], in0=CIf[:], scalar1=1.0 / 16, scalar2=-0.46875, op0=ALU.mult, op1=ALU.add))
                            ro("dve", lambda e: e.tensor_copy(out=irf[:], in_=ii[:]))
                            ro("dve", lambda e: e.scalar_tensor_tensor(out=jrf[:], in0=irf[:], scalar=-16.0, in1=CIf[:], op0=ALU.mult, op1=ALU.add))
                            for a, sel in ((0, irf), (1, jrf)):
                                for hh in range(8):
                                    ro("dve", lambda e: e.tensor_tensor(out=tmpc[:, hh, :].rearrange("p (k i) -> p k i", k=16), in0=sel[:, hh, :].unsqueeze(2).broadcast_to([128, 16, 16]),
                                                                        in1=io16.unsqueeze(1).broadcast_to([128, 16, 16]), op=ALU.is_equal))
                                    ro("dve", lambda e: e.tensor_tensor(out=tmpc[:, hh, :].rearrange("p (k i) -> p k i", k=16), in0=tmpc[:, hh, :].rearrange("p (k i) -> p k i", k=16),
                                                                        in1=If_[a][:, hh, :].unsqueeze(1).broadcast_to([128, 16, 16]), op=ALU.mult))
                                ro("dve", lambda e: e.reduce_sum(out=e12[a][:].rearrange("p h k -> p (h k)"),
                                                                 in_=tmpc[:].rearrange("p h (k i) -> p (h k) i", k=16), axis=AX.X))
                            TRB, bTRB = C.ps[4], C.bps[4]
                            for n_, src in enumerate((e12[0], e12[1], gate)):
                                ro("pe", lambda e: e.transpose(out=TRB[:, n_ * 128:(n_ + 1) * 128], in_=src[:].rearrange("p h k -> p (h k)"),
                                                               identity=C.identf[:]), extra_r=[C.bidentf], extra_w=[bTRB])
                            ro("act", lambda e: e.activation(out=tr3[:].rearrange("p a t -> p (a t)"), in_=TRB[:, 0:384], func=AF.Copy), extra_r=[bTRB])
                            if "R3d" in T and tt == 0:
                                S.dma("sp", T["R3d"][:], tr3[:], r=[bR], is_output=True)
                                S.dma("sp", T["S1d"][:], S1[0][:], r=[bR], is_output=True)
                                S.dma("sp", T["V1d"][:], Vv[0][:], r=[bR], is_output=True)
                                S.dma("sp", T["SCd"][:], SC[:], r=[bR], is_output=True)
                                S.dma("sp", T["CId"][:], CIf[:], r=[bR], is_output=True)
                                S.dma("sp", T["I1d"][:], If_[0][:], r=[bR], is_output=True)
                            ro("dve", lambda e: e.tensor_scalar(out=ne2[:], in0=tr3[:, 1, :], scalar1=-1.0, scalar2=None, op0=ALU.mult))
                            g_t, bg = GTt[0]
                            for t4 in range(32):
                                pb_, bpb = C.ps[5 + t4 % 3], C.bps[5 + t4 % 3]
                                for tk in range(4):
                                    t = t4 * 4 + tk
                                    (a_t, ba), (b_t, bb) = At[t % 4], Bt[t % 4]
                                    S.op("dve", lambda e: e.tensor_scalar(out=a_t[:], in0=C.iota128[:], scalar1=tr3[:, 0, t:t + 1],
                                                                          scalar2=tr3[:, 2, t:t + 1], op0=ALU.is_equal, op1=ALU.mult),
                                         r=[bR, C.biota], w=[ba])
                                    (sq_, bsq) = sqt[t % 2]
                                    S.op("act", lambda e: e.activation(out=sq_[:], in_=C.iota128[:], func=AF.Square, bias=ne2[:, t:t + 1]),
                                         r=[bR, C.biota], w=[bsq])
                                    S.op("act", lambda e: e.activation(out=b_t[:], in_=sq_[:], func=AF.Relu, scale=-1.0, bias=onef[:, 0:1]),
                                         r=[bsq, bR], w=[bb])
                                    S.op("pe", lambda e: e.matmul(pb_[:, tk * 128:(tk + 1) * 128], b_t[:], a_t[:], start=True, stop=True),
                                         r=[ba, bb], w=[bpb])
                                S.op("act", lambda e: e.activation(out=g_t[:, :, t4 * 4:(t4 + 1) * 4].rearrange("p e t -> p t e"),
                                                                   in_=pb_[:].rearrange("p (t e) -> p t e", t=4), func=AF.Copy), r=[bpb], w=[bg])
                            S.dma("sp", T["GT"][st * 4 + tt], g_t[:], r=[bg])
                        S.barrier()
                if stop_after == "B4":
                    continue
                with contextlib.ExitStack() as b5:
                    tl = lambda name, shape, dt: _tile(nc, b5, name, shape, dt)
                    Dn = [tl(f"Dn{i}", [128, 32, 256], BF16) for i in range(2)]
                    Up = [tl(f"Up{i}", [128, 8, 512], BF16) for i in range(2)]
                    act = [tl(f"act{i}", [128, 8, TS], BF16) for i in range(2)]
                    Gc = [tl(f"Gc{i}", [128, 8, TS], BF16) for i in range(2)]
                    gel = [tl(f"gel{i}", [128, TS], BF16) for i in range(2)]
                    dn_v = T["downT"].rearrange("(c p) e -> p c e", p=128)
                    up_v = T["up"].rearrange("(c p) d -> p c d", p=128)
                    nd = nu = ng = 0
                    first_pass = False
                    for cg in range(C.ncg):
                        (g_c, bgc), (a_c, bac) = Gc[cg % 2], act[cg % 2]
                        for tt in range(4):
                            S.dma("sp", g_c[:, :, tt * 128:(tt + 1) * 128], T["GT"][st * 4 + tt][:, cg * 8:(cg + 1) * 8, :], w=[bgc])
                        for cp in range(4):
                            (d_t, bd) = Dn[nd % 2]
                            nd += 1
                            e0 = (cg * 8 + cp * 2) * 128
                            if first_pass:
                                S.dma("pool", d_t[:], dn_v[:, :, e0:e0 + 256], w=[bd])
                                S.dma("sp", T["DnS"][cg * 4 + cp], d_t[:], r=[bd])
                            else:
                                S.dma("sp", d_t[:], T["DnS"][cg * 4 + cp], w=[bd])
                            for ck in range(2):
                                ci = cp * 2 + ck
                                pb_, bpb = C.ps[nb % 8], C.bps[nb % 8]
                                nb += 1
                                for dc in range(32):
                                    S.op("pe", lambda e: e.matmul(pb_[:], d_t[:, dc, ck * 128:(ck + 1) * 128], xnT[:, dc, :],
                                                                  start=(dc == 0), stop=(dc == 31)), r=[bd, bxnT], w=[bpb], signal=(dc == 31))
                                (ge, bge) = gel[ng % 2]
                                ng += 1
                                S.op("act", lambda e: e.activation(out=ge[:], in_=pb_[:], func=GELU), r=[bpb], w=[bge])
                                S.op("dve", lambda e: e.tensor_tensor(out=a_c[:, ci, :], in0=ge[:], in1=g_c[:, ci, :], op=ALU.mult),
                                     r=[bge, bgc], w=[bac])
                        for ds in range(8):
                            (u_t, bu) = Up[nu % 2]
                            nu += 1
                            if first_pass:
                                S.dma("pool", u_t[:], up_v[:, cg * 8:(cg + 1) * 8, ds * 512:(ds + 1) * 512], w=[bu])
                                S.dma("sp", T["UpS"][cg * 8 + ds], u_t[:], r=[bu])
                            else:
                                S.dma("pool", u_t[:], T["UpS"][cg * 8 + ds], w=[bu])
                            for tt in range(4):
                                pb_, bpb = C.ps[nb % 8], C.bps[nb % 8]
                                nb += 1
                                for ci in range(8):
                                    S.op("pe", lambda e: e.matmul(pb_[:], a_c[:, ci, tt * 128:(tt + 1) * 128], u_t[:, ci, :],
                                                                  start=(ci == 0), stop=(ci == 7)), r=[bac, bu], w=[bpb], signal=(ci == 7))
                                hs = hT[tt][0][:, ds * 512:(ds + 1) * 512]
                                S.op("dve", lambda e: e.tensor_tensor(out=hs, in0=pb_[:], in1=hs, op=ALU.add), r=[bpb, hT[tt][1]], w=[hT[tt][1]])
                    S.barrier()
            if "Hd" in T:
                for tt in range(4):
                    S.dma("sp", T["Hd"][tt * 128:(tt + 1) * 128, :], hT[tt][0][:], r=[hT[tt][1]], is_output=True)
            with contextlib.ExitStack() as b6:
                tl = lambda name, shape, dt: _tile(nc, b6, name, shape, dt)
                gfin, bgfin = tl("gfin", [128, D], F32)
                S.dma("sp", gfin[:], T["gfin_r"][:], w=[bgfin])
                junk, bjunk = tl("junkC", [128, D], BF16)
                ot = [tl(f"ot{i}", [128, D], F32) for i in range(2)]
                s8 = [tl(f"s8c{i}", [128, 8], F32) for i in range(2)]
                for tt in range(4):
                    (o_t, bo), (s_, bs_) = ot[tt % 2], s8[tt % 2]
                    h_t, bh = hT[tt]
                    S.op("dve", lambda e: e.memset(s_[:], 0.0), w=[bs_])
                    S.op("act", lambda e: e.activation(out=junk[:], in_=h_t[:], func=AF.Square, accum_out=s_[:, 0:1]), r=[bh], w=[bjunk, bs_])
                    rstd_ops(S, s_[:, 0:1], s_[:, 1:2], bs_, s_[:, 2:4], 1, 1.0 / D, [])
                    S.op("dve", lambda e: e.scalar_tensor_tensor(out=o_t[:], in0=h_t[:], scalar=s_[:, 1:2], in1=gfin[:],
                                                                 op0=ALU.mult, op1=ALU.mult), r=[bh, bs_, bgfin], w=[bo])
                    S.dma("sp", T["out"][t0 + tt * 128:t0 + (tt + 1) * 128, :], o_t[:], r=[bo], is_output=True)
                S.barrier()
        S.barrier()


def build(dbg=None):
    nc = bass.Bass("TRN2", target_bir_lowering=False)
    T = {}

    def din(name, shape, dt=F32):
        T[name] = nc.dram_tensor(name, list(shape), dt, kind="ExternalInput").ap()

    def dscr(name, shape, dt, out=False):
        T[name] = nc.dram_tensor(name, list(shape), dt, kind="ExternalOutput" if out else "Internal").ap()

    din("x_own", [NTOK, D])
    din("x_prev", [NTOK, D])
    din("gmix_r", [128, D])
    din("w_in", [D, 6144])
    din("ident", [128, 128])
    for n in ("a_re_q", "a_im_q", "ldt_q"):
        din(n, [128, 128])
    for n in ("a_re_b", "a_im_b", "ldt_b", "b_re_b", "b_im_b"):
        din(n, [128, 1024])
    din("cmix1", [128, 2048])
    din("cmix2", [128, 2048])
    din("d_col", [128, 16])
    din("sgn1", [128, 1])
    din("rowmask", [128, 8])
    din("colmask", [128, 8, 128])
    din("iota512", [128, 512])
    din("swapI", [128, 128])
    din("riota512", [128, 512])
    dscr("YG", [2048, NTOK], BF16, out=(dbg == "S5"))
    din("w_glu", [2048, 2048])
    if dbg == "B2":
        dscr("YTd", [4096, TS], BF16, out=True)
        dscr("RSd", [128, 8], F32, out=True)
    din("w_out", [D, D])
    din("wmT", [128, 8, 128])
    din("bs_rep", [128, 8, 128])
    din("gm_cols", [128, 4, 16])
    din("gffn_r", [128, D])
    din("gfin_r", [128, D])
    din("w_q", [D, 2048])
    din("k1T", [128, 128])
    din("k2T", [128, 128])
    din("downT", [D, 16384])
    din("up", [16384, D])
    din("iota128", [128, 128])
    dscr("GT", [16, 128, 128, 128], BF16)
    dscr("DnS", [64, 128, 32, 256], BF16)
    dscr("WinS", [24, 128, 32, 256], BF16)
    dscr("WoS", [8, 128, 32, 512], BF16)
    dscr("WqS", [8, 128, 32, 256], BF16)
    dscr("UpS", [128, 128, 8, 512], BF16)
    if dbg == "B6x":
        dscr("Qd", [2048, TS], BF16, out=True)
        dscr("R3d", [128, 3, 128], F32, out=True)
        dscr("S1d", [128, 8, 128], F32, out=True)
        dscr("V1d", [128, 8, 16], F32, out=True)
        dscr("SCd", [128, 8, 16], F32, out=True)
        dscr("CId", [128, 8, 16], F32, out=True)
        dscr("I1d", [128, 8, 16], F32, out=True)
        dscr("Hd", [TS, D], F32, out=True)
    dscr("ZT", [2048, 4096], BF16, out=(dbg == "A"))
    dscr("UT", [2048, NTOK], BF16, out=(dbg == "A"))
    dscr("VN", [NTOK, 2048], BF16, out=(dbg == "A"))
    dscr("out", [NTOK, D], F32, out=True)

    with contextlib.ExitStack() as st:
        C = Ctx()
        C.nc, C.T = nc, T
        C.S = S = Sched(nc, st)
        C.ps = [st.enter_context(nc.psum_tensor(f"ps{i}", [128, 512], F32)) for i in range(8)]
        C.bps = [Buf(f"ps{i}") for i in range(8)]
        C.bZT, C.bUT, C.bVN, C.bYG, C.bGT = Buf("ZT"), Buf("UT"), Buf("VN"), Buf("YG"), Buf("GT")
        C.identb, C.bidentb = _tile(nc, st, "identb", [128, 128], BF16)
        C.identf, C.bidentf = _tile(nc, st, "identf", [128, 128], F32)
        S.dma("pool", C.identb[:], T["ident"][:], w=[C.bidentb])
        S.dma("sp", C.identf[:], T["ident"][:], w=[C.bidentf])
        C.iota128, C.biota = _tile(nc, st, "iota128", [128, 128], F32)
        S.dma("sp", C.iota128[:], T["iota128"][:], w=[C.biota])
        C.bkT = Buf("kT")
        C.kT = [st.enter_context(nc.sbuf_tensor(f"sb_k{a}T", [128, 128], BF16)) for a in range(2)]
        S.dma("pool", C.kT[0][:], T["k1T"][:], w=[C.bkT])
        S.dma("pool", C.kT[1][:], T["k2T"][:], w=[C.bkT])
        C.ncg = 16
        C.win_cached, C.win_fresh = set(), set()
        if dbg == "A":
            phase_a(C, [0, 4])
        else:
            phase_a(C, list(range(8)))
        if dbg == "S5":
            phase_s5(C, [0, 5])
        elif dbg != "TA":
            phase_s5(C, list(range(16)))
        if dbg == "B2":
            phase_b(C, [0], stop_after="B2")
        elif dbg == "B6":
            phase_b(C, [0])
        elif dbg == "B7":
            phase_b(C, [0, 1])
        elif dbg in ("TA", "TS"):
            pass
        elif dbg == "TB2":
            phase_b(C, [0], stop_after="B2")
        elif dbg == "TB4":
            phase_b(C, [0], stop_after="B4")
        elif dbg is None:
            phase_b(C, list(range(4)))
        S.finish()
        print("instructions:", S.nins, "sems:", S.nsem)
    return nc


def host_layout(inputs):
    f = lambda k: np.ascontiguousarray(np.asarray(inputs[k], dtype=np.float32))
    x = f("x")
    shared = {}
    shared["gmix_r"] = np.ascontiguousarray(np.broadcast_to(f("norm_mix_g")[0], (128, D)))
    shared["w_in"] = f("w_in")[0]
    shared["ident"] = np.eye(128, dtype=np.float32)
    a_re, a_im, ldt = f("s5_a_re")[0], f("s5_a_im")[0], f("s5_log_dt")[0]
    b_re, b_im = f("s5_b_re")[0], f("s5_b_im")[0]
    c_re, c_im = f("s5_c_re")[0], f("s5_c_im")[0]
    cp = np.ascontiguousarray
    shared["a_re_q"] = cp(np.concatenate([a_re.T, a_re.T], axis=0))
    shared["a_im_q"] = cp(np.concatenate([a_im.T, a_im.T], axis=0))
    shared["ldt_q"] = cp(np.broadcast_to(ldt[None, :], (128, 128)))
    def blay(a_gp):
        t = a_gp.reshape(16, 8, 64)
        t = np.broadcast_to(t[:, :, None, :], (16, 8, 16, 64))
        return cp(t.transpose(1, 2, 0, 3).reshape(128, 1024))
    shared["a_re_b"] = blay(a_re)
    shared["a_im_b"] = blay(a_im)
    shared["ldt_b"] = blay(np.broadcast_to(ldt[:, None], (128, 64)))
    bl = lambda b: cp(b.reshape(16, 8, 64, 16).transpose(1, 3, 0, 2).reshape(128, 1024))
    shared["b_re_b"] = bl(b_re)
    shared["b_im_b"] = bl(b_im)
    cl = lambda c: c.transpose(2, 0, 1).reshape(64, 2048)
    shared["cmix1"] = cp(np.concatenate([cl(c_re), cl(c_im)], axis=0))
    shared["cmix2"] = cp(np.concatenate([cl(c_im), cl(c_re)], axis=0))
    shared["d_col"] = cp(f("s5_d")[0].reshape(16, 128).T)
    shared["sgn1"] = np.concatenate([np.ones((64, 1), np.float32), -np.ones((64, 1), np.float32)], axis=0)
    rm = np.zeros((128, 8), np.float32)
    cmk = np.zeros((128, 8, 128), np.float32)
    for g8 in range(8):
        rm[g8 * 16:(g8 + 1) * 16, g8] = 1.0
        cmk[:, g8, g8 * 16:(g8 + 1) * 16] = 1.0
    shared["rowmask"] = rm
    shared["colmask"] = cmk
    shared["w_glu"] = f("s5_w_glu")[0]
    shared["w_out"] = f("w_out")[0]
    shared["wmT"] = cp(f("gm_w_s")[0].transpose(2, 0, 1))
    shared["bs_rep"] = cp(np.broadcast_to(f("gm_b_s")[0][None], (128, 8, 128)))
    colv = lambda v: v.reshape(16, 128).T
    shared["gm_cols"] = cp(np.stack([colv(f("gm_ln_g")[0]), colv(f("gm_ln_b")[0]), colv(f("norm_s5_out_g")[0]),
                                     colv(f("norm_gm_out_g")[0])], axis=1))
    shared["gffn_r"] = cp(np.broadcast_to(f("norm_ffn_g")[0], (128, D)))
    shared["gfin_r"] = cp(np.broadcast_to(f("norm_final_g"), (128, D)))
    shared["w_q"] = f("peer_w_q")[0]
    shared["k1T"] = cp(f("peer_keys_1")[0].T)
    shared["k2T"] = cp(f("peer_keys_2")[0].T)
    shared["downT"] = cp(f("peer_down")[0].T)
    shared["up"] = f("peer_up")[0]
    shared["iota128"] = cp(np.broadcast_to(np.arange(128, dtype=np.float32)[None, :], (128, 128)))
    shared["swapI"] = cp(np.roll(np.eye(128, dtype=np.float32), 64, axis=1))
    shared["riota512"] = cp(np.broadcast_to(np.arange(511, -1, -1, dtype=np.float32)[None, :], (128, 512)))
    shared["iota512"] = cp(np.broadcast_to(np.arange(512, dtype=np.float32)[None, :], (128, 512)))
    maps = []
    for c in range(8):
        b, s = c // 2, c % 2
        m = dict(shared)
        m["x_own"] = np.ascontiguousarray(x[b, s * NTOK:(s + 1) * NTOK])
        m["x_prev"] = np.ascontiguousarray(x[b, 0:NTOK]) if s == 1 else np.zeros((NTOK, D), np.float32)
        maps.append(m)
    return maps


def kernel(**inputs):
    maps = host_layout(inputs)
    nc = build()
    res = run_bass_kernel_spmd(nc, maps, core_ids=list(range(8)))
    out = np.zeros((4, 4096, D), np.float32)
    for c in range(8):
        b, s = c // 2, c % 2
        out[b, s * NTOK:(s + 1) * NTOK] = res.results[c]["out"]
    return out
```

```python
import contextlib
import math

import ml_dtypes
import numpy as np

import concourse.bass as bass
import concourse.mybir as mybir
from concourse.bass_utils import run_bass_kernel_spmd

F32 = mybir.dt.float32
BF16 = mybir.dt.bfloat16
U32 = mybir.dt.uint32
ALU = mybir.AluOpType
AF = mybir.ActivationFunctionType
AX = mybir.AxisListType

D = 4096
NTOK = 2048
TS = 512
NST = NTOK // TS
EPS = 1e-6
TWO_PI = 2.0 * math.pi
GELU = AF.Gelu_apprx_tanh
CONV_INTERLEAVE = True


class Buf:
    __slots__ = ("name", "w", "r")

    def __init__(self, name=""):
        self.name = name
        self.w = None
        self.r = []


class Sched:
    EPOCH = 3500

    def __init__(self, nc, stack):
        self.nc = nc
        self.stack = stack
        self.eng = {"pe": nc.tensor, "dve": nc.vector, "act": nc.scalar, "pool": nc.gpsimd, "sp": nc.sync}
        self.sem = {}
        self.cnt = {}
        self.pending = {}
        self.seen = {k: {} for k in self.eng}
        self.nsem = 0
        self.last_old = {}
        for k in self.eng:
            self._new_epoch(k)
        self.dpool = {}
        for k, n in (("sp", 24), ("pool", 24), ("act", 8)):
            self.dpool[k] = [[self._mksem(f"d{k}{i}"), 0] for i in range(n)]
        self.dnext = {k: 0 for k in self.dpool}
        self.out_events = []
        self.nins = 0

    def _mksem(self, name):
        self.nsem += 1
        return self.stack.enter_context(self.nc.semaphore(f"{name}_{self.nsem}"))

    def _new_epoch(self, k):
        if k in self.sem and self.cnt[k] > 0:
            self.last_old[k] = (self.sem[k], self.cnt[k])
        self.sem[k] = self._mksem(f"e{k}")
        self.cnt[k] = 0
        self.pending[k] = False

    def _wait(self, k, evs):
        need = {}
        for (s, v) in evs:
            if self.seen[k].get(id(s), (None, 0))[1] >= v:
                continue
            if id(s) not in need or need[id(s)][1] < v:
                need[id(s)] = (s, v)
        for s, v in need.values():
            self.eng[k].wait_ge(s, v)
            self.seen[k][id(s)] = (s, v)

    def _deps(self, k, r, w):
        evs = []
        for b in r:
            if b.w is not None:
                evs.append(b.w)
        for b in w:
            if b.w is not None:
                evs.append(b.w)
            evs.extend(b.r)
        if k == "pe":
            evs = [e for e in evs if e[0] is not self.sem["pe"]]
        return evs

    def _mark(self, ev, r, w):
        for b in r:
            b.r.append(ev)
            if len(b.r) > 48:
                best = {}
                for (s, v) in b.r:
                    if id(s) not in best or best[id(s)][1] < v:
                        best[id(s)] = (s, v)
                b.r = list(best.values())
        for b in w:
            b.w = ev
            b.r = []

    def op(self, k, fn, r=(), w=(), signal=True):
        self._wait(k, self._deps(k, r, w))
        ins = fn(self.eng[k])
        self.nins += 1
        if signal:
            ins.then_inc(self.sem[k], 1)
            self.cnt[k] += 1
            self.pending[k] = False
            ev = (self.sem[k], self.cnt[k])
        else:
            self.pending[k] = True
            ev = (self.sem[k], self.cnt[k] + 1)
        self._mark(ev, r, w)
        if signal and self.cnt[k] >= self.EPOCH:
            self._new_epoch(k)
        return ev

    def dma(self, k, out, in_, r=(), w=(), is_output=False, **kw):
        pool = self.dpool[k]
        i = self.dnext[k]
        self.dnext[k] = (i + 1) % len(pool)
        slot = pool[i]
        evs = self._deps(k, r, w)
        if slot[1] > 0:
            evs.append((slot[0], slot[1]))
        self._wait(k, evs)
        ins = self.eng[k].dma_start(out=out, in_=in_, **kw)
        self.nins += 1
        slot[1] += 16
        ins.then_inc(slot[0], 16)
        ev = (slot[0], slot[1])
        self._mark(ev, r, w)
        if is_output:
            self.out_events.append(ev)
        return ev

    def barrier(self):
        evs = []
        for k in self.eng:
            assert not self.pending[k], k
            if self.cnt[k] > 0:
                evs.append((self.sem[k], self.cnt[k]))
            elif k in self.last_old:
                evs.append(self.last_old[k])
        for k in self.dpool:
            for s, v in self.dpool[k]:
                if v > 0:
                    evs.append((s, v))
        for k in self.eng:
            self._wait(k, evs)

    def barrier_dma(self):
        evs = []
        for k in self.dpool:
            for s_, v in self.dpool[k]:
                if v > 0:
                    evs.append((s_, v))
        for k in ("pool", "sp"):
            self._wait(k, evs)

    def finish(self):
        self._wait("sp", self.out_events)
        self.barrier()


class Ctx:
    pass


_UID = [0]


def _uname(name):
    _UID[0] += 1
    return f"{name}_{_UID[0]}"


def _tile(nc, stack, name, shape, dt):
    return stack.enter_context(nc.sbuf_tensor(_uname("sb_" + name), list(shape), dt)), Buf(name)


def phase_a(C, st_list):
    nc, S, T = C.nc, C.S, C.T
    with contextlib.ExitStack() as ph:
        tl = lambda name, shape, dt: _tile(nc, ph, name, shape, dt)
        gmix, bgmix = tl("gmix", [128, D], F32)
        S.dma("sp", gmix[:], T["gmix_r"][:], w=[bgmix])
        xt = [tl(f"xt{i}", [128, D], F32) for i in range(2)]
        abf = [tl(f"abf{i}", [128, D], BF16) for i in range(4)]
        aT, baT = tl("aT", [128, 32, TS], BF16)
        wb = [tl(f"wb{i}", [128, 32, 256], BF16) for i in range(2)]
        zs = [tl(f"zs{i}", [128, TS], BF16) for i in range(3)]
        V = [tl(f"V{i}", [128, 2048], F32) for i in range(4)]
        vn = [tl(f"vn{i}", [128, 2048], BF16) for i in range(2)]
        junk, bjunk = tl("junkA", [128, 2048], BF16)
        st8 = [tl(f"st8{i}", [128, 8], F32) for i in range(2)]
        nx = 0
        nw = 0
        nz = 0
        nb = 0
        def norm_part(st):
            own = st >= NST
            xsrc = T["x_own"] if own else T["x_prev"]
            t0 = (st - NST if own else st) * TS
            for tt in range(4):
                (x_t, bx), (a_t, ba), (s8, bs8) = xt[tt % 2], abf[tt], st8[tt % 2]
                S.dma("sp", x_t[:], xsrc[t0 + tt * 128:t0 + (tt + 1) * 128, :], w=[bx])
                S.op("dve", lambda e: e.memset(s8[:], 0.0), w=[bs8])
                S.op("act", lambda e: e.activation(out=a_t[:], in_=x_t[:], func=AF.Square, accum_out=s8[:, 0:1]),
                     r=[bx], w=[ba, bs8])
                S.op("dve", lambda e: e.tensor_scalar(out=s8[:, 1:2], in0=s8[:, 0:1], scalar1=1.0 / D, scalar2=EPS,
                                                      op0=ALU.mult, op1=ALU.add), r=[bs8], w=[bs8])
                S.op("act", lambda e: e.activation(out=s8[:, 3:4], in_=s8[:, 1:2], func=AF.Sqrt), r=[bs8], w=[bs8])
                S.op("dve", lambda e: e.reciprocal(out=s8[:, 2:3], in_=s8[:, 3:4]), r=[bs8], w=[bs8])
                S.op("dve", lambda e: e.scalar_tensor_tensor(out=a_t[:], in0=x_t[:], scalar=s8[:, 2:3], in1=gmix[:],
                                                             op0=ALU.mult, op1=ALU.mult), r=[bx, bs8, bgmix], w=[ba])

        def transpose_part():
            nonlocal_nb = cntA["nb"]
            for tt in range(4):
                (a_t, ba) = abf[tt]
                for dcg in range(4):
                    pb, bpb = C.ps[nonlocal_nb % 8], C.bps[nonlocal_nb % 8]
                    nonlocal_nb += 1
                    pbb = pb[:].bitcast(BF16)
                    for j in range(8):
                        dc = dcg * 8 + j
                        S.op("pe", lambda e: e.transpose(out=pbb[:, j * 128:(j + 1) * 128],
                                                         in_=a_t[:, dc * 128:(dc + 1) * 128], identity=C.identb[:]),
                             r=[ba, C.bidentb], w=[bpb], signal=(j == 7))
                    src = pbb[:, 0:1024].rearrange("p (j t) -> p j t", j=8)
                    dst = aT[:, dcg * 8:(dcg + 1) * 8, tt * 128:(tt + 1) * 128]
                    if dcg % 2 == 0:
                        S.op("act", lambda e: e.activation(out=dst, in_=src, func=AF.Copy), r=[bpb], w=[baT])
                    else:
                        S.op("dve", lambda e: e.tensor_copy(out=dst, in_=src), r=[bpb], w=[baT])
            cntA["nb"] = nonlocal_nb

        cntA = {"nb": 0}
        norm_part(st_list[0])
        for si, st in enumerate(st_list):
            own = st >= NST
            t0 = (st - NST if own else st) * TS
            transpose_part()
            if si + 1 < len(st_list):
                norm_part(st_list[si + 1])
            nb = cntA["nb"]
            ncol = 24 if own else 8
            for ct in range(ncol):
                (w_t, bw) = wb[nw % 2]
                nw += 1
                if ct not in C.win_cached:
                    S.dma("pool", w_t[:], T["w_in"][ct], w=[bw])
                    S.dma("sp", T["WinS"][ct], w_t[:], r=[bw])
                    C.win_cached.add(ct)
                    C.win_fresh.add(ct)
                else:
                    if ct in C.win_fresh:
                        S.barrier_dma()
                        C.win_fresh.clear()
                    S.dma("pool", w_t[:], T["WinS"][ct], w=[bw])
                if ct < 16:
                    for sub in range(2):
                        pb, bpb = C.ps[nb % 8], C.bps[nb % 8]
                        nb += 1
                        for dc in range(32):
                            S.op("pe", lambda e: e.matmul(pb[:], w_t[:, dc, sub * 128:(sub + 1) * 128], aT[:, dc, :],
                                                          start=(dc == 0), stop=(dc == 31)),
                                 r=[bw, baT], w=[bpb], signal=(dc == 31))
                        (z_t, bz) = zs[nz % 3]
                        nz += 1
                        if ct < 8:
                            S.op("act", lambda e: e.activation(out=z_t[:], in_=pb[:], func=AF.Copy), r=[bpb], w=[bz])
                            row = (ct * 2 + sub) * 128
                            S.dma("sp", T["ZT"][row:row + 128, st * TS:(st + 1) * TS], z_t[:], r=[bz])
                        else:
                            S.op("act", lambda e: e.activation(out=z_t[:], in_=pb[:], func=GELU), r=[bpb], w=[bz])
                            row = ((ct - 8) * 2 + sub) * 128
                            S.dma("sp", T["UT"][row:row + 128, t0:t0 + TS], z_t[:], r=[bz])
                else:
                    vc = ct - 16
                    for tt in range(4):
                        pb, bpb = C.ps[nb % 8], C.bps[nb % 8]
                        nb += 1
                        for dc in range(32):
                            S.op("pe", lambda e: e.matmul(pb[:, 0:256], aT[:, dc, tt * 128:(tt + 1) * 128], w_t[:, dc, :],
                                                          start=(dc == 0), stop=(dc == 31)),
                                 r=[bw, baT], w=[bpb], signal=(dc == 31))
                        S.op("act", lambda e: e.activation(out=V[tt][0][:, vc * 256:(vc + 1) * 256], in_=pb[:, 0:256],
                                                           func=GELU), r=[bpb], w=[V[tt][1]])
            cntA["nb"] = nb
            if own:
                for tt in range(4):
                    v_t, bv = V[tt]
                    (s8, bs8) = st8[nx % 2]
                    (vn_t, bvn) = vn[nx % 2]
                    nx += 1
                    S.op("dve", lambda e: e.memset(s8[:], 0.0), w=[bs8])
                    S.op("act", lambda e: e.activation(out=vn_t[:], in_=v_t[:], func=AF.Square,
                                                       accum_out=s8[:, 0:1]), r=[bv], w=[bvn, bs8])
                    S.op("dve", lambda e: e.tensor_scalar(out=junk[:, 0:2048], in0=v_t[:], scalar1=1.0, scalar2=0.0,
                                                          op0=ALU.mult, op1=ALU.add, accum_out=s8[:, 1:2]),
                         r=[bv], w=[bjunk, bs8])
                    S.op("dve", lambda e: e.tensor_scalar(out=s8[:, 2:3], in0=s8[:, 1:2], scalar1=1.0 / 2048, scalar2=None,
                                                          op0=ALU.mult), r=[bs8], w=[bs8])
                    S.op("dve", lambda e: e.tensor_tensor(out=s8[:, 3:4], in0=s8[:, 2:3], in1=s8[:, 2:3], op=ALU.mult),
                         r=[bs8], w=[bs8])
                    S.op("dve", lambda e: e.scalar_tensor_tensor(out=s8[:, 4:5], in0=s8[:, 0:1], scalar=1.0 / 2048,
                                                                 in1=s8[:, 3:4], op0=ALU.mult, op1=ALU.subtract),
                         r=[bs8], w=[bs8])
                    S.op("dve", lambda e: e.tensor_scalar(out=s8[:, 6:7], in0=s8[:, 4:5], scalar1=EPS, scalar2=None,
                                                          op0=ALU.add), r=[bs8], w=[bs8])
                    S.op("act", lambda e: e.activation(out=s8[:, 7:8], in_=s8[:, 6:7], func=AF.Sqrt), r=[bs8], w=[bs8])
                    S.op("dve", lambda e: e.reciprocal(out=s8[:, 5:6], in_=s8[:, 7:8]), r=[bs8], w=[bs8])
                    S.op("dve", lambda e: e.tensor_scalar(out=vn_t[:], in0=v_t[:], scalar1=s8[:, 2:3], scalar2=s8[:, 5:6],
                                                          op0=ALU.subtract, op1=ALU.mult), r=[bv, bs8], w=[bvn])
                    S.dma("sp", T["VN"][t0 + tt * 128:t0 + (tt + 1) * 128, :], vn_t[:], r=[bvn])
        S.barrier()


def phase_s5(C, gb_list):
    nc, S, T = C.nc, C.S, C.T
    PI = math.pi
    with contextlib.ExitStack() as ph:
        tl = lambda name, shape, dt: _tile(nc, ph, name, shape, dt)
        bset = Buf("s5setup")
        raw = lambda name, shape, dt: ph.enter_context(nc.sbuf_tensor(_uname("sr_" + name), list(shape), dt))
        so = lambda k, fn: S.op(k, fn, r=[bset], w=[bset])
        aq, iq, lq = raw("aq", [128, 128], F32), raw("iq", [128, 128], F32), raw("lq", [128, 128], F32)
        theta, mag, tq = raw("theta", [128, 128], F32), raw("mag", [128, 128], F32), raw("tq", [128, 128], F32)
        offs = raw("offs", [128, 8, 128], F32)
        S.dma("sp", aq[:], T["a_re_q"][:], w=[bset])
        S.dma("sp", iq[:], T["a_im_q"][:], w=[bset])
        S.dma("sp", lq[:], T["ldt_q"][:], w=[bset])
        so("act", lambda e: e.activation(out=lq[:], in_=lq[:], func=AF.Exp))
        so("dve", lambda e: e.tensor_tensor(out=tq[:], in0=aq[:], in1=lq[:], op=ALU.mult))
        so("act", lambda e: e.activation(out=mag[:], in_=tq[:], func=AF.Exp))
        so("dve", lambda e: e.tensor_tensor(out=theta[:], in0=iq[:], in1=lq[:], op=ALU.mult))
        kq = raw("kq", [128, 128], mybir.dt.int32)
        so("dve", lambda e: e.tensor_scalar(out=tq[:], in0=theta[:], scalar1=1.0 / TWO_PI, scalar2=None, op0=ALU.mult))
        so("dve", lambda e: e.tensor_copy(out=kq[:], in_=tq[:]))
        so("dve", lambda e: e.tensor_tensor(out=theta[:], in0=tq[:], in1=kq[:], op=ALU.subtract))
        for seg in range(8):
            so("dve", lambda e: e.tensor_scalar(out=tq[:], in0=theta[:], scalar1=float(512 * seg), scalar2=None, op0=ALU.mult))
            so("dve", lambda e: e.tensor_copy(out=kq[:], in_=tq[:]))
            so("dve", lambda e: e.tensor_tensor(out=offs[:, seg, :], in0=tq[:], in1=kq[:], op=ALU.subtract))
        hp = raw("hp", [128, 1], F32)
        BST1, BST2 = raw("BST1", [128, 16, 128], BF16), raw("BST2", [128, 16, 128], BF16)
        sg = raw("sg", [128, 1], F32)
        CST1, CST2 = raw("CST1", [128, 16, 128], BF16), raw("CST2", [128, 16, 128], BF16)
        dcol, DST = raw("dcol", [128, 16], F32), raw("DST", [128, 16, 128], BF16)
        rmask, cmask = raw("rmask", [128, 8], F32), raw("cmask", [128, 8, 128], BF16)
        iota = raw("iota512", [128, 512], F32)
        riota = raw("riota512", [128, 512], F32)
        c512, s512 = raw("c512", [128, 128], F32), raw("s512", [128, 128], F32)
        swapI = raw("swapI", [128, 128], F32)
        eq_, mag512 = raw("eq", [128, 128], F32), raw("mag512", [128, 128], F32)
        tmpst = contextlib.ExitStack()
        rawt = lambda name, shape, dt: tmpst.enter_context(nc.sbuf_tensor(_uname("st_" + name), list(shape), dt))
        names = ["arb", "aib", "ldb", "brb", "bib", "magb", "ang", "nsin", "ncos", "nr", "ni", "u1", "u2", "cr", "ci"]
        B = {n: rawt("s5" + n, [128, 1024], F32) for n in names}
        for n, src in (("arb", "a_re_b"), ("aib", "a_im_b"), ("ldb", "ldt_b"), ("brb", "b_re_b"), ("bib", "b_im_b")):
            S.dma("sp", B[n][:], T[src][:], w=[bset])
        tt_ = lambda o, a, b, op: so("dve", lambda e: e.tensor_tensor(out=B[o][:], in0=B[a][:], in1=B[b][:], op=op))
        so("act", lambda e: e.activation(out=B["ldb"][:], in_=B["ldb"][:], func=AF.Exp))
        tt_("u1", "arb", "ldb", ALU.mult)
        so("act", lambda e: e.activation(out=B["magb"][:], in_=B["u1"][:], func=AF.Exp))
        tt_("ang", "aib", "ldb", ALU.mult)
        kb = rawt("kb", [128, 1024], mybir.dt.int32)
        so("dve", lambda e: e.memset(hp[:], PI / 2))
        so("dve", lambda e: e.tensor_scalar(out=B["u1"][:], in0=B["ang"][:], scalar1=1.0 / TWO_PI, scalar2=None, op0=ALU.mult))
        so("dve", lambda e: e.tensor_copy(out=kb[:], in_=B["u1"][:]))
        so("dve", lambda e: e.tensor_tensor(out=B["u1"][:], in0=B["u1"][:], in1=kb[:], op=ALU.subtract))
        so("dve", lambda e: e.scalar_tensor_tensor(out=B["u2"][:], in0=B["u1"][:], scalar=-1.0, in1=B["u1"][:],
                                                   op0=ALU.mult, op1=ALU.max))
        so("act", lambda e: e.activation(out=B["nsin"][:], in_=B["u1"][:], func=AF.Sin, scale=TWO_PI))
        so("act", lambda e: e.activation(out=B["ncos"][:], in_=B["u2"][:], func=AF.Sin, scale=-TWO_PI, bias=hp[:, 0:1]))
        tt_("u1", "magb", "ncos", ALU.mult)
        so("dve", lambda e: e.tensor_scalar(out=B["nr"][:], in0=B["u1"][:], scalar1=-1.0, scalar2=None, op0=ALU.add))
        tt_("ni", "magb", "nsin", ALU.mult)
        tt_("u1", "arb", "arb", ALU.mult)
        tt_("u2", "aib", "aib", ALU.mult)
        tt_("u1", "u1", "u2", ALU.add)
        so("dve", lambda e: e.reciprocal(out=B["u2"][:], in_=B["u1"][:]))
        tt_("cr", "nr", "arb", ALU.mult)
        tt_("u1", "ni", "aib", ALU.mult)
        tt_("cr", "cr", "u1", ALU.add)
        tt_("cr", "cr", "u2", ALU.mult)
        tt_("ci", "ni", "arb", ALU.mult)
        tt_("u1", "nr", "aib", ALU.mult)
        tt_("ci", "ci", "u1", ALU.subtract)
        tt_("ci", "ci", "u2", ALU.mult)
        tt_("nr", "cr", "brb", ALU.mult)
        tt_("u1", "ci", "bib", ALU.mult)
        tt_("nr", "nr", "u1", ALU.subtract)
        tt_("ni", "cr", "bib", ALU.mult)
        tt_("u1", "ci", "brb", ALU.mult)
        tt_("ni", "ni", "u1", ALU.add)
        v3 = lambda n: B[n][:].rearrange("p (g q) -> p g q", g=16)
        so("dve", lambda e: e.tensor_copy(out=BST1[:, :, 0:64], in_=v3("nr")))
        so("dve", lambda e: e.tensor_copy(out=BST1[:, :, 64:128], in_=v3("ni")))
        so("dve", lambda e: e.tensor_copy(out=BST2[:, :, 0:64], in_=v3("ni")))
        so("dve", lambda e: e.tensor_scalar(out=BST2[:, :, 64:128], in0=v3("nr"), scalar1=-1.0, scalar2=None, op0=ALU.mult))
        cm1 = B["arb"]
        cmA = rawt("cmA", [128, 2048], F32)
        S.dma("sp", cmA[:], T["cmix1"][:], w=[bset])
        S.dma("sp", sg[:], T["sgn1"][:], w=[bset])
        cmB = rawt("cmB", [128, 2048], F32)
        S.dma("sp", cmB[:], T["cmix2"][:], w=[bset])
        so("dve", lambda e: e.tensor_scalar(out=CST2[:].rearrange("p g q -> p (g q)"), in0=cmB[:], scalar1=-1.0,
                                            scalar2=None, op0=ALU.mult))
        so("dve", lambda e: e.tensor_scalar(out=CST1[:].rearrange("p g q -> p (g q)"), in0=cmA[:], scalar1=sg[:, 0:1],
                                            scalar2=None, op0=ALU.mult))
        S.dma("sp", dcol[:], T["d_col"][:], w=[bset])
        for gb in range(16):
            so("dve", lambda e: e.tensor_scalar(out=DST[:, gb, :], in0=C.identf[:], scalar1=dcol[:, gb:gb + 1],
                                                scalar2=None, op0=ALU.mult))
        S.dma("sp", rmask[:], T["rowmask"][:], w=[bset])
        S.dma("pool", cmask[:], T["colmask"][:], w=[bset])
        S.dma("sp", iota[:], T["iota512"][:], w=[bset])

        S.dma("sp", swapI[:], T["swapI"][:], w=[bset])
        S.dma("sp", riota[:], T["riota512"][:], w=[bset])
        so("dve", lambda e: e.tensor_tensor(out=eq_[:], in0=aq[:], in1=lq[:], op=ALU.mult))
        so("act", lambda e: e.activation(out=mag512[:], in_=eq_[:], func=AF.Exp, scale=512.0))
        so("dve", lambda e: e.tensor_scalar(out=tq[:], in0=theta[:], scalar1=512.0, scalar2=None, op0=ALU.mult))
        so("dve", lambda e: e.tensor_copy(out=kq[:], in_=tq[:]))
        so("dve", lambda e: e.tensor_tensor(out=tq[:], in0=tq[:], in1=kq[:], op=ALU.subtract))
        so("dve", lambda e: e.scalar_tensor_tensor(out=c512[:], in0=tq[:], scalar=-1.0, in1=tq[:], op0=ALU.mult, op1=ALU.max))
        so("act", lambda e: e.activation(out=s512[:], in_=tq[:], func=AF.Sin, scale=TWO_PI))
        so("act", lambda e: e.activation(out=c512[:], in_=c512[:], func=AF.Sin, scale=-TWO_PI, bias=hp[:, 0:1]))
        so("dve", lambda e: e.tensor_scalar(out=s512[:], in0=s512[:], scalar1=sg[:, 0:1], scalar2=None, op0=ALU.mult))

        S.barrier()
        tmpst.close()
        NSET = 4
        zt = [tl(f"zt{i}", [128, 4096], BF16) for i in range(2)]
        bm = [[tl(f"bm{k}{i}", [128, 128], BF16) for i in range(4)] for k in range(NSET)]
        ROT = [tl(f"ROT{k}", [128, 128], F32) for k in range(NSET)]
        rtm = [tl(f"rtm{k}", [128, 128], F32) for k in range(2)]
        SNt = [tl(f"SN{k}", [128, 512], F32) for k in range(NSET)]
        CSt = [tl(f"CS{k}", [128, 512], F32) for k in range(NSET)]
        kph = [tl(f"kph{k}", [128, 512], mybir.dt.int32) for k in range(2)]
        t1 = [tl(f"t1{k}", [128, 512], F32) for k in range(4)]
        t2 = [tl(f"t2{k}", [128, 512], F32) for k in range(4)]
        vv = [tl(f"vv{k}", [128, 512], F32) for k in range(4)]
        q1 = [tl(f"q1{k}", [128, 512], BF16) for k in range(4)]
        q2 = [tl(f"q2{k}", [128, 512], BF16) for k in range(4)]
        ini = [tl(f"ini{k}", [128, 1], F32) for k in range(4)]
        accs = [tl(f"accs{k}", [128, 4], F32) for k in range(4)]
        CSb = [tl(f"CSb{k}", [128, 512], BF16) for k in range(NSET)]
        SNb = [tl(f"SNb{k}", [128, 512], BF16) for k in range(NSET)]
        vb = [tl(f"vb{k}", [128, 512], BF16) for k in range(4)]
        junkS = [tl(f"junkS{k}", [128, 512], F32) for k in range(2)]
        DEC = [tl(f"DEC{k}", [128, 512], F32) for k in range(2)]
        MCS = [tl(f"MCS{k}", [128, 512], F32) for k in range(NSET)]
        MSN = [tl(f"MSN{k}", [128, 512], F32) for k in range(NSET)]
        conv_jobs = []
        for ds in range(8):
            conv_jobs.append((T["WoS"][ds], T["w_out"][ds]))
        for qc in range(8):
            conv_jobs.append((T["WqS"][qc], T["w_q"][qc]))
        for cg in range(16):
            for cp in range(4):
                i_ = cg * 4 + cp
                conv_jobs.append((T["DnS"][i_], T["downT"][i_]))
            for ds in range(8):
                conv_jobs.append((T["UpS"][cg * 8 + ds], T["up"][cg * 8 + ds]))
        ysb = [tl(f"ysb{k}", [128, 512], BF16) for k in range(2)]
        P1, bP1, P2, bP2, RB, bRB = C.ps[0], C.bps[0], C.ps[1], C.bps[1], C.ps[2], C.bps[2]
        cnt = {"prep": 0, "it": 0, "y": 0}

        def prep_group(g):
            k = g % NSET
            gb, g8 = divmod(g, 8)
            S.op("act", lambda e: e.activation(out=bm[k][0][0][:], in_=BST1[:, gb, :], func=AF.Copy, scale=rmask[:, g8:g8 + 1]),
                 r=[bset], w=[bm[k][0][1]])
            S.op("act", lambda e: e.activation(out=bm[k][1][0][:], in_=BST2[:, gb, :], func=AF.Copy, scale=rmask[:, g8:g8 + 1]),
                 r=[bset], w=[bm[k][1][1]])
            S.op("dve", lambda e: e.tensor_tensor(out=bm[k][2][0][:], in0=CST1[:, gb, :], in1=cmask[:, g8, :],
                                                  op=ALU.mult), r=[bset], w=[bm[k][2][1]])
            S.op("dve", lambda e: e.tensor_tensor(out=bm[k][3][0][:], in0=CST2[:, gb, :], in1=cmask[:, g8, :],
                                                  op=ALU.mult), r=[bset], w=[bm[k][3][1]])
            (rt, brt), (rm_, brm) = ROT[k], rtm[cnt["prep"] % 2]
            S.op("act", lambda e: e.activation(out=rt[:], in_=C.identf[:], func=AF.Copy, scale=c512[:, g:g + 1]),
                 r=[bset, C.bidentf], w=[brt])
            S.op("act", lambda e: e.activation(out=rm_[:], in_=swapI[:], func=AF.Copy, scale=s512[:, g:g + 1]), r=[bset], w=[brm])
            S.op("dve", lambda e: e.tensor_tensor(out=rt[:], in0=rt[:], in1=rm_[:], op=ALU.add), r=[brt, brm], w=[brt])
            (sn, bsn), (cs, bcs), (ki_, bki) = SNt[k], CSt[k], kph[cnt["prep"] % 2]
            cnt["prep"] += 1
            S.op("act", lambda e: e.activation(out=sn[:], in_=iota[:], func=AF.Copy, scale=theta[:, g:g + 1]), r=[bset], w=[bsn])
            S.op("dve", lambda e: e.tensor_copy(out=ki_[:], in_=sn[:]), r=[bsn], w=[bki])
            S.op("dve", lambda e: e.tensor_tensor(out=sn[:], in0=sn[:], in1=ki_[:], op=ALU.subtract), r=[bki, bsn], w=[bsn])
            S.op("dve", lambda e: e.scalar_tensor_tensor(out=cs[:], in0=sn[:], scalar=-1.0, in1=sn[:], op0=ALU.mult, op1=ALU.max),
                 r=[bsn], w=[bcs])
            S.op("act", lambda e: e.activation(out=sn[:], in_=sn[:], func=AF.Sin, scale=TWO_PI), r=[bsn], w=[bsn])
            S.op("act", lambda e: e.activation(out=cs[:], in_=cs[:], func=AF.Sin, scale=-TWO_PI, bias=hp[:, 0:1]), r=[bcs, bset], w=[bcs])
            S.op("act", lambda e: e.activation(out=CSb[k][0][:], in_=cs[:], func=AF.Copy), r=[bcs], w=[CSb[k][1]])
            S.op("act", lambda e: e.activation(out=SNb[k][0][:], in_=sn[:], func=AF.Copy), r=[bsn], w=[SNb[k][1]])
            (dc_, bdc) = DEC[cnt["prep"] % 2]
            S.op("act", lambda e: e.activation(out=dc_[:], in_=riota[:], func=AF.Exp, scale=eq_[:, g:g + 1]), r=[bset], w=[bdc])
            S.op("dve", lambda e: e.tensor_tensor(out=MCS[k][0][:], in0=dc_[:], in1=cs[:], op=ALU.mult), r=[bdc, bcs], w=[MCS[k][1]])
            S.op("dve", lambda e: e.tensor_tensor(out=MSN[k][0][:], in0=dc_[:], in1=sn[:], op=ALU.mult), r=[bdc, bsn], w=[MSN[k][1]])

        def stage_a(itn, g, seg, z_t, bz):
            k, ix = g % NSET, itn % 4
            zseg = z_t[:, seg * 512:(seg + 1) * 512]
            S.op("pe", lambda e: e.matmul(P1[:], bm[k][0][0][:], zseg, start=True, stop=True), r=[bm[k][0][1], bz], w=[bP1])
            S.op("pe", lambda e: e.matmul(P2[:], bm[k][1][0][:], zseg, start=True, stop=True), r=[bm[k][1][1], bz], w=[bP2])
            if seg < 4:
                (ac, bac), (jk, bjk) = accs[ix], junkS[itn % 2]
                S.op("dve", lambda e: e.memset(ac[:, 0:2], 0.0), w=[bac])
                S.op("dve", lambda e: e.scalar_tensor_tensor(out=jk[:], in0=P1[:], scalar=1.0, in1=MCS[k][0][:], op0=ALU.mult, op1=ALU.mult,
                                                             accum_out=ac[:, 0:1]), r=[bP1, MCS[k][1]], w=[bjk, bac])
                S.op("dve", lambda e: e.scalar_tensor_tensor(out=jk[:], in0=P2[:], scalar=1.0, in1=MSN[k][0][:], op0=ALU.mult, op1=ALU.mult,
                                                             accum_out=ac[:, 1:2]), r=[bP2, MSN[k][1]], w=[bjk, bac])
                return
            (a1, ba1), (a2, ba2) = t1[ix], t2[ix]
            S.op("dve", lambda e: e.tensor_tensor(out=a1[:], in0=P1[:], in1=CSt[k][0][:], op=ALU.mult), r=[bP1, CSt[k][1]], w=[ba1])
            S.op("dve", lambda e: e.tensor_tensor(out=a2[:], in0=P2[:], in1=SNt[k][0][:], op=ALU.mult), r=[bP2, SNt[k][1]], w=[ba2])
            S.op("dve", lambda e: e.tensor_tensor(out=a1[:], in0=a1[:], in1=a2[:], op=ALU.add), r=[ba1, ba2], w=[ba1])

        def stage_b(itn, g, seg, g8):
            k, ix = g % NSET, itn % 4
            if seg < 4:
                (ac, bac) = accs[ix]
                S.op("dve", lambda e: e.tensor_tensor(out=ac[:, 2:3], in0=ac[:, 0:1], in1=ac[:, 1:2], op=ALU.add), r=[bac], w=[bac])
                if seg > 0:
                    S.op("dve", lambda e: e.scalar_tensor_tensor(out=ac[:, 3:4], in0=ini[ix][0][:, 0:1], scalar=mag512[:, g:g + 1],
                                                                 in1=ac[:, 2:3], op0=ALU.mult, op1=ALU.add), r=[bac, ini[ix][1], bset], w=[bac])
                    vend = ac[:, 3:4]
                else:
                    vend = ac[:, 2:3]
                col = itn % 8
                S.op("pe", lambda e: e.matmul(RB[:, col:col + 1], ROT[k][0][:], vend, start=True, stop=True), r=[ROT[k][1], bac], w=[bRB])
                nx_ = ini[(itn + 2) % 4]
                S.op("act", lambda e: e.activation(out=nx_[0][:], in_=RB[:, col:col + 1], func=AF.Copy), r=[bRB], w=[nx_[1]])
                return
            (a1, ba1), (v_t, bv) = t1[ix], vv[ix]
            if seg == 0:
                init, rr = 0.0, [ba1, bset]
            else:
                init, rr = ini[ix][0][:, 0:1], [ba1, bset, ini[ix][1]]
            S.op("dve", lambda e: e.tensor_tensor_scan(out=v_t[:], data0=mag[:, g:g + 1].broadcast_to([128, 512]), data1=a1[:],
                                                       initial=init, op0=ALU.mult, op1=ALU.add), r=rr, w=[bv])
            if seg < 7:
                col = itn % 8
                S.op("pe", lambda e: e.matmul(RB[:, col:col + 1], ROT[k][0][:], v_t[:, 511:512], start=True, stop=True),
                     r=[ROT[k][1], bv], w=[bRB])
                nx_ = ini[(itn + 2) % 4]
                S.op("act", lambda e: e.activation(out=nx_[0][:], in_=RB[:, col:col + 1], func=AF.Copy), r=[bRB], w=[nx_[1]])
            if seg >= 4:
                (x1, bx1), (x2, bx2) = q1[ix], q2[ix]
                (vb_, bvb) = vb[ix]
                S.op("act", lambda e: e.activation(out=vb_[:], in_=v_t[:], func=AF.Copy), r=[bv], w=[bvb])
                S.op("dve", lambda e: e.tensor_tensor(out=x1[:], in0=vb_[:], in1=CSb[k][0][:], op=ALU.mult), r=[bvb, CSb[k][1]], w=[bx1])
                S.op("dve", lambda e: e.tensor_tensor(out=x2[:], in0=vb_[:], in1=SNb[k][0][:], op=ALU.mult), r=[bvb, SNb[k][1]], w=[bx2])

        def stage_c(itn, g, seg, g8):
            k, ix = g % NSET, itn % 4
            if seg >= 4:
                (x1, bx1), (x2, bx2) = q1[ix], q2[ix]
                Y, bY = C.ps[4 + seg - 4], C.bps[4 + seg - 4]
                S.op("pe", lambda e: e.matmul(Y[:], bm[k][2][0][:], x1[:], start=False, stop=False), r=[bm[k][2][1], bx1], w=[bY])
                S.op("pe", lambda e: e.matmul(Y[:], bm[k][3][0][:], x2[:], start=False, stop=(g8 == 7)), r=[bm[k][3][1], bx2], w=[bY])

        prep_group(gb_list[0] * 8)
        prep_group(gb_list[0] * 8 + 1)
        for gi, gb in enumerate(gb_list):
            z_t, bz = zt[gi % 2]
            S.dma("sp", z_t[:], T["ZT"][gb * 128:(gb + 1) * 128, :], w=[bz])
            for so_ in range(4):
                S.op("pe", lambda e: e.matmul(C.ps[4 + so_][:], DST[:, gb, :], z_t[:, 2048 + so_ * 512:2048 + (so_ + 1) * 512],
                                              start=True, stop=False), r=[bset, bz], w=[C.bps[4 + so_]])
            prev = None
            prev2 = None
            for pr in range(4):
                pair = (gb * 8 + pr * 2, gb * 8 + pr * 2 + 1)
                for seg in range(8):
                    for g in pair:
                        itn = cnt["it"]
                        cnt["it"] += 1
                        stage_a(itn, g, seg, z_t, bz)
                        if prev is not None:
                            stage_b(*prev)
                        if prev2 is not None:
                            stage_c(*prev2)
                        prev2 = prev
                        prev = (itn, g, seg, g % 8)
                    if seg == 3:
                        if pr < 3:
                            nxt = (pair[0] + 2, pair[1] + 2)
                        elif gi + 1 < len(gb_list):
                            nxt = (gb_list[gi + 1] * 8, gb_list[gi + 1] * 8 + 1)
                        else:
                            nxt = ()
                        for g_ in nxt:
                            prep_group(g_)
                    if CONV_INTERLEAVE and seg in (1, 3, 5, 7) and conv_jobs:
                        o_ap, i_ap = conv_jobs.pop(0)
                        S.dma("pool", o_ap, i_ap)
            stage_b(*prev)
            stage_c(*prev2)
            stage_c(*prev)
            for so_ in range(4):
                y_t, by = ysb[cnt["y"] % 2]
                cnt["y"] += 1
                S.op("act", lambda e: e.activation(out=y_t[:], in_=C.ps[4 + so_][:], func=GELU), r=[C.bps[4 + so_]], w=[by])
                S.dma("sp", T["YG"][gb * 128:(gb + 1) * 128, so_ * 512:(so_ + 1) * 512], y_t[:], r=[by])
        while conv_jobs:
            o_ap, i_ap = conv_jobs.pop(0)
            S.dma("pool", o_ap, i_ap)
        S.barrier()


def rstd_ops(S, src_ap, dst, bdst, tmp, n, scale, rsrc):
    S.op("dve", lambda e: e.tensor_scalar(out=tmp[:, 0:n], in0=src_ap, scalar1=scale, scalar2=EPS, op0=ALU.mult, op1=ALU.add),
         r=list(rsrc) + [bdst], w=[bdst])
    S.op("act", lambda e: e.activation(out=tmp[:, n:2 * n], in_=tmp[:, 0:n], func=AF.Sqrt), r=[bdst], w=[bdst])
    S.op("dve", lambda e: e.reciprocal(out=dst, in_=tmp[:, n:2 * n]), r=[bdst], w=[bdst])


def phase_b(C, st_list, stop_after=None):
    nc, S, T = C.nc, C.S, C.T
    with contextlib.ExitStack() as pbs:
        raw = lambda name, shape, dt: pbs.enter_context(nc.sbuf_tensor(_uname("sr_" + name), list(shape), dt))
        bK = Buf("bconst")
        so = lambda k, fn: S.op(k, fn, r=[bK], w=[bK])
        wmT = raw("wmT", [128, 8, 128], BF16)
        bsr = raw("bsr", [128, 8, 128], F32)
        BIAS = raw("BIAS", [128, 16, 128], F32)
        cols = raw("cols", [128, 4, 16], F32)
        ones = raw("ones", [128, 128], BF16)
        S.dma("pool", wmT[:], T["wmT"][:], w=[bK])
        S.dma("sp", bsr[:], T["bs_rep"][:], w=[bK])
        S.dma("sp", cols[:], T["gm_cols"][:], w=[bK])
        so("dve", lambda e: e.memset(ones[:], 1.0))
        so("dve", lambda e: e.memset(wmT[64:128, :, 0:64], 0.0))
        for hh in range(8):
            bank = C.ps[hh // 4]
            S.op("pe", lambda e: e.matmul(bank[:, (hh % 4) * 128:(hh % 4 + 1) * 128], ones[:], wmT[:, hh, :], start=True, stop=True),
                 r=[bK], w=[C.bps[hh // 4]])
        for ct in range(16):
            hh = ct // 2
            bank = C.ps[hh // 4]
            S.op("dve", lambda e: e.scalar_tensor_tensor(out=BIAS[:, ct, :], in0=bank[:, (hh % 4) * 128:(hh % 4 + 1) * 128],
                                                         scalar=cols[:, 1, ct:ct + 1], in1=bsr[:, hh, :], op0=ALU.mult, op1=ALU.add),
                 r=[bK, C.bps[hh // 4]], w=[bK])
        hT = [_tile(nc, pbs, f"h{i}", [128, D], F32) for i in range(4)]
        rstd, brstd = _tile(nc, pbs, "rstdAB", [128, 8], F32)
        rtmp = raw("rtmp", [128, 16], F32)
        rtmp2 = raw("rtmp2", [128, 16], F32)
        wglu_v = T["w_glu"].rearrange("(c p) n -> p c n", p=128)
        nb = 0
        for st in st_list:
            t0 = st * TS
            with contextlib.ExitStack() as b1:
                tl = lambda name, shape, dt: _tile(nc, b1, name, shape, dt)
                yT, byT = _tile(nc, b1, "yT", [128, 32, TS], BF16)
                with contextlib.ExitStack() as b1a:
                    tla = lambda name, shape, dt: _tile(nc, b1a, name, shape, dt)
                    ygT, bygT = tla("ygT", [128, 16, TS], BF16)
                    uT, buT = tla("uT", [128, 16, TS], BF16)
                    wg = [tla(f"wg{i}", [128, 16, 256], BF16) for i in range(2)]
                    sig = [tla(f"sig{i}", [128, TS], F32) for i in range(3)]
                    ypre = [tla(f"ypre{i}", [128, TS], BF16) for i in range(3)]
                    ysq = [tla(f"ysq{i}", [128, TS], BF16) for i in range(3)]
                    vnt = [tla(f"vnt{i}", [128, 2048], BF16) for i in range(2)]
                    S.dma("sp", ygT[:], T["YG"].rearrange("(c p) t -> p c t", p=128)[:, :, t0:t0 + TS], w=[bygT])
                    S.dma("sp", uT[:], T["UT"].rearrange("(c p) t -> p c t", p=128)[:, :, t0:t0 + TS], w=[buT])
                    SSQ, bSSQ = C.ps[7], C.bps[7]
                    n2 = 0
                    deferred = []
                    for oc2 in range(8):
                        (w_t, bw) = wg[oc2 % 2]
                        S.dma("pool", w_t[:], wglu_v[:, :, oc2 * 256:(oc2 + 1) * 256], w=[bw])
                        for sub in range(2):
                            oc = oc2 * 2 + sub
                            pb_, bpb = C.ps[nb % 6], C.bps[nb % 6]
                            nb += 1
                            for ci in range(16):
                                S.op("pe", lambda e: e.matmul(pb_[:], w_t[:, ci, sub * 128:(sub + 1) * 128], ygT[:, ci, :],
                                                              start=(ci == 0), stop=(ci == 15)), r=[bw, bygT], w=[bpb], signal=(ci == 15))
                            while len(deferred) > 1:
                                deferred.pop(0)()
                            (sg_, bsg), (yp, byp), (yq, byq) = sig[n2 % 3], ypre[n2 % 3], ysq[n2 % 3]
                            n2 += 1
                            S.op("act", lambda e: e.activation(out=sg_[:], in_=pb_[:], func=AF.Sigmoid), r=[bpb], w=[bsg])
                            S.op("dve", lambda e: e.tensor_tensor(out=yp[:], in0=ygT[:, oc, :], in1=sg_[:], op=ALU.mult), r=[bygT, bsg], w=[byp])
                            S.op("act", lambda e: e.activation(out=yq[:], in_=yp[:], func=AF.Square), r=[byp], w=[byq])
                            S.op("dve", lambda e: e.tensor_scalar(out=yT[:, oc, :], in0=yp[:], scalar1=cols[:, 2, oc:oc + 1], scalar2=None,
                                                                  op0=ALU.mult), r=[byp, bK], w=[byT])
                            def ssq_a(oc=oc, yq=yq, byq=byq):
                                for tt in range(4):
                                    S.op("pe", lambda e: e.matmul(SSQ[:, oc * 4 + tt:oc * 4 + tt + 1], yq[:, tt * 128:(tt + 1) * 128],
                                                                  ones[:, 0:1], start=True, stop=True), r=[byq, bK], w=[bSSQ])
                            deferred.append(ssq_a)
                    for tt in range(4):
                        (v_t, bv) = vnt[tt % 2]
                        S.dma("sp", v_t[:], T["VN"][t0 + tt * 128:t0 + (tt + 1) * 128, :], w=[bv])
                        for cq in range(4):
                            pb_, bpb = C.ps[nb % 6], C.bps[nb % 6]
                            nb += 1
                            for j in range(4):
                                ct = cq * 4 + j
                                S.op("pe", lambda e: e.matmul(pb_[:, j * 128:(j + 1) * 128], v_t[:, ct * 128:(ct + 1) * 128], wmT[:, ct // 2, :],
                                                              start=True, stop=True), r=[bv, bK], w=[bpb], signal=(j == 3))
                            while len(deferred) > 1:
                                deferred.pop(0)()
                            (sg_, bsg), (yp, byp), (yq, byq) = sig[n2 % 3], ypre[n2 % 3], ysq[n2 % 3]
                            n2 += 1
                            for j in range(4):
                                ct = cq * 4 + j
                                S.op("dve", lambda e: e.scalar_tensor_tensor(out=sg_[:, j * 128:(j + 1) * 128], in0=pb_[:, j * 128:(j + 1) * 128],
                                                                             scalar=cols[:, 0, ct:ct + 1], in1=BIAS[:, ct, :],
                                                                             op0=ALU.mult, op1=ALU.add), r=[bpb, bK], w=[bsg])
                            S.op("dve", lambda e: e.tensor_tensor(out=yp[:].rearrange("p (j t) -> p j t", j=4),
                                                                  in0=sg_[:].rearrange("p (j t) -> p j t", j=4),
                                                                  in1=uT[:, cq * 4:(cq + 1) * 4, tt * 128:(tt + 1) * 128], op=ALU.mult),
                                 r=[bsg, buT], w=[byp])
                            S.op("act", lambda e: e.activation(out=yq[:], in_=yp[:], func=AF.Square), r=[byp], w=[byq])
                            for j in range(4):
                                ct = cq * 4 + j
                                S.op("act", lambda e: e.activation(out=yT[:, 16 + ct, tt * 128:(tt + 1) * 128], in_=yp[:, j * 128:(j + 1) * 128],
                                                                   func=AF.Copy, scale=cols[:, 3, ct:ct + 1]), r=[byp, bK], w=[byT])

                            def ssq_b(cq=cq, tt=tt, yq=yq, byq=byq):
                                for j in range(4):
                                    ct = cq * 4 + j
                                    S.op("pe", lambda e: e.matmul(SSQ[:, 64 + ct * 4 + tt:64 + ct * 4 + tt + 1], yq[:, j * 128:(j + 1) * 128],
                                                                  ones[:, 0:1], start=True, stop=True), r=[byq, bK], w=[bSSQ])
                            deferred.append(ssq_b)
                    while deferred:
                        deferred.pop(0)()
                    S.op("dve", lambda e: e.reduce_sum(out=rtmp[:, 8:16].rearrange("p (a t) -> p a t", a=2),
                                                       in_=SSQ[:, 0:128].rearrange("p (a o t) -> p a t o", a=2, t=4),
                                                       axis=AX.X), r=[bSSQ, brstd], w=[brstd])
                    rstd_ops(S, rtmp[:, 8:16], rstd[:], brstd, rtmp2, 8, 1.0 / 2048, [])
                    if "YTd" in T:
                        S.dma("sp", T["YTd"].rearrange("(c p) t -> p c t", p=128), yT[:], r=[byT], is_output=True)
                        S.dma("sp", T["RSd"][:], rstd[:], r=[brstd], is_output=True)
                    S.barrier()
                with contextlib.ExitStack() as b2:
                    tlb = lambda name, shape, dt: _tile(nc, b2, name, shape, dt)
                    wo = [tlb(f"wo{i}", [128, 32, 512], BF16) for i in range(2)]
                    xs = [tlb(f"xs{i}", [128, 512], F32) for i in range(2)]
                    tm = [tlb(f"tm{i}", [128, 512], F32) for i in range(2)]
                    n3 = 0
                    for ds in range(8):
                        (w_t, bw) = wo[ds % 2]
                        S.dma("pool", w_t[:], T["WoS"][ds], w=[bw])
                        for tt in range(4):
                            PA, bPA = C.ps[nb % 8], C.bps[nb % 8]
                            PB, bPB = C.ps[(nb + 1) % 8], C.bps[(nb + 1) % 8]
                            nb += 2
                            for ci in range(16):
                                S.op("pe", lambda e: e.matmul(PA[:], yT[:, ci, tt * 128:(tt + 1) * 128], w_t[:, ci, :],
                                                              start=(ci == 0), stop=(ci == 15)), r=[byT, bw], w=[bPA], signal=(ci == 15))
                            for ci in range(16, 32):
                                S.op("pe", lambda e: e.matmul(PB[:], yT[:, ci, tt * 128:(tt + 1) * 128], w_t[:, ci, :],
                                                              start=(ci == 16), stop=(ci == 31)), r=[byT, bw], w=[bPB], signal=(ci == 31))
                            (x_t, bx), (t_t, bt) = xs[n3 % 2], tm[n3 % 2]
                            n3 += 1
                            S.dma("sp", x_t[:], T["x_own"][t0 + tt * 128:t0 + (tt + 1) * 128, ds * 512:(ds + 1) * 512], w=[bx])
                            S.op("dve", lambda e: e.scalar_tensor_tensor(out=t_t[:], in0=PA[:], scalar=rstd[:, tt:tt + 1], in1=x_t[:],
                                                                         op0=ALU.mult, op1=ALU.add), r=[bPA, brstd, bx], w=[bt])
                            S.op("dve", lambda e: e.scalar_tensor_tensor(out=hT[tt][0][:, ds * 512:(ds + 1) * 512], in0=PB[:],
                                                                         scalar=rstd[:, 4 + tt:5 + tt], in1=t_t[:],
                                                                         op0=ALU.mult, op1=ALU.add), r=[bPB, brstd, bt], w=[hT[tt][1]])
                    S.barrier()
            if stop_after == "B2":
                for tt in range(4):
                    S.dma("sp", T["out"][t0 + tt * 128:t0 + (tt + 1) * 128, :], hT[tt][0][:], r=[hT[tt][1]], is_output=True)
                S.barrier()
                continue
            with contextlib.ExitStack() as pk:
                xnT, bxnT = _tile(nc, pk, "xnT", [128, 32, TS], BF16)
                with contextlib.ExitStack() as b34:
                    qT, bqT = _tile(nc, b34, "qT", [128, 16, TS], BF16)
                    with contextlib.ExitStack() as b3:
                        tl = lambda name, shape, dt: _tile(nc, b3, name, shape, dt)
                        gffn, bgffn = tl("gffn", [128, D], F32)
                        S.dma("sp", gffn[:], T["gffn_r"][:], w=[bgffn])
                        xn = [tl(f"xn{i}", [128, D], BF16) for i in range(2)]
                        junk, bjunk = tl("junkB", [128, D], BF16)
                        s8 = [tl(f"s8b{i}", [128, 8], F32) for i in range(2)]
                        wq = [tl(f"wq{i}", [128, 32, 256], BF16) for i in range(2)]
                        for tt in range(4):
                            (x_t, bx), (s_, bs_) = xn[tt % 2], s8[tt % 2]
                            h_t, bh = hT[tt]
                            S.op("dve", lambda e: e.memset(s_[:], 0.0), w=[bs_])
                            S.op("act", lambda e: e.activation(out=junk[:], in_=h_t[:], func=AF.Square, accum_out=s_[:, 0:1]),
                                 r=[bh], w=[bjunk, bs_])
                            rstd_ops(S, s_[:, 0:1], s_[:, 1:2], bs_, s_[:, 2:4], 1, 1.0 / D, [])
                            S.op("dve", lambda e: e.scalar_tensor_tensor(out=x_t[:], in0=h_t[:], scalar=s_[:, 1:2], in1=gffn[:],
                                                                         op0=ALU.mult, op1=ALU.mult), r=[bh, bs_, bgffn], w=[bx])
                            for dcg in range(4):
                                pb_, bpb = C.ps[nb % 8], C.bps[nb % 8]
                                nb += 1
                                pbb = pb_[:].bitcast(BF16)
                                for j in range(8):
                                    dc = dcg * 8 + j
                                    S.op("pe", lambda e: e.transpose(out=pbb[:, j * 128:(j + 1) * 128], in_=x_t[:, dc * 128:(dc + 1) * 128],
                                                                     identity=C.identb[:]), r=[bx, C.bidentb], w=[bpb], signal=(j == 7))
                                src = pbb[:, 0:1024].rearrange("p (j t) -> p j t", j=8)
                                dst = xnT[:, dcg * 8:(dcg + 1) * 8, tt * 128:(tt + 1) * 128]
                                if dcg % 2 == 0:
                                    S.op("act", lambda e: e.activation(out=dst, in_=src, func=AF.Copy), r=[bpb], w=[bxnT])
                                else:
                                    S.op("dve", lambda e: e.tensor_copy(out=dst, in_=src), r=[bpb], w=[bxnT])
                        for qc in range(8):
                            (w_t, bw) = wq[qc % 2]
                            S.dma("pool", w_t[:], T["WqS"][qc], w=[bw])
                            for sub in range(2):
                                pb_, bpb = C.ps[nb % 8], C.bps[nb % 8]
                                nb += 1
                                for dc in range(32):
                                    S.op("pe", lambda e: e.matmul(pb_[:], w_t[:, dc, sub * 128:(sub + 1) * 128], xnT[:, dc, :],
                                                                  start=(dc == 0), stop=(dc == 31)), r=[bw, bxnT], w=[bpb], signal=(dc == 31))
                                S.op("act", lambda e: e.activation(out=qT[:, qc * 2 + sub, :], in_=pb_[:], func=AF.Copy), r=[bpb], w=[bqT])
                        if "Qd" in T:
                            S.dma("sp", T["Qd"].rearrange("(c p) t -> p c t", p=128), qT[:], r=[bqT], is_output=True)
                        S.barrier()
                    with contextlib.ExitStack() as b4:
                        raw4 = lambda name, shape, dt: b4.enter_context(nc.sbuf_tensor(_uname("sr_" + name), list(shape), dt))
                        bR = Buf("route")
                        ro = lambda k, fn, extra_r=(), extra_w=(): S.op(k, fn, r=[bR] + list(extra_r), w=[bR] + list(extra_w))
                        S1 = [raw4(f"S{a}sb", [128, 8, 128], F32) for a in range(2)]
                        Vv = [raw4(f"V{a}", [128, 8, 16], F32) for a in range(2)]
                        Iu = [raw4(f"I{a}u", [128, 8, 16], U32) for a in range(2)]
                        If_ = [raw4(f"I{a}f", [128, 8, 16], F32) for a in range(2)]
                        tmp16 = raw4("tmp16", [128, 16, 128], F32)
                        tmpc = raw4("tmpc", [128, 8, 256], F32)
                        bAH = [[Buf(f"ah{a}{h}") for h in range(8)] for a in range(2)]
                        bH = [Buf(f"h{h}") for h in range(8)]
                        cand = raw4("cand", [128, 8, 256], F32)
                        SC = raw4("SC", [128, 8, 16], F32)
                        CIu = raw4("CIu", [128, 8, 16], U32)
                        CIf = raw4("CIf", [128, 8, 16], F32)
                        ii = raw4("ii", [128, 8, 16], mybir.dt.int32)
                        irf = raw4("irf", [128, 8, 16], F32)
                        jrf = raw4("jrf", [128, 8, 16], F32)
                        ex = raw4("ex", [128, 8, 16], F32)
                        zz = raw4("zz", [128, 16], F32)
                        gate = raw4("gate", [128, 8, 16], F32)
                        e12 = [raw4(f"e{a}r", [128, 8, 16], F32) for a in range(2)]
                        tr3 = raw4("tr3", [128, 3, 128], F32)
                        At = [_tile(nc, b4, f"At{i}", [128, 128], BF16) for i in range(4)]
                        Bt = [_tile(nc, b4, f"Bt{i}", [128, 128], BF16) for i in range(4)]
                        GTt = [_tile(nc, b4, f"GTt{i}", [128, 128, 128], BF16) for i in range(1)]
                        io16 = C.iota128[:, 0:16]
                        for tt in range(4):
                            tsl = slice(tt * 128, (tt + 1) * 128)
                            for hh in range(8):
                                for a in range(2):
                                    bk = a * 2 + hh // 4
                                    S.op("pe", lambda e: e.matmul(C.ps[bk][:, (hh % 4) * 128:(hh % 4 + 1) * 128], qT[:, 2 * hh + a, tsl],
                                                                  C.kT[a][:], start=True, stop=True), r=[bqT, C.bkT], w=[C.bps[bk]])
                            for a in range(2):
                                for hb in range(2):
                                    ro("act", lambda e: e.activation(out=S1[a][:, hb * 4:(hb + 1) * 4, :],
                                                                     in_=C.ps[a * 2 + hb][:].rearrange("p (h n) -> p h n", h=4), func=AF.Copy),
                                       extra_r=[C.bps[a * 2 + hb]])
                            ah = [(a, hh) for a in range(2) for hh in range(8)]
                            for (a, hh) in ah:
                                S.op("dve", lambda e: e.max(out=Vv[a][:, hh, 0:8], in_=S1[a][:, hh, :]), r=[bR], w=[bAH[a][hh]])
                            for (a, hh) in ah:
                                S.op("dve", lambda e: e.max_index(out=Iu[a][:, hh, 0:8], in_max=Vv[a][:, hh, 0:8], in_values=S1[a][:, hh, :]),
                                     r=[bR, bAH[a][hh]], w=[bAH[a][hh]])
                            for (a, hh) in ah:
                                S.op("dve", lambda e: e.match_replace(out=tmp16[:, a * 8 + hh, :], in_to_replace=Vv[a][:, hh, 0:8],
                                                                      in_values=S1[a][:, hh, :], imm_value=-1e30), r=[bR, bAH[a][hh]], w=[bAH[a][hh]])
                            for (a, hh) in ah:
                                S.op("dve", lambda e: e.max(out=Vv[a][:, hh, 8:16], in_=tmp16[:, a * 8 + hh, :]), r=[bAH[a][hh]], w=[bAH[a][hh]])
                            for (a, hh) in ah:
                                S.op("dve", lambda e: e.max_index(out=Iu[a][:, hh, 8:16], in_max=Vv[a][:, hh, 8:16], in_values=tmp16[:, a * 8 + hh, :]),
                                     r=[bAH[a][hh]], w=[bAH[a][hh]])
                            for a in range(2):
                                ro("dve", lambda e: e.tensor_copy(out=If_[a][:], in_=Iu[a][:]), extra_r=bAH[a], extra_w=bAH[a])
                            for hh in range(8):
                                S.op("dve", lambda e: e.tensor_tensor(out=cand[:, hh, :].rearrange("p (i j) -> p i j", i=16),
                                                                      in0=Vv[0][:, hh, :].unsqueeze(2).broadcast_to([128, 16, 16]),
                                                                      in1=Vv[1][:, hh, :].unsqueeze(1).broadcast_to([128, 16, 16]), op=ALU.add),
                                     r=[bAH[0][hh], bAH[1][hh], bR], w=[bH[hh]])
                            for hh in range(8):
                                S.op("dve", lambda e: e.max(out=SC[:, hh, 0:8], in_=cand[:, hh, :]), r=[bH[hh]], w=[bH[hh]])
                            for hh in range(8):
                                S.op("dve", lambda e: e.max_index(out=CIu[:, hh, 0:8], in_max=SC[:, hh, 0:8], in_values=cand[:, hh, :]), r=[bH[hh]], w=[bH[hh]])
                            for hh in range(8):
                                S.op("dve", lambda e: e.match_replace(out=tmpc[:, hh, :], in_to_replace=SC[:, hh, 0:8], in_values=cand[:, hh, :],
                                                                      imm_value=-1e30), r=[bH[hh], bR], w=[bH[hh]])
                            for hh in range(8):
                                S.op("dve", lambda e: e.max(out=SC[:, hh, 8:16], in_=tmpc[:, hh, :]), r=[bH[hh]], w=[bH[hh]])
                            for hh in range(8):
                                S.op("dve", lambda e: e.max_index(out=CIu[:, hh, 8:16], in_max=SC[:, hh, 8:16], in_values=tmpc[:, hh, :]), r=[bH[hh]], w=[bH[hh]])
                            ro("dve", lambda e: e.tensor_copy(out=CIf[:], in_=CIu[:]), extra_r=bH, extra_w=bH)
                            ro("dve", lambda e: e.tensor_tensor(out=ex[:], in0=SC[:], in1=SC[:, :, 0:1].broadcast_to([128, 8, 16]), op=ALU.subtract))
                            ro("act", lambda e: e.activation(out=ex[:], in_=ex[:], func=AF.Exp))
                            ro("dve", lambda e: e.reduce_sum(out=zz[:, 0:8], in_=ex[:], axis=AX.X))
                            ro("dve", lambda e: e.reciprocal(out=zz[:, 8:16], in_=zz[:, 0:8]))
                            ro("dve", lambda e: e.tensor_tensor(out=gate[:], in0=ex[:], in1=zz[:, 8:16].unsqueeze(2).broadcast_to([128, 8, 16]),
                                                                op=ALU.mult))
                            ro("dve", lambda e: e.tensor_scalar(out=ii[:], in0=CIf[:], scalar1=1.0 / 16, scalar2=-0.46875, op0=ALU.mult, op1=ALU.add))
                            ro("dve", lambda e: e.tensor_copy(out=irf[:], in_=ii[:]))
                            ro("dve", lambda e: e.scalar_tensor_tensor(out=jrf[:], in0=irf[:], scalar=-16.0, in1=CIf[:], op0=ALU.mult, op1=ALU.add))
                            for a, sel in ((0, irf), (1, jrf)):
                                for hh in range(8):
                                    ro("dve", lambda e: e.tensor_tensor(out=tmpc[:, hh, :].rearrange("p (k i) -> p k i", k=16), in0=sel[:, hh, :].unsqueeze(2).broadcast_to([128, 16, 16]),
                                                                        in1=io16.unsqueeze(1).broadcast_to([128, 16, 16]), op=ALU.is_equal))
                                    ro("dve", lambda e: e.tensor_tensor(out=tmpc[:, hh, :].rearrange("p (k i) -> p k i", k=16), in0=tmpc[:, hh, :].rearrange("p (k i) -> p k i", k=16),
                                                                        in1=If_[a][:, hh, :].unsqueeze(1).broadcast_to([128, 16, 16]), op=ALU.mult))
                                ro("dve", lambda e: e.reduce_sum(out=e12[a][:].rearrange("p h k -> p (h k)"),
                                                                 in_=tmpc[:].rearrange("p h (k i) -> p (h k) i", k=16), axis=AX.X))
                            TRB, bTRB = C.ps[4], C.bps[4]
                            for n_, src in enumerate((e12[0], e12[1], gate)):
                                ro("pe", lambda e: e.transpose(out=TRB[:, n_ * 128:(n_ + 1) * 128], in_=src[:].rearrange("p h k -> p (h k)"),
                                                               identity=C.identf[:]), extra_r=[C.bidentf], extra_w=[bTRB])
                            ro("act", lambda e: e.activation(out=tr3[:].rearrange("p a t -> p (a t)"), in_=TRB[:, 0:384], func=AF.Copy), extra_r=[bTRB])
                            if "R3d" in T and tt == 0:
                                S.dma("sp", T["R3d"][:], tr3[:], r=[bR], is_output=True)
                                S.dma("sp", T["S1d"][:], S1[0][:], r=[bR], is_output=True)
                                S.dma("sp", T["V1d"][:], Vv[0][:], r=[bR], is_output=True)
                                S.dma("sp", T["SCd"][:], SC[:], r=[bR], is_output=True)
                                S.dma("sp", T["CId"][:], CIf[:], r=[bR], is_output=True)
                                S.dma("sp", T["I1d"][:], If_[0][:], r=[bR], is_output=True)
                            g_t, bg = GTt[0]
                            for t4 in range(32):
                                pb_, bpb = C.ps[5 + t4 % 3], C.bps[5 + t4 % 3]
                                for tk in range(4):
                                    t = t4 * 4 + tk
                                    (a_t, ba), (b_t, bb) = At[t % 4], Bt[t % 4]
                                    S.op("dve", lambda e: e.tensor_scalar(out=a_t[:], in0=C.iota128[:], scalar1=tr3[:, 0, t:t + 1],
                                                                          scalar2=tr3[:, 2, t:t + 1], op0=ALU.is_equal, op1=ALU.mult),
                                         r=[bR, C.biota], w=[ba])
                                    S.op("dve", lambda e: e.tensor_scalar(out=b_t[:], in0=C.iota128[:], scalar1=tr3[:, 1, t:t + 1],
                                                                          scalar2=None, op0=ALU.is_equal), r=[bR, C.biota], w=[bb])
                                    S.op("pe", lambda e: e.matmul(pb_[:, tk * 128:(tk + 1) * 128], b_t[:], a_t[:], start=True, stop=True),
                                         r=[ba, bb], w=[bpb])
                                S.op("act", lambda e: e.activation(out=g_t[:, :, t4 * 4:(t4 + 1) * 4].rearrange("p e t -> p t e"),
                                                                   in_=pb_[:].rearrange("p (t e) -> p t e", t=4), func=AF.Copy), r=[bpb], w=[bg])
                            S.dma("sp", T["GT"][st * 4 + tt], g_t[:], r=[bg])
                        S.barrier()
                if stop_after == "B4":
                    continue
                with contextlib.ExitStack() as b5:
                    tl = lambda name, shape, dt: _tile(nc, b5, name, shape, dt)
                    Dn = [tl(f"Dn{i}", [128, 32, 256], BF16) for i in range(2)]
                    Up = [tl(f"Up{i}", [128, 8, 512], BF16) for i in range(2)]
                    act = [tl(f"act{i}", [128, 8, TS], BF16) for i in range(2)]
                    Gc = [tl(f"Gc{i}", [128, 8, TS], BF16) for i in range(2)]
                    gel = [tl(f"gel{i}", [128, TS], BF16) for i in range(2)]
                    nd = nu = ng = 0
                    first_pass = False
                    for cg in range(C.ncg):
                        (g_c, bgc), (a_c, bac) = Gc[cg % 2], act[cg % 2]
                        for tt in range(4):
                            S.dma("sp", g_c[:, :, tt * 128:(tt + 1) * 128], T["GT"][st * 4 + tt][:, cg * 8:(cg + 1) * 8, :], w=[bgc])
                        for cp in range(4):
                            (d_t, bd) = Dn[nd % 2]
                            nd += 1
                            e0 = (cg * 8 + cp * 2) * 128
                            if first_pass:
                                S.dma("pool", d_t[:], T["downT"][cg * 4 + cp], w=[bd])
                                S.dma("sp", T["DnS"][cg * 4 + cp], d_t[:], r=[bd])
                            else:
                                S.dma("sp", d_t[:], T["DnS"][cg * 4 + cp], w=[bd])
                            for ck in range(2):
                                ci = cp * 2 + ck
                                pb_, bpb = C.ps[nb % 8], C.bps[nb % 8]
                                nb += 1
                                for dc in range(32):
                                    S.op("pe", lambda e: e.matmul(pb_[:], d_t[:, dc, ck * 128:(ck + 1) * 128], xnT[:, dc, :],
                                                                  start=(dc == 0), stop=(dc == 31)), r=[bd, bxnT], w=[bpb], signal=(dc == 31))
                                (ge, bge) = gel[ng % 2]
                                ng += 1
                                S.op("act", lambda e: e.activation(out=ge[:], in_=pb_[:], func=GELU), r=[bpb], w=[bge])
                                S.op("dve", lambda e: e.tensor_tensor(out=a_c[:, ci, :], in0=ge[:], in1=g_c[:, ci, :], op=ALU.mult),
                                     r=[bge, bgc], w=[bac])
                        for ds in range(8):
                            (u_t, bu) = Up[nu % 2]
                            nu += 1
                            if first_pass:
                                S.dma("pool", u_t[:], T["up"][cg * 8 + ds], w=[bu])
                                S.dma("sp", T["UpS"][cg * 8 + ds], u_t[:], r=[bu])
                            else:
                                S.dma("pool", u_t[:], T["UpS"][cg * 8 + ds], w=[bu])
                            for tt in range(4):
                                pb_, bpb = C.ps[nb % 8], C.bps[nb % 8]
                                nb += 1
                                for ci in range(8):
                                    S.op("pe", lambda e: e.matmul(pb_[:], a_c[:, ci, tt * 128:(tt + 1) * 128], u_t[:, ci, :],
                                                                  start=(ci == 0), stop=(ci == 7)), r=[bac, bu], w=[bpb], signal=(ci == 7))
                                hs = hT[tt][0][:, ds * 512:(ds + 1) * 512]
                                S.op("dve", lambda e: e.tensor_tensor(out=hs, in0=pb_[:], in1=hs, op=ALU.add), r=[bpb, hT[tt][1]], w=[hT[tt][1]])
                    S.barrier()
            if "Hd" in T:
                for tt in range(4):
                    S.dma("sp", T["Hd"][tt * 128:(tt + 1) * 128, :], hT[tt][0][:], r=[hT[tt][1]], is_output=True)
            with contextlib.ExitStack() as b6:
                tl = lambda name, shape, dt: _tile(nc, b6, name, shape, dt)
                gfin, bgfin = tl("gfin", [128, D], F32)
                S.dma("sp", gfin[:], T["gfin_r"][:], w=[bgfin])
                junk, bjunk = tl("junkC", [128, D], BF16)
                ot = [tl(f"ot{i}", [128, D], F32) for i in range(2)]
                s8 = [tl(f"s8c{i}", [128, 8], F32) for i in range(2)]
                for tt in range(4):
                    (o_t, bo), (s_, bs_) = ot[tt % 2], s8[tt % 2]
                    h_t, bh = hT[tt]
                    S.op("dve", lambda e: e.memset(s_[:], 0.0), w=[bs_])
                    S.op("act", lambda e: e.activation(out=junk[:], in_=h_t[:], func=AF.Square, accum_out=s_[:, 0:1]), r=[bh], w=[bjunk, bs_])
                    rstd_ops(S, s_[:, 0:1], s_[:, 1:2], bs_, s_[:, 2:4], 1, 1.0 / D, [])
                    S.op("dve", lambda e: e.scalar_tensor_tensor(out=o_t[:], in0=h_t[:], scalar=s_[:, 1:2], in1=gfin[:],
                                                                 op0=ALU.mult, op1=ALU.mult), r=[bh, bs_, bgfin], w=[bo])
                    S.dma("sp", T["out"][t0 + tt * 128:t0 + (tt + 1) * 128, :], o_t[:], r=[bo], is_output=True)
                S.barrier()
        S.barrier()


def build(dbg=None):
    nc = bass.Bass("TRN2", target_bir_lowering=False)
    T = {}

    def din(name, shape, dt=F32):
        T[name] = nc.dram_tensor(name, list(shape), dt, kind="ExternalInput").ap()

    def dscr(name, shape, dt, out=False):
        T[name] = nc.dram_tensor(name, list(shape), dt, kind="ExternalOutput" if out else "Internal").ap()

    din("x_own", [NTOK, D])
    din("x_prev", [NTOK, D])
    din("gmix_r", [128, D])
    din("w_in", [24, 128, 32, 256])
    din("ident", [128, 128])
    for n in ("a_re_q", "a_im_q", "ldt_q"):
        din(n, [128, 128])
    for n in ("a_re_b", "a_im_b", "ldt_b", "b_re_b", "b_im_b"):
        din(n, [128, 1024])
    din("cmix1", [128, 2048])
    din("cmix2", [128, 2048])
    din("d_col", [128, 16])
    din("sgn1", [128, 1])
    din("rowmask", [128, 8])
    din("colmask", [128, 8, 128])
    din("iota512", [128, 512])
    din("swapI", [128, 128])
    din("riota512", [128, 512])
    dscr("YG", [2048, NTOK], BF16, out=(dbg == "S5"))
    din("w_glu", [2048, 2048])
    if dbg == "B2":
        dscr("YTd", [4096, TS], BF16, out=True)
        dscr("RSd", [128, 8], F32, out=True)
    din("w_out", [8, 128, 32, 512])
    din("wmT", [128, 8, 128])
    din("bs_rep", [128, 8, 128])
    din("gm_cols", [128, 4, 16])
    din("gffn_r", [128, D])
    din("gfin_r", [128, D])
    din("w_q", [8, 128, 32, 256])
    din("k1T", [128, 128])
    din("k2T", [128, 128])
    din("downT", [64, 128, 32, 256])
    din("up", [128, 128, 8, 512])
    din("iota128", [128, 128])
    dscr("GT", [16, 128, 128, 128], BF16)
    dscr("DnS", [64, 128, 32, 256], BF16)
    dscr("WinS", [24, 128, 32, 256], BF16)
    dscr("WoS", [8, 128, 32, 512], BF16)
    dscr("WqS", [8, 128, 32, 256], BF16)
    dscr("UpS", [128, 128, 8, 512], BF16)
    if dbg == "B6x":
        dscr("Qd", [2048, TS], BF16, out=True)
        dscr("R3d", [128, 3, 128], F32, out=True)
        dscr("S1d", [128, 8, 128], F32, out=True)
        dscr("V1d", [128, 8, 16], F32, out=True)
        dscr("SCd", [128, 8, 16], F32, out=True)
        dscr("CId", [128, 8, 16], F32, out=True)
        dscr("I1d", [128, 8, 16], F32, out=True)
        dscr("Hd", [TS, D], F32, out=True)
    dscr("ZT", [2048, 4096], BF16, out=(dbg == "A"))
    dscr("UT", [2048, NTOK], BF16, out=(dbg == "A"))
    dscr("VN", [NTOK, 2048], BF16, out=(dbg == "A"))
    dscr("out", [NTOK, D], F32, out=True)

    with contextlib.ExitStack() as st:
        C = Ctx()
        C.nc, C.T = nc, T
        C.S = S = Sched(nc, st)
        C.ps = [st.enter_context(nc.psum_tensor(f"ps{i}", [128, 512], F32)) for i in range(8)]
        C.bps = [Buf(f"ps{i}") for i in range(8)]
        C.bZT, C.bUT, C.bVN, C.bYG, C.bGT = Buf("ZT"), Buf("UT"), Buf("VN"), Buf("YG"), Buf("GT")
        C.identb, C.bidentb = _tile(nc, st, "identb", [128, 128], BF16)
        C.identf, C.bidentf = _tile(nc, st, "identf", [128, 128], F32)
        S.dma("pool", C.identb[:], T["ident"][:], w=[C.bidentb])
        S.dma("sp", C.identf[:], T["ident"][:], w=[C.bidentf])
        C.iota128, C.biota = _tile(nc, st, "iota128", [128, 128], F32)
        S.dma("sp", C.iota128[:], T["iota128"][:], w=[C.biota])
        C.bkT = Buf("kT")
        C.kT = [st.enter_context(nc.sbuf_tensor(f"sb_k{a}T", [128, 128], BF16)) for a in range(2)]
        S.dma("pool", C.kT[0][:], T["k1T"][:], w=[C.bkT])
        S.dma("pool", C.kT[1][:], T["k2T"][:], w=[C.bkT])
        C.ncg = 16
        C.win_cached, C.win_fresh = set(), set()
        if dbg == "A":
            phase_a(C, [0, 4])
        else:
            phase_a(C, list(range(8)))
        if dbg == "S5":
            phase_s5(C, [0, 5])
        elif dbg != "TA":
            phase_s5(C, list(range(16)))
        if dbg == "B2":
            phase_b(C, [0], stop_after="B2")
        elif dbg == "B6":
            phase_b(C, [0])
        elif dbg == "B7":
            phase_b(C, [0, 1])
        elif dbg in ("TA", "TS"):
            pass
        elif dbg == "TB2":
            phase_b(C, [0], stop_after="B2")
        elif dbg == "TB4":
            phase_b(C, [0], stop_after="B4")
        elif dbg is None:
            phase_b(C, list(range(4)))
        S.finish()
        print("instructions:", S.nins, "sems:", S.nsem)
    return nc


def host_layout(inputs):
    f = lambda k: np.ascontiguousarray(np.asarray(inputs[k], dtype=np.float32))
    x = f("x")
    shared = {}
    shared["gmix_r"] = np.ascontiguousarray(np.broadcast_to(f("norm_mix_g")[0], (128, D)))
    shared["w_in"] = np.ascontiguousarray(f("w_in")[0].reshape(32, 128, 24, 256).transpose(2, 1, 0, 3))
    shared["ident"] = np.eye(128, dtype=np.float32)
    a_re, a_im, ldt = f("s5_a_re")[0], f("s5_a_im")[0], f("s5_log_dt")[0]
    b_re, b_im = f("s5_b_re")[0], f("s5_b_im")[0]
    c_re, c_im = f("s5_c_re")[0], f("s5_c_im")[0]
    cp = np.ascontiguousarray
    shared["a_re_q"] = cp(np.concatenate([a_re.T, a_re.T], axis=0))
    shared["a_im_q"] = cp(np.concatenate([a_im.T, a_im.T], axis=0))
    shared["ldt_q"] = cp(np.broadcast_to(ldt[None, :], (128, 128)))
    def blay(a_gp):
        t = a_gp.reshape(16, 8, 64)
        t = np.broadcast_to(t[:, :, None, :], (16, 8, 16, 64))
        return cp(t.transpose(1, 2, 0, 3).reshape(128, 1024))
    shared["a_re_b"] = blay(a_re)
    shared["a_im_b"] = blay(a_im)
    shared["ldt_b"] = blay(np.broadcast_to(ldt[:, None], (128, 64)))
    bl = lambda b: cp(b.reshape(16, 8, 64, 16).transpose(1, 3, 0, 2).reshape(128, 1024))
    shared["b_re_b"] = bl(b_re)
    shared["b_im_b"] = bl(b_im)
    cl = lambda c: c.transpose(2, 0, 1).reshape(64, 2048)
    shared["cmix1"] = cp(np.concatenate([cl(c_re), cl(c_im)], axis=0))
    shared["cmix2"] = cp(np.concatenate([cl(c_im), cl(c_re)], axis=0))
    shared["d_col"] = cp(f("s5_d")[0].reshape(16, 128).T)
    shared["sgn1"] = np.concatenate([np.ones((64, 1), np.float32), -np.ones((64, 1), np.float32)], axis=0)
    rm = np.zeros((128, 8), np.float32)
    cmk = np.zeros((128, 8, 128), np.float32)
    for g8 in range(8):
        rm[g8 * 16:(g8 + 1) * 16, g8] = 1.0
        cmk[:, g8, g8 * 16:(g8 + 1) * 16] = 1.0
    shared["rowmask"] = rm
    shared["colmask"] = cmk
    shared["w_glu"] = f("s5_w_glu")[0]
    shared["w_out"] = cp(f("w_out")[0].reshape(32, 128, 8, 512).transpose(2, 1, 0, 3))
    shared["wmT"] = cp(f("gm_w_s")[0].transpose(2, 0, 1))
    shared["bs_rep"] = cp(np.broadcast_to(f("gm_b_s")[0][None], (128, 8, 128)))
    colv = lambda v: v.reshape(16, 128).T
    shared["gm_cols"] = cp(np.stack([colv(f("gm_ln_g")[0]), colv(f("gm_ln_b")[0]), colv(f("norm_s5_out_g")[0]),
                                     colv(f("norm_gm_out_g")[0])], axis=1))
    shared["gffn_r"] = cp(np.broadcast_to(f("norm_ffn_g")[0], (128, D)))
    shared["gfin_r"] = cp(np.broadcast_to(f("norm_final_g"), (128, D)))
    shared["w_q"] = cp(f("peer_w_q")[0].reshape(32, 128, 8, 256).transpose(2, 1, 0, 3))
    shared["k1T"] = cp(f("peer_keys_1")[0].T)
    shared["k2T"] = cp(f("peer_keys_2")[0].T)
    shared["downT"] = cp(f("peer_down")[0].T.reshape(32, 128, 64, 256).transpose(2, 1, 0, 3))
    shared["up"] = cp(f("peer_up")[0].reshape(16, 8, 128, 8, 512).transpose(0, 3, 2, 1, 4).reshape(128, 128, 8, 512))
    shared["iota128"] = cp(np.broadcast_to(np.arange(128, dtype=np.float32)[None, :], (128, 128)))
    shared["swapI"] = cp(np.roll(np.eye(128, dtype=np.float32), 64, axis=1))
    shared["riota512"] = cp(np.broadcast_to(np.arange(511, -1, -1, dtype=np.float32)[None, :], (128, 512)))
    shared["iota512"] = cp(np.broadcast_to(np.arange(512, dtype=np.float32)[None, :], (128, 512)))
    maps = []
    for c in range(8):
        b, s = c // 2, c % 2
        m = dict(shared)
        m["x_own"] = np.ascontiguousarray(x[b, s * NTOK:(s + 1) * NTOK])
        m["x_prev"] = np.ascontiguousarray(x[b, 0:NTOK]) if s == 1 else np.zeros((NTOK, D), np.float32)
        maps.append(m)
    return maps


def kernel(**inputs):
    maps = host_layout(inputs)
    nc = build()
    res = run_bass_kernel_spmd(nc, maps, core_ids=list(range(8)))
    out = np.zeros((4, 4096, D), np.float32)
    for c in range(8):
        b, s = c // 2, c % 2
        out[b, s * NTOK:(s + 1) * NTOK] = res.results[c]["out"]
    return out
```

```python
import contextlib
import math

import ml_dtypes
import numpy as np

import concourse.bass as bass
import concourse.mybir as mybir
from concourse.bass_utils import run_bass_kernel_spmd

F32 = mybir.dt.float32
BF16 = mybir.dt.bfloat16
U32 = mybir.dt.uint32
ALU = mybir.AluOpType
AF = mybir.ActivationFunctionType
AX = mybir.AxisListType

D = 4096
NTOK = 2048
TS = 512
NST = NTOK // TS
EPS = 1e-6
TWO_PI = 2.0 * math.pi
GELU = AF.Gelu_apprx_tanh
CONV_INTERLEAVE = True


class Buf:
    __slots__ = ("name", "w", "r")

    def __init__(self, name=""):
        self.name = name
        self.w = None
        self.r = []


class Sched:
    EPOCH = 3500

    def __init__(self, nc, stack):
        self.nc = nc
        self.stack = stack
        self.eng = {"pe": nc.tensor, "dve": nc.vector, "act": nc.scalar, "pool": nc.gpsimd, "sp": nc.sync}
        self.sem = {}
        self.cnt = {}
        self.pending = {}
        self.seen = {k: {} for k in self.eng}
        self.nsem = 0
        self.last_old = {}
        for k in self.eng:
            self._new_epoch(k)
        self.dpool = {}
        for k, n in (("sp", 24), ("pool", 24), ("act", 8)):
            self.dpool[k] = [[self._mksem(f"d{k}{i}"), 0] for i in range(n)]
        self.dnext = {k: 0 for k in self.dpool}
        self.out_events = []
        self.nins = 0

    def _mksem(self, name):
        self.nsem += 1
        return self.stack.enter_context(self.nc.semaphore(f"{name}_{self.nsem}"))

    def _new_epoch(self, k):
        if k in self.sem and self.cnt[k] > 0:
            self.last_old[k] = (self.sem[k], self.cnt[k])
        self.sem[k] = self._mksem(f"e{k}")
        self.cnt[k] = 0
        self.pending[k] = False

    def _wait(self, k, evs):
        need = {}
        for (s, v) in evs:
            if self.seen[k].get(id(s), (None, 0))[1] >= v:
                continue
            if id(s) not in need or need[id(s)][1] < v:
                need[id(s)] = (s, v)
        for s, v in need.values():
            self.eng[k].wait_ge(s, v)
            self.seen[k][id(s)] = (s, v)

    def _deps(self, k, r, w):
        evs = []
        for b in r:
            if b.w is not None:
                evs.append(b.w)
        for b in w:
            if b.w is not None:
                evs.append(b.w)
            evs.extend(b.r)
        if k == "pe":
            evs = [e for e in evs if e[0] is not self.sem["pe"]]
        return evs

    def _mark(self, ev, r, w):
        for b in r:
            b.r.append(ev)
            if len(b.r) > 48:
                best = {}
                for (s, v) in b.r:
                    if id(s) not in best or best[id(s)][1] < v:
                        best[id(s)] = (s, v)
                b.r = list(best.values())
        for b in w:
            b.w = ev
            b.r = []

    def op(self, k, fn, r=(), w=(), signal=True):
        self._wait(k, self._deps(k, r, w))
        ins = fn(self.eng[k])
        self.nins += 1
        if signal:
            ins.then_inc(self.sem[k], 1)
            self.cnt[k] += 1
            self.pending[k] = False
            ev = (self.sem[k], self.cnt[k])
        else:
            self.pending[k] = True
            ev = (self.sem[k], self.cnt[k] + 1)
        self._mark(ev, r, w)
        if signal and self.cnt[k] >= self.EPOCH:
            self._new_epoch(k)
        return ev

    def dma(self, k, out, in_, r=(), w=(), is_output=False, **kw):
        pool = self.dpool[k]
        i = self.dnext[k]
        self.dnext[k] = (i + 1) % len(pool)
        slot = pool[i]
        evs = self._deps(k, r, w)
        if slot[1] > 0:
            evs.append((slot[0], slot[1]))
        self._wait(k, evs)
        ins = self.eng[k].dma_start(out=out, in_=in_, **kw)
        self.nins += 1
        slot[1] += 16
        ins.then_inc(slot[0], 16)
        ev = (slot[0], slot[1])
        self._mark(ev, r, w)
        if is_output:
            self.out_events.append(ev)
        return ev

    def barrier(self):
        evs = []
        for k in self.eng:
            assert not self.pending[k], k
            if self.cnt[k] > 0:
                evs.append((self.sem[k], self.cnt[k]))
            elif k in self.last_old:
                evs.append(self.last_old[k])
        for k in self.dpool:
            for s, v in self.dpool[k]:
                if v > 0:
                    evs.append((s, v))
        for k in self.eng:
            self._wait(k, evs)

    def barrier_dma(self):
        evs = []
        for k in self.dpool:
            for s_, v in self.dpool[k]:
                if v > 0:
                    evs.append((s_, v))
        for k in ("pool", "sp"):
            self._wait(k, evs)

    def finish(self):
        self._wait("sp", self.out_events)
        self.barrier()


class Ctx:
    pass


_UID = [0]


def _uname(name):
    _UID[0] += 1
    return f"{name}_{_UID[0]}"


def _tile(nc, stack, name, shape, dt):
    return stack.enter_context(nc.sbuf_tensor(_uname("sb_" + name), list(shape), dt)), Buf(name)


def phase_a(C, st_list):
    nc, S, T = C.nc, C.S, C.T
    with contextlib.ExitStack() as ph:
        tl = lambda name, shape, dt: _tile(nc, ph, name, shape, dt)
        gmix, bgmix = tl("gmix", [128, D], F32)
        S.dma("sp", gmix[:], T["gmix_r"][:], w=[bgmix])
        xt = [tl(f"xt{i}", [128, D], F32) for i in range(2)]
        abf = [tl(f"abf{i}", [128, D], BF16) for i in range(4)]
        aT, baT = tl("aT", [128, 32, TS], BF16)
        wb = [tl(f"wb{i}", [128, 32, 256], BF16) for i in range(2)]
        zs = [tl(f"zs{i}", [128, TS], BF16) for i in range(3)]
        V = [tl(f"V{i}", [128, 2048], F32) for i in range(4)]
        vn = [tl(f"vn{i}", [128, 2048], BF16) for i in range(2)]
        junk, bjunk = tl("junkA", [128, 2048], BF16)
        st8 = [tl(f"st8{i}", [128, 8], F32) for i in range(2)]
        w_in_v = T["w_in"].rearrange("(c p) n -> p c n", p=128)
        nx = 0
        nw = 0
        nz = 0
        nb = 0
        def norm_part(st):
            own = st >= NST
            xsrc = T["x_own"] if own else T["x_prev"]
            t0 = (st - NST if own else st) * TS
            for tt in range(4):
                (x_t, bx), (a_t, ba), (s8, bs8) = xt[tt % 2], abf[tt], st8[tt % 2]
                S.dma("sp", x_t[:], xsrc[t0 + tt * 128:t0 + (tt + 1) * 128, :], w=[bx])
                S.op("dve", lambda e: e.memset(s8[:], 0.0), w=[bs8])
                S.op("act", lambda e: e.activation(out=a_t[:], in_=x_t[:], func=AF.Square, accum_out=s8[:, 0:1]),
                     r=[bx], w=[ba, bs8])
                S.op("dve", lambda e: e.tensor_scalar(out=s8[:, 1:2], in0=s8[:, 0:1], scalar1=1.0 / D, scalar2=EPS,
                                                      op0=ALU.mult, op1=ALU.add), r=[bs8], w=[bs8])
                S.op("act", lambda e: e.activation(out=s8[:, 3:4], in_=s8[:, 1:2], func=AF.Sqrt), r=[bs8], w=[bs8])
                S.op("dve", lambda e: e.reciprocal(out=s8[:, 2:3], in_=s8[:, 3:4]), r=[bs8], w=[bs8])
                S.op("dve", lambda e: e.scalar_tensor_tensor(out=a_t[:], in0=x_t[:], scalar=s8[:, 2:3], in1=gmix[:],
                                                             op0=ALU.mult, op1=ALU.mult), r=[bx, bs8, bgmix], w=[ba])

        def transpose_part():
            nonlocal_nb = cntA["nb"]
            for tt in range(4):
                (a_t, ba) = abf[tt]
                for dcg in range(4):
                    pb, bpb = C.ps[nonlocal_nb % 8], C.bps[nonlocal_nb % 8]
                    nonlocal_nb += 1
                    pbb = pb[:].bitcast(BF16)
                    for j in range(8):
                        dc = dcg * 8 + j
                        S.op("pe", lambda e: e.transpose(out=pbb[:, j * 128:(j + 1) * 128],
                                                         in_=a_t[:, dc * 128:(dc + 1) * 128], identity=C.identb[:]),
                             r=[ba, C.bidentb], w=[bpb], signal=(j == 7))
                    src = pbb[:, 0:1024].rearrange("p (j t) -> p j t", j=8)
                    dst = aT[:, dcg * 8:(dcg + 1) * 8, tt * 128:(tt + 1) * 128]
                    if dcg % 2 == 0:
                        S.op("act", lambda e: e.activation(out=dst, in_=src, func=AF.Copy), r=[bpb], w=[baT])
                    else:
                        S.op("dve", lambda e: e.tensor_copy(out=dst, in_=src), r=[bpb], w=[baT])
            cntA["nb"] = nonlocal_nb

        cntA = {"nb": 0}
        norm_part(st_list[0])
        for si, st in enumerate(st_list):
            own = st >= NST
            t0 = (st - NST if own else st) * TS
            transpose_part()
            if si + 1 < len(st_list):
                norm_part(st_list[si + 1])
            nb = cntA["nb"]
            ncol = 24 if own else 8
            for ct in range(ncol):
                (w_t, bw) = wb[nw % 2]
                nw += 1
                if ct not in C.win_cached:
                    S.dma("pool", w_t[:], w_in_v[:, :, ct * 256:(ct + 1) * 256], w=[bw])
                    S.dma("sp", T["WinS"][ct], w_t[:], r=[bw])
                    C.win_cached.add(ct)
                    C.win_fresh.add(ct)
                else:
                    if ct in C.win_fresh:
                        S.barrier_dma()
                        C.win_fresh.clear()
                    S.dma("pool", w_t[:], T["WinS"][ct], w=[bw])
                if ct < 16:
                    for sub in range(2):
                        pb, bpb = C.ps[nb % 8], C.bps[nb % 8]
                        nb += 1
                        for dc in range(32):
                            S.op("pe", lambda e: e.matmul(pb[:], w_t[:, dc, sub * 128:(sub + 1) * 128], aT[:, dc, :],
                                                          start=(dc == 0), stop=(dc == 31)),
                                 r=[bw, baT], w=[bpb], signal=(dc == 31))
                        (z_t, bz) = zs[nz % 3]
                        nz += 1
                        if ct < 8:
                            S.op("act", lambda e: e.activation(out=z_t[:], in_=pb[:], func=AF.Copy), r=[bpb], w=[bz])
                            row = (ct * 2 + sub) * 128
                            S.dma("sp", T["ZT"][row:row + 128, st * TS:(st + 1) * TS], z_t[:], r=[bz])
                        else:
                            S.op("act", lambda e: e.activation(out=z_t[:], in_=pb[:], func=GELU), r=[bpb], w=[bz])
                            row = ((ct - 8) * 2 + sub) * 128
                            S.dma("sp", T["UT"][row:row + 128, t0:t0 + TS], z_t[:], r=[bz])
                else:
                    vc = ct - 16
                    for tt in range(4):
                        pb, bpb = C.ps[nb % 8], C.bps[nb % 8]
                        nb += 1
                        for dc in range(32):
                            S.op("pe", lambda e: e.matmul(pb[:, 0:256], aT[:, dc, tt * 128:(tt + 1) * 128], w_t[:, dc, :],
                                                          start=(dc == 0), stop=(dc == 31)),
                                 r=[bw, baT], w=[bpb], signal=(dc == 31))
                        S.op("act", lambda e: e.activation(out=V[tt][0][:, vc * 256:(vc + 1) * 256], in_=pb[:, 0:256],
                                                           func=GELU), r=[bpb], w=[V[tt][1]])
            cntA["nb"] = nb
            if own:
                for tt in range(4):
                    v_t, bv = V[tt]
                    (s8, bs8) = st8[nx % 2]
                    (vn_t, bvn) = vn[nx % 2]
                    nx += 1
                    S.op("dve", lambda e: e.memset(s8[:], 0.0), w=[bs8])
                    S.op("act", lambda e: e.activation(out=vn_t[:], in_=v_t[:], func=AF.Square,
                                                       accum_out=s8[:, 0:1]), r=[bv], w=[bvn, bs8])
                    S.op("dve", lambda e: e.tensor_scalar(out=junk[:, 0:2048], in0=v_t[:], scalar1=1.0, scalar2=0.0,
                                                          op0=ALU.mult, op1=ALU.add, accum_out=s8[:, 1:2]),
                         r=[bv], w=[bjunk, bs8])
                    S.op("dve", lambda e: e.tensor_scalar(out=s8[:, 2:3], in0=s8[:, 1:2], scalar1=1.0 / 2048, scalar2=None,
                                                          op0=ALU.mult), r=[bs8], w=[bs8])
                    S.op("dve", lambda e: e.tensor_tensor(out=s8[:, 3:4], in0=s8[:, 2:3], in1=s8[:, 2:3], op=ALU.mult),
                         r=[bs8], w=[bs8])
                    S.op("dve", lambda e: e.scalar_tensor_tensor(out=s8[:, 4:5], in0=s8[:, 0:1], scalar=1.0 / 2048,
                                                                 in1=s8[:, 3:4], op0=ALU.mult, op1=ALU.subtract),
                         r=[bs8], w=[bs8])
                    S.op("dve", lambda e: e.tensor_scalar(out=s8[:, 6:7], in0=s8[:, 4:5], scalar1=EPS, scalar2=None,
                                                          op0=ALU.add), r=[bs8], w=[bs8])
                    S.op("act", lambda e: e.activation(out=s8[:, 7:8], in_=s8[:, 6:7], func=AF.Sqrt), r=[bs8], w=[bs8])
                    S.op("dve", lambda e: e.reciprocal(out=s8[:, 5:6], in_=s8[:, 7:8]), r=[bs8], w=[bs8])
                    S.op("dve", lambda e: e.tensor_scalar(out=vn_t[:], in0=v_t[:], scalar1=s8[:, 2:3], scalar2=s8[:, 5:6],
                                                          op0=ALU.subtract, op1=ALU.mult), r=[bv, bs8], w=[bvn])
                    S.dma("sp", T["VN"][t0 + tt * 128:t0 + (tt + 1) * 128, :], vn_t[:], r=[bvn])
        S.barrier()


def phase_s5(C, gb_list):
    nc, S, T = C.nc, C.S, C.T
    PI = math.pi
    with contextlib.ExitStack() as ph:
        tl = lambda name, shape, dt: _tile(nc, ph, name, shape, dt)
        bset = Buf("s5setup")
        raw = lambda name, shape, dt: ph.enter_context(nc.sbuf_tensor(_uname("sr_" + name), list(shape), dt))
        so = lambda k, fn: S.op(k, fn, r=[bset], w=[bset])
        aq, iq, lq = raw("aq", [128, 128], F32), raw("iq", [128, 128], F32), raw("lq", [128, 128], F32)
        theta, mag, tq = raw("theta", [128, 128], F32), raw("mag", [128, 128], F32), raw("tq", [128, 128], F32)
        offs = raw("offs", [128, 8, 128], F32)
        S.dma("sp", aq[:], T["a_re_q"][:], w=[bset])
        S.dma("sp", iq[:], T["a_im_q"][:], w=[bset])
        S.dma("sp", lq[:], T["ldt_q"][:], w=[bset])
        so("act", lambda e: e.activation(out=lq[:], in_=lq[:], func=AF.Exp))
        so("dve", lambda e: e.tensor_tensor(out=tq[:], in0=aq[:], in1=lq[:], op=ALU.mult))
        so("act", lambda e: e.activation(out=mag[:], in_=tq[:], func=AF.Exp))
        so("dve", lambda e: e.tensor_tensor(out=theta[:], in0=iq[:], in1=lq[:], op=ALU.mult))
        kq = raw("kq", [128, 128], mybir.dt.int32)
        so("dve", lambda e: e.tensor_scalar(out=tq[:], in0=theta[:], scalar1=1.0 / TWO_PI, scalar2=None, op0=ALU.mult))
        so("dve", lambda e: e.tensor_copy(out=kq[:], in_=tq[:]))
        so("dve", lambda e: e.tensor_tensor(out=theta[:], in0=tq[:], in1=kq[:], op=ALU.subtract))
        for seg in range(8):
            so("dve", lambda e: e.tensor_scalar(out=tq[:], in0=theta[:], scalar1=float(512 * seg), scalar2=None, op0=ALU.mult))
            so("dve", lambda e: e.tensor_copy(out=kq[:], in_=tq[:]))
            so("dve", lambda e: e.tensor_tensor(out=offs[:, seg, :], in0=tq[:], in1=kq[:], op=ALU.subtract))
        hp = raw("hp", [128, 1], F32)
        BST1, BST2 = raw("BST1", [128, 16, 128], BF16), raw("BST2", [128, 16, 128], BF16)
        sg = raw("sg", [128, 1], F32)
        CST1, CST2 = raw("CST1", [128, 16, 128], BF16), raw("CST2", [128, 16, 128], BF16)
        dcol, DST = raw("dcol", [128, 16], F32), raw("DST", [128, 16, 128], BF16)
        rmask, cmask = raw("rmask", [128, 8], F32), raw("cmask", [128, 8, 128], BF16)
        iota = raw("iota512", [128, 512], F32)
        riota = raw("riota512", [128, 512], F32)
        c512, s512 = raw("c512", [128, 128], F32), raw("s512", [128, 128], F32)
        swapI = raw("swapI", [128, 128], F32)
        eq_, mag512 = raw("eq", [128, 128], F32), raw("mag512", [128, 128], F32)
        tmpst = contextlib.ExitStack()
        rawt = lambda name, shape, dt: tmpst.enter_context(nc.sbuf_tensor(_uname("st_" + name), list(shape), dt))
        names = ["arb", "aib", "ldb", "brb", "bib", "magb", "ang", "nsin", "ncos", "nr", "ni", "u1", "u2", "cr", "ci"]
        B = {n: rawt("s5" + n, [128, 1024], F32) for n in names}
        for n, src in (("arb", "a_re_b"), ("aib", "a_im_b"), ("ldb", "ldt_b"), ("brb", "b_re_b"), ("bib", "b_im_b")):
            S.dma("sp", B[n][:], T[src][:], w=[bset])
        tt_ = lambda o, a, b, op: so("dve", lambda e: e.tensor_tensor(out=B[o][:], in0=B[a][:], in1=B[b][:], op=op))
        so("act", lambda e: e.activation(out=B["ldb"][:], in_=B["ldb"][:], func=AF.Exp))
        tt_("u1", "arb", "ldb", ALU.mult)
        so("act", lambda e: e.activation(out=B["magb"][:], in_=B["u1"][:], func=AF.Exp))
        tt_("ang", "aib", "ldb", ALU.mult)
        kb = rawt("kb", [128, 1024], mybir.dt.int32)
        so("dve", lambda e: e.memset(hp[:], PI / 2))
        so("dve", lambda e: e.tensor_scalar(out=B["u1"][:], in0=B["ang"][:], scalar1=1.0 / TWO_PI, scalar2=None, op0=ALU.mult))
        so("dve", lambda e: e.tensor_copy(out=kb[:], in_=B["u1"][:]))
        so("dve", lambda e: e.tensor_tensor(out=B["u1"][:], in0=B["u1"][:], in1=kb[:], op=ALU.subtract))
        so("dve", lambda e: e.scalar_tensor_tensor(out=B["u2"][:], in0=B["u1"][:], scalar=-1.0, in1=B["u1"][:],
                                                   op0=ALU.mult, op1=ALU.max))
        so("act", lambda e: e.activation(out=B["nsin"][:], in_=B["u1"][:], func=AF.Sin, scale=TWO_PI))
        so("act", lambda e: e.activation(out=B["ncos"][:], in_=B["u2"][:], func=AF.Sin, scale=-TWO_PI, bias=hp[:, 0:1]))
        tt_("u1", "magb", "ncos", ALU.mult)
        so("dve", lambda e: e.tensor_scalar(out=B["nr"][:], in0=B["u1"][:], scalar1=-1.0, scalar2=None, op0=ALU.add))
        tt_("ni", "magb", "nsin", ALU.mult)
        tt_("u1", "arb", "arb", ALU.mult)
        tt_("u2", "aib", "aib", ALU.mult)
        tt_("u1", "u1", "u2", ALU.add)
        so("dve", lambda e: e.reciprocal(out=B["u2"][:], in_=B["u1"][:]))
        tt_("cr", "nr", "arb", ALU.mult)
        tt_("u1", "ni", "aib", ALU.mult)
        tt_("cr", "cr", "u1", ALU.add)
        tt_("cr", "cr", "u2", ALU.mult)
        tt_("ci", "ni", "arb", ALU.mult)
        tt_("u1", "nr", "aib", ALU.mult)
        tt_("ci", "ci", "u1", ALU.subtract)
        tt_("ci", "ci", "u2", ALU.mult)
        tt_("nr", "cr", "brb", ALU.mult)
        tt_("u1", "ci", "bib", ALU.mult)
        tt_("nr", "nr", "u1", ALU.subtract)
        tt_("ni", "cr", "bib", ALU.mult)
        tt_("u1", "ci", "brb", ALU.mult)
        tt_("ni", "ni", "u1", ALU.add)
        v3 = lambda n: B[n][:].rearrange("p (g q) -> p g q", g=16)
        so("dve", lambda e: e.tensor_copy(out=BST1[:, :, 0:64], in_=v3("nr")))
        so("dve", lambda e: e.tensor_copy(out=BST1[:, :, 64:128], in_=v3("ni")))
        so("dve", lambda e: e.tensor_copy(out=BST2[:, :, 0:64], in_=v3("ni")))
        so("dve", lambda e: e.tensor_scalar(out=BST2[:, :, 64:128], in0=v3("nr"), scalar1=-1.0, scalar2=None, op0=ALU.mult))
        cm1 = B["arb"]
        cmA = rawt("cmA", [128, 2048], F32)
        S.dma("sp", cmA[:], T["cmix1"][:], w=[bset])
        S.dma("sp", sg[:], T["sgn1"][:], w=[bset])
        cmB = rawt("cmB", [128, 2048], F32)
        S.dma("sp", cmB[:], T["cmix2"][:], w=[bset])
        so("dve", lambda e: e.tensor_scalar(out=CST2[:].rearrange("p g q -> p (g q)"), in0=cmB[:], scalar1=-1.0,
                                            scalar2=None, op0=ALU.mult))
        so("dve", lambda e: e.tensor_scalar(out=CST1[:].rearrange("p g q -> p (g q)"), in0=cmA[:], scalar1=sg[:, 0:1],
                                            scalar2=None, op0=ALU.mult))
        S.dma("sp", dcol[:], T["d_col"][:], w=[bset])
        for gb in range(16):
            so("dve", lambda e: e.tensor_scalar(out=DST[:, gb, :], in0=C.identf[:], scalar1=dcol[:, gb:gb + 1],
                                                scalar2=None, op0=ALU.mult))
        S.dma("sp", rmask[:], T["rowmask"][:], w=[bset])
        S.dma("pool", cmask[:], T["colmask"][:], w=[bset])
        S.dma("sp", iota[:], T["iota512"][:], w=[bset])

        S.dma("sp", swapI[:], T["swapI"][:], w=[bset])
        S.dma("sp", riota[:], T["riota512"][:], w=[bset])
        so("dve", lambda e: e.tensor_tensor(out=eq_[:], in0=aq[:], in1=lq[:], op=ALU.mult))
        so("act", lambda e: e.activation(out=mag512[:], in_=eq_[:], func=AF.Exp, scale=512.0))
        so("dve", lambda e: e.tensor_scalar(out=tq[:], in0=theta[:], scalar1=512.0, scalar2=None, op0=ALU.mult))
        so("dve", lambda e: e.tensor_copy(out=kq[:], in_=tq[:]))
        so("dve", lambda e: e.tensor_tensor(out=tq[:], in0=tq[:], in1=kq[:], op=ALU.subtract))
        so("dve", lambda e: e.scalar_tensor_tensor(out=c512[:], in0=tq[:], scalar=-1.0, in1=tq[:], op0=ALU.mult, op1=ALU.max))
        so("act", lambda e: e.activation(out=s512[:], in_=tq[:], func=AF.Sin, scale=TWO_PI))
        so("act", lambda e: e.activation(out=c512[:], in_=c512[:], func=AF.Sin, scale=-TWO_PI, bias=hp[:, 0:1]))
        so("dve", lambda e: e.tensor_scalar(out=s512[:], in0=s512[:], scalar1=sg[:, 0:1], scalar2=None, op0=ALU.mult))

        S.barrier()
        tmpst.close()
        NSET = 4
        zt = [tl(f"zt{i}", [128, 4096], BF16) for i in range(2)]
        bm = [[tl(f"bm{k}{i}", [128, 128], BF16) for i in range(4)] for k in range(NSET)]
        ROT = [tl(f"ROT{k}", [128, 128], F32) for k in range(NSET)]
        rtm = [tl(f"rtm{k}", [128, 128], F32) for k in range(2)]
        SNt = [tl(f"SN{k}", [128, 512], F32) for k in range(NSET)]
        CSt = [tl(f"CS{k}", [128, 512], F32) for k in range(NSET)]
        kph = [tl(f"kph{k}", [128, 512], mybir.dt.int32) for k in range(2)]
        t1 = [tl(f"t1{k}", [128, 512], F32) for k in range(4)]
        t2 = [tl(f"t2{k}", [128, 512], F32) for k in range(4)]
        vv = [tl(f"vv{k}", [128, 512], F32) for k in range(4)]
        q1 = [tl(f"q1{k}", [128, 512], BF16) for k in range(4)]
        q2 = [tl(f"q2{k}", [128, 512], BF16) for k in range(4)]
        ini = [tl(f"ini{k}", [128, 1], F32) for k in range(4)]
        accs = [tl(f"accs{k}", [128, 4], F32) for k in range(4)]
        CSb = [tl(f"CSb{k}", [128, 512], BF16) for k in range(NSET)]
        SNb = [tl(f"SNb{k}", [128, 512], BF16) for k in range(NSET)]
        vb = [tl(f"vb{k}", [128, 512], BF16) for k in range(4)]
        junkS = [tl(f"junkS{k}", [128, 512], F32) for k in range(2)]
        DEC = [tl(f"DEC{k}", [128, 512], F32) for k in range(2)]
        MCS = [tl(f"MCS{k}", [128, 512], F32) for k in range(NSET)]
        MSN = [tl(f"MSN{k}", [128, 512], F32) for k in range(NSET)]
        conv_jobs = []
        wout_v = T["w_out"].rearrange("(c p) n -> p c n", p=128)
        wq_v = T["w_q"].rearrange("(c p) n -> p c n", p=128)
        dn_v = T["downT"].rearrange("(c p) e -> p c e", p=128)
        up_v = T["up"].rearrange("(c p) d -> p c d", p=128)
        for ds in range(8):
            conv_jobs.append((T["WoS"][ds], wout_v[:, :, ds * 512:(ds + 1) * 512]))
        for qc in range(8):
            conv_jobs.append((T["WqS"][qc], wq_v[:, :, qc * 256:(qc + 1) * 256]))
        for cg in range(16):
            for cp in range(4):
                i_ = cg * 4 + cp
                conv_jobs.append((T["DnS"][i_], dn_v[:, :, i_ * 256:(i_ + 1) * 256]))
            for ds in range(8):
                conv_jobs.append((T["UpS"][cg * 8 + ds], up_v[:, cg * 8:(cg + 1) * 8, ds * 512:(ds + 1) * 512]))
        ysb = [tl(f"ysb{k}", [128, 512], BF16) for k in range(2)]
        P1, bP1, P2, bP2, RB, bRB = C.ps[0], C.bps[0], C.ps[1], C.bps[1], C.ps[2], C.bps[2]
        cnt = {"prep": 0, "it": 0, "y": 0}

        def prep_group(g):
            k = g % NSET
            gb, g8 = divmod(g, 8)
            S.op("act", lambda e: e.activation(out=bm[k][0][0][:], in_=BST1[:, gb, :], func=AF.Copy, scale=rmask[:, g8:g8 + 1]),
                 r=[bset], w=[bm[k][0][1]])
            S.op("act", lambda e: e.activation(out=bm[k][1][0][:], in_=BST2[:, gb, :], func=AF.Copy, scale=rmask[:, g8:g8 + 1]),
                 r=[bset], w=[bm[k][1][1]])
            S.op("dve", lambda e: e.tensor_tensor(out=bm[k][2][0][:], in0=CST1[:, gb, :], in1=cmask[:, g8, :],
                                                  op=ALU.mult), r=[bset], w=[bm[k][2][1]])
            S.op("dve", lambda e: e.tensor_tensor(out=bm[k][3][0][:], in0=CST2[:, gb, :], in1=cmask[:, g8, :],
                                                  op=ALU.mult), r=[bset], w=[bm[k][3][1]])
            (rt, brt), (rm_, brm) = ROT[k], rtm[cnt["prep"] % 2]
            S.op("act", lambda e: e.activation(out=rt[:], in_=C.identf[:], func=AF.Copy, scale=c512[:, g:g + 1]),
                 r=[bset, C.bidentf], w=[brt])
            S.op("act", lambda e: e.activation(out=rm_[:], in_=swapI[:], func=AF.Copy, scale=s512[:, g:g + 1]), r=[bset], w=[brm])
            S.op("dve", lambda e: e.tensor_tensor(out=rt[:], in0=rt[:], in1=rm_[:], op=ALU.add), r=[brt, brm], w=[brt])
            (sn, bsn), (cs, bcs), (ki_, bki) = SNt[k], CSt[k], kph[cnt["prep"] % 2]
            cnt["prep"] += 1
            S.op("act", lambda e: e.activation(out=sn[:], in_=iota[:], func=AF.Copy, scale=theta[:, g:g + 1]), r=[bset], w=[bsn])
            S.op("dve", lambda e: e.tensor_copy(out=ki_[:], in_=sn[:]), r=[bsn], w=[bki])
            S.op("dve", lambda e: e.tensor_tensor(out=sn[:], in0=sn[:], in1=ki_[:], op=ALU.subtract), r=[bki, bsn], w=[bsn])
            S.op("dve", lambda e: e.scalar_tensor_tensor(out=cs[:], in0=sn[:], scalar=-1.0, in1=sn[:], op0=ALU.mult, op1=ALU.max),
                 r=[bsn], w=[bcs])
            S.op("act", lambda e: e.activation(out=sn[:], in_=sn[:], func=AF.Sin, scale=TWO_PI), r=[bsn], w=[bsn])
            S.op("act", lambda e: e.activation(out=cs[:], in_=cs[:], func=AF.Sin, scale=-TWO_PI, bias=hp[:, 0:1]), r=[bcs, bset], w=[bcs])
            S.op("act", lambda e: e.activation(out=CSb[k][0][:], in_=cs[:], func=AF.Copy), r=[bcs], w=[CSb[k][1]])
            S.op("act", lambda e: e.activation(out=SNb[k][0][:], in_=sn[:], func=AF.Copy), r=[bsn], w=[SNb[k][1]])
            (dc_, bdc) = DEC[cnt["prep"] % 2]
            S.op("act", lambda e: e.activation(out=dc_[:], in_=riota[:], func=AF.Exp, scale=eq_[:, g:g + 1]), r=[bset], w=[bdc])
            S.op("dve", lambda e: e.tensor_tensor(out=MCS[k][0][:], in0=dc_[:], in1=cs[:], op=ALU.mult), r=[bdc, bcs], w=[MCS[k][1]])
            S.op("dve", lambda e: e.tensor_tensor(out=MSN[k][0][:], in0=dc_[:], in1=sn[:], op=ALU.mult), r=[bdc, bsn], w=[MSN[k][1]])

        def stage_a(itn, g, seg, z_t, bz):
            k, ix = g % NSET, itn % 4
            zseg = z_t[:, seg * 512:(seg + 1) * 512]
            S.op("pe", lambda e: e.matmul(P1[:], bm[k][0][0][:], zseg, start=True, stop=True), r=[bm[k][0][1], bz], w=[bP1])
            S.op("pe", lambda e: e.matmul(P2[:], bm[k][1][0][:], zseg, start=True, stop=True), r=[bm[k][1][1], bz], w=[bP2])
            if seg < 4:
                (ac, bac), (jk, bjk) = accs[ix], junkS[itn % 2]
                S.op("dve", lambda e: e.memset(ac[:, 0:2], 0.0), w=[bac])
                S.op("dve", lambda e: e.scalar_tensor_tensor(out=jk[:], in0=P1[:], scalar=1.0, in1=MCS[k][0][:], op0=ALU.mult, op1=ALU.mult,
                                                             accum_out=ac[:, 0:1]), r=[bP1, MCS[k][1]], w=[bjk, bac])
                S.op("dve", lambda e: e.scalar_tensor_tensor(out=jk[:], in0=P2[:], scalar=1.0, in1=MSN[k][0][:], op0=ALU.mult, op1=ALU.mult,
                                                             accum_out=ac[:, 1:2]), r=[bP2, MSN[k][1]], w=[bjk, bac])
                return
            (a1, ba1), (a2, ba2) = t1[ix], t2[ix]
            S.op("dve", lambda e: e.tensor_tensor(out=a1[:], in0=P1[:], in1=CSt[k][0][:], op=ALU.mult), r=[bP1, CSt[k][1]], w=[ba1])
            S.op("dve", lambda e: e.tensor_tensor(out=a2[:], in0=P2[:], in1=SNt[k][0][:], op=ALU.mult), r=[bP2, SNt[k][1]], w=[ba2])
            S.op("dve", lambda e: e.tensor_tensor(out=a1[:], in0=a1[:], in1=a2[:], op=ALU.add), r=[ba1, ba2], w=[ba1])

        def stage_b(itn, g, seg, g8):
            k, ix = g % NSET, itn % 4
            if seg < 4:
                (ac, bac) = accs[ix]
                S.op("dve", lambda e: e.tensor_tensor(out=ac[:, 2:3], in0=ac[:, 0:1], in1=ac[:, 1:2], op=ALU.add), r=[bac], w=[bac])
                if seg > 0:
                    S.op("dve", lambda e: e.scalar_tensor_tensor(out=ac[:, 3:4], in0=ini[ix][0][:, 0:1], scalar=mag512[:, g:g + 1],
                                                                 in1=ac[:, 2:3], op0=ALU.mult, op1=ALU.add), r=[bac, ini[ix][1], bset], w=[bac])
                    vend = ac[:, 3:4]
                else:
                    vend = ac[:, 2:3]
                col = itn % 8
                S.op("pe", lambda e: e.matmul(RB[:, col:col + 1], ROT[k][0][:], vend, start=True, stop=True), r=[ROT[k][1], bac], w=[bRB])
                nx_ = ini[(itn + 2) % 4]
                S.op("act", lambda e: e.activation(out=nx_[0][:], in_=RB[:, col:col + 1], func=AF.Copy), r=[bRB], w=[nx_[1]])
                return
            (a1, ba1), (v_t, bv) = t1[ix], vv[ix]
            if seg == 0:
                init, rr = 0.0, [ba1, bset]
            else:
                init, rr = ini[ix][0][:, 0:1], [ba1, bset, ini[ix][1]]
            S.op("dve", lambda e: e.tensor_tensor_scan(out=v_t[:], data0=mag[:, g:g + 1].broadcast_to([128, 512]), data1=a1[:],
                                                       initial=init, op0=ALU.mult, op1=ALU.add), r=rr, w=[bv])
            if seg < 7:
                col = itn % 8
                S.op("pe", lambda e: e.matmul(RB[:, col:col + 1], ROT[k][0][:], v_t[:, 511:512], start=True, stop=True),
                     r=[ROT[k][1], bv], w=[bRB])
                nx_ = ini[(itn + 2) % 4]
                S.op("act", lambda e: e.activation(out=nx_[0][:], in_=RB[:, col:col + 1], func=AF.Copy), r=[bRB], w=[nx_[1]])
            if seg >= 4:
                (x1, bx1), (x2, bx2) = q1[ix], q2[ix]
                (vb_, bvb) = vb[ix]
                S.op("act", lambda e: e.activation(out=vb_[:], in_=v_t[:], func=AF.Copy), r=[bv], w=[bvb])
                S.op("dve", lambda e: e.tensor_tensor(out=x1[:], in0=vb_[:], in1=CSb[k][0][:], op=ALU.mult), r=[bvb, CSb[k][1]], w=[bx1])
                S.op("dve", lambda e: e.tensor_tensor(out=x2[:], in0=vb_[:], in1=SNb[k][0][:], op=ALU.mult), r=[bvb, SNb[k][1]], w=[bx2])

        def stage_c(itn, g, seg, g8):
            k, ix = g % NSET, itn % 4
            if seg >= 4:
                (x1, bx1), (x2, bx2) = q1[ix], q2[ix]
                Y, bY = C.ps[4 + seg - 4], C.bps[4 + seg - 4]
                S.op("pe", lambda e: e.matmul(Y[:], bm[k][2][0][:], x1[:], start=False, stop=False), r=[bm[k][2][1], bx1], w=[bY])
                S.op("pe", lambda e: e.matmul(Y[:], bm[k][3][0][:], x2[:], start=False, stop=(g8 == 7)), r=[bm[k][3][1], bx2], w=[bY])

        prep_group(gb_list[0] * 8)
        prep_group(gb_list[0] * 8 + 1)
        S.dma("sp", zt[0][0][:], T["ZT"][gb_list[0] * 128:(gb_list[0] + 1) * 128, :], w=[zt[0][1]])
        for gi, gb in enumerate(gb_list):
            z_t, bz = zt[gi % 2]
            if gi + 1 < len(gb_list):
                nz_t, nbz = zt[(gi + 1) % 2]
                ngb = gb_list[gi + 1]
                S.dma("sp", nz_t[:], T["ZT"][ngb * 128:(ngb + 1) * 128, :], w=[nbz])
            for so_ in range(4):
                S.op("pe", lambda e: e.matmul(C.ps[4 + so_][:], DST[:, gb, :], z_t[:, 2048 + so_ * 512:2048 + (so_ + 1) * 512],
                                              start=True, stop=False), r=[bset, bz], w=[C.bps[4 + so_]])
            prev = None
            prev2 = None
            for pr in range(4):
                pair = (gb * 8 + pr * 2, gb * 8 + pr * 2 + 1)
                for seg in range(8):
                    for g in pair:
                        itn = cnt["it"]
                        cnt["it"] += 1
                        stage_a(itn, g, seg, z_t, bz)
                        if prev is not None:
                            stage_b(*prev)
                        if prev2 is not None:
                            stage_c(*prev2)
                        prev2 = prev
                        prev = (itn, g, seg, g % 8)
                    if seg == 3:
                        if pr < 3:
                            nxt = (pair[0] + 2, pair[1] + 2)
                        elif gi + 1 < len(gb_list):
                            nxt = (gb_list[gi + 1] * 8, gb_list[gi + 1] * 8 + 1)
                        else:
                            nxt = ()
                        for g_ in nxt:
                            prep_group(g_)
                    if CONV_INTERLEAVE and seg in (1, 3, 5, 7) and conv_jobs:
                        o_ap, i_ap = conv_jobs.pop(0)
                        S.dma("pool", o_ap, i_ap)
            stage_b(*prev)
            stage_c(*prev2)
            stage_c(*prev)
            for so_ in range(4):
                y_t, by = ysb[cnt["y"] % 2]
                cnt["y"] += 1
                S.op("act", lambda e: e.activation(out=y_t[:], in_=C.ps[4 + so_][:], func=GELU), r=[C.bps[4 + so_]], w=[by])
                S.dma("sp", T["YG"][gb * 128:(gb + 1) * 128, so_ * 512:(so_ + 1) * 512], y_t[:], r=[by])
        while conv_jobs:
            o_ap, i_ap = conv_jobs.pop(0)
            S.dma("pool", o_ap, i_ap)
        S.barrier()


def rstd_ops(S, src_ap, dst, bdst, tmp, n, scale, rsrc):
    S.op("dve", lambda e: e.tensor_scalar(out=tmp[:, 0:n], in0=src_ap, scalar1=scale, scalar2=EPS, op0=ALU.mult, op1=ALU.add),
         r=list(rsrc) + [bdst], w=[bdst])
    S.op("act", lambda e: e.activation(out=tmp[:, n:2 * n], in_=tmp[:, 0:n], func=AF.Sqrt), r=[bdst], w=[bdst])
    S.op("dve", lambda e: e.reciprocal(out=dst, in_=tmp[:, n:2 * n]), r=[bdst], w=[bdst])


def phase_b(C, st_list, stop_after=None):
    nc, S, T = C.nc, C.S, C.T
    with contextlib.ExitStack() as pbs:
        raw = lambda name, shape, dt: pbs.enter_context(nc.sbuf_tensor(_uname("sr_" + name), list(shape), dt))
        bK = Buf("bconst")
        so = lambda k, fn: S.op(k, fn, r=[bK], w=[bK])
        wmT = raw("wmT", [128, 8, 128], BF16)
        bsr = raw("bsr", [128, 8, 128], F32)
        BIAS = raw("BIAS", [128, 16, 128], F32)
        cols = raw("cols", [128, 4, 16], F32)
        ones = raw("ones", [128, 128], BF16)
        S.dma("pool", wmT[:], T["wmT"][:], w=[bK])
        S.dma("sp", bsr[:], T["bs_rep"][:], w=[bK])
        S.dma("sp", cols[:], T["gm_cols"][:], w=[bK])
        so("dve", lambda e: e.memset(ones[:], 1.0))
        so("dve", lambda e: e.memset(wmT[64:128, :, 0:64], 0.0))
        for hh in range(8):
            bank = C.ps[hh // 4]
            S.op("pe", lambda e: e.matmul(bank[:, (hh % 4) * 128:(hh % 4 + 1) * 128], ones[:], wmT[:, hh, :], start=True, stop=True),
                 r=[bK], w=[C.bps[hh // 4]])
        for ct in range(16):
            hh = ct // 2
            bank = C.ps[hh // 4]
            S.op("dve", lambda e: e.scalar_tensor_tensor(out=BIAS[:, ct, :], in0=bank[:, (hh % 4) * 128:(hh % 4 + 1) * 128],
                                                         scalar=cols[:, 1, ct:ct + 1], in1=bsr[:, hh, :], op0=ALU.mult, op1=ALU.add),
                 r=[bK, C.bps[hh // 4]], w=[bK])
        hT = [_tile(nc, pbs, f"h{i}", [128, D], F32) for i in range(4)]
        rstd, brstd = _tile(nc, pbs, "rstdAB", [128, 8], F32)
        rtmp = raw("rtmp", [128, 16], F32)
        rtmp2 = raw("rtmp2", [128, 16], F32)
        wglu_v = T["w_glu"].rearrange("(c p) n -> p c n", p=128)
        wout_v = T["w_out"].rearrange("(c p) n -> p c n", p=128)
        nb = 0
        for st in st_list:
            t0 = st * TS
            with contextlib.ExitStack() as b1:
                tl = lambda name, shape, dt: _tile(nc, b1, name, shape, dt)
                yT, byT = _tile(nc, b1, "yT", [128, 32, TS], BF16)
                with contextlib.ExitStack() as b1a:
                    tla = lambda name, shape, dt: _tile(nc, b1a, name, shape, dt)
                    ygT, bygT = tla("ygT", [128, 16, TS], BF16)
                    uT, buT = tla("uT", [128, 16, TS], BF16)
                    wg = [tla(f"wg{i}", [128, 16, 256], BF16) for i in range(2)]
                    sig = [tla(f"sig{i}", [128, TS], F32) for i in range(3)]
                    ypre = [tla(f"ypre{i}", [128, TS], BF16) for i in range(3)]
                    ysq = [tla(f"ysq{i}", [128, TS], BF16) for i in range(3)]
                    vnt = [tla(f"vnt{i}", [128, 2048], BF16) for i in range(2)]
                    S.dma("sp", ygT[:], T["YG"].rearrange("(c p) t -> p c t", p=128)[:, :, t0:t0 + TS], w=[bygT])
                    S.dma("sp", uT[:], T["UT"].rearrange("(c p) t -> p c t", p=128)[:, :, t0:t0 + TS], w=[buT])
                    SSQ, bSSQ = C.ps[7], C.bps[7]
                    n2 = 0
                    deferred = []
                    for oc2 in range(8):
                        (w_t, bw) = wg[oc2 % 2]
                        S.dma("pool", w_t[:], wglu_v[:, :, oc2 * 256:(oc2 + 1) * 256], w=[bw])
                        for sub in range(2):
                            oc = oc2 * 2 + sub
                            pb_, bpb = C.ps[nb % 6], C.bps[nb % 6]
                            nb += 1
                            for ci in range(16):
                                S.op("pe", lambda e: e.matmul(pb_[:], w_t[:, ci, sub * 128:(sub + 1) * 128], ygT[:, ci, :],
                                                              start=(ci == 0), stop=(ci == 15)), r=[bw, bygT], w=[bpb], signal=(ci == 15))
                            while len(deferred) > 1:
                                deferred.pop(0)()
                            (sg_, bsg), (yp, byp), (yq, byq) = sig[n2 % 3], ypre[n2 % 3], ysq[n2 % 3]
                            n2 += 1
                            S.op("act", lambda e: e.activation(out=sg_[:], in_=pb_[:], func=AF.Sigmoid), r=[bpb], w=[bsg])
                            S.op("dve", lambda e: e.tensor_tensor(out=yp[:], in0=ygT[:, oc, :], in1=sg_[:], op=ALU.mult), r=[bygT, bsg], w=[byp])
                            S.op("act", lambda e: e.activation(out=yq[:], in_=yp[:], func=AF.Square), r=[byp], w=[byq])
                            S.op("dve", lambda e: e.tensor_scalar(out=yT[:, oc, :], in0=yp[:], scalar1=cols[:, 2, oc:oc + 1], scalar2=None,
                                                                  op0=ALU.mult), r=[byp, bK], w=[byT])
                            def ssq_a(oc=oc, yq=yq, byq=byq):
                                for tt in range(4):
                                    S.op("pe", lambda e: e.matmul(SSQ[:, oc * 4 + tt:oc * 4 + tt + 1], yq[:, tt * 128:(tt + 1) * 128],
                                                                  ones[:, 0:1], start=True, stop=True), r=[byq, bK], w=[bSSQ])
                            deferred.append(ssq_a)
                    for tt in range(4):
                        (v_t, bv) = vnt[tt % 2]
                        S.dma("sp", v_t[:], T["VN"][t0 + tt * 128:t0 + (tt + 1) * 128, :], w=[bv])
                        for cq in range(4):
                            pb_, bpb = C.ps[nb % 6], C.bps[nb % 6]
                            nb += 1
                            for j in range(4):
                                ct = cq * 4 + j
                                S.op("pe", lambda e: e.matmul(pb_[:, j * 128:(j + 1) * 128], v_t[:, ct * 128:(ct + 1) * 128], wmT[:, ct // 2, :],
                                                              start=True, stop=True), r=[bv, bK], w=[bpb], signal=(j == 3))
                            while len(deferred) > 1:
                                deferred.pop(0)()
                            (sg_, bsg), (yp, byp), (yq, byq) = sig[n2 % 3], ypre[n2 % 3], ysq[n2 % 3]
                            n2 += 1
                            for j in range(4):
                                ct = cq * 4 + j
                                S.op("dve", lambda e: e.scalar_tensor_tensor(out=sg_[:, j * 128:(j + 1) * 128], in0=pb_[:, j * 128:(j + 1) * 128],
                                                                             scalar=cols[:, 0, ct:ct + 1], in1=BIAS[:, ct, :],
                                                                             op0=ALU.mult, op1=ALU.add), r=[bpb, bK], w=[bsg])
                            S.op("dve", lambda e: e.tensor_tensor(out=yp[:].rearrange("p (j t) -> p j t", j=4),
                                                                  in0=sg_[:].rearrange("p (j t) -> p j t", j=4),
                                                                  in1=uT[:, cq * 4:(cq + 1) * 4, tt * 128:(tt + 1) * 128], op=ALU.mult),
                                 r=[bsg, buT], w=[byp])
                            S.op("act", lambda e: e.activation(out=yq[:], in_=yp[:], func=AF.Square), r=[byp], w=[byq])
                            for j in range(4):
                                ct = cq * 4 + j
                                S.op("act", lambda e: e.activation(out=yT[:, 16 + ct, tt * 128:(tt + 1) * 128], in_=yp[:, j * 128:(j + 1) * 128],
                                                                   func=AF.Copy, scale=cols[:, 3, ct:ct + 1]), r=[byp, bK], w=[byT])

                            def ssq_b(cq=cq, tt=tt, yq=yq, byq=byq):
                                for j in range(4):
                                    ct = cq * 4 + j
                                    S.op("pe", lambda e: e.matmul(SSQ[:, 64 + ct * 4 + tt:64 + ct * 4 + tt + 1], yq[:, j * 128:(j + 1) * 128],
                                                                  ones[:, 0:1], start=True, stop=True), r=[byq, bK], w=[bSSQ])
                            deferred.append(ssq_b)
                    while deferred:
                        deferred.pop(0)()
                    S.op("dve", lambda e: e.reduce_sum(out=rtmp[:, 8:16].rearrange("p (a t) -> p a t", a=2),
                                                       in_=SSQ[:, 0:128].rearrange("p (a o t) -> p a t o", a=2, t=4),
                                                       axis=AX.X), r=[bSSQ, brstd], w=[brstd])
                    rstd_ops(S, rtmp[:, 8:16], rstd[:], brstd, rtmp2, 8, 1.0 / 2048, [])
                    if "YTd" in T:
                        S.dma("sp", T["YTd"].rearrange("(c p) t -> p c t", p=128), yT[:], r=[byT], is_output=True)
                        S.dma("sp", T["RSd"][:], rstd[:], r=[brstd], is_output=True)
                    S.barrier()
                with contextlib.ExitStack() as b2:
                    tlb = lambda name, shape, dt: _tile(nc, b2, name, shape, dt)
                    wo = [tlb(f"wo{i}", [128, 32, 512], BF16) for i in range(2)]
                    xs = [tlb(f"xs{i}", [128, 512], F32) for i in range(2)]
                    tm = [tlb(f"tm{i}", [128, 512], F32) for i in range(2)]
                    n3 = 0
                    for ds in range(8):
                        (w_t, bw) = wo[ds % 2]
                        S.dma("pool", w_t[:], T["WoS"][ds], w=[bw])
                        for tt in range(4):
                            PA, bPA = C.ps[nb % 8], C.bps[nb % 8]
                            PB, bPB = C.ps[(nb + 1) % 8], C.bps[(nb + 1) % 8]
                            nb += 2
                            for ci in range(16):
                                S.op("pe", lambda e: e.matmul(PA[:], yT[:, ci, tt * 128:(tt + 1) * 128], w_t[:, ci, :],
                                                              start=(ci == 0), stop=(ci == 15)), r=[byT, bw], w=[bPA], signal=(ci == 15))
                            for ci in range(16, 32):
                                S.op("pe", lambda e: e.matmul(PB[:], yT[:, ci, tt * 128:(tt + 1) * 128], w_t[:, ci, :],
                                                              start=(ci == 16), stop=(ci == 31)), r=[byT, bw], w=[bPB], signal=(ci == 31))
                            (x_t, bx), (t_t, bt) = xs[n3 % 2], tm[n3 % 2]
                            n3 += 1
                            S.dma("sp", x_t[:], T["x_own"][t0 + tt * 128:t0 + (tt + 1) * 128, ds * 512:(ds + 1) * 512], w=[bx])
                            S.op("dve", lambda e: e.scalar_tensor_tensor(out=t_t[:], in0=PA[:], scalar=rstd[:, tt:tt + 1], in1=x_t[:],
                                                                         op0=ALU.mult, op1=ALU.add), r=[bPA, brstd, bx], w=[bt])
                            S.op("dve", lambda e: e.scalar_tensor_tensor(out=hT[tt][0][:, ds * 512:(ds + 1) * 512], in0=PB[:],
                                                                         scalar=rstd[:, 4 + tt:5 + tt], in1=t_t[:],
                                                                         op0=ALU.mult, op1=ALU.add), r=[bPB, brstd, bt], w=[hT[tt][1]])
                    S.barrier()
            if stop_after == "B2":
                for tt in range(4):
                    S.dma("sp", T["out"][t0 + tt * 128:t0 + (tt + 1) * 128, :], hT[tt][0][:], r=[hT[tt][1]], is_output=True)
                S.barrier()
                continue
            with contextlib.ExitStack() as pk:
                xnT, bxnT = _tile(nc, pk, "xnT", [128, 32, TS], BF16)
                with contextlib.ExitStack() as b34:
                    qT, bqT = _tile(nc, b34, "qT", [128, 16, TS], BF16)
                    with contextlib.ExitStack() as b3:
                        tl = lambda name, shape, dt: _tile(nc, b3, name, shape, dt)
                        gffn, bgffn = tl("gffn", [128, D], F32)
                        S.dma("sp", gffn[:], T["gffn_r"][:], w=[bgffn])
                        xn = [tl(f"xn{i}", [128, D], BF16) for i in range(2)]
                        junk, bjunk = tl("junkB", [128, D], BF16)
                        s8 = [tl(f"s8b{i}", [128, 8], F32) for i in range(2)]
                        wq = [tl(f"wq{i}", [128, 32, 256], BF16) for i in range(2)]
                        for tt in range(4):
                            (x_t, bx), (s_, bs_) = xn[tt % 2], s8[tt % 2]
                            h_t, bh = hT[tt]
                            S.op("dve", lambda e: e.memset(s_[:], 0.0), w=[bs_])
                            S.op("act", lambda e: e.activation(out=junk[:], in_=h_t[:], func=AF.Square, accum_out=s_[:, 0:1]),
                                 r=[bh], w=[bjunk, bs_])
                            rstd_ops(S, s_[:, 0:1], s_[:, 1:2], bs_, s_[:, 2:4], 1, 1.0 / D, [])
                            S.op("dve", lambda e: e.scalar_tensor_tensor(out=x_t[:], in0=h_t[:], scalar=s_[:, 1:2], in1=gffn[:],
                                                                         op0=ALU.mult, op1=ALU.mult), r=[bh, bs_, bgffn], w=[bx])
                            for dcg in range(4):
                                pb_, bpb = C.ps[nb % 8], C.bps[nb % 8]
                                nb += 1
                                pbb = pb_[:].bitcast(BF16)
                                for j in range(8):
                                    dc = dcg * 8 + j
                                    S.op("pe", lambda e: e.transpose(out=pbb[:, j * 128:(j + 1) * 128], in_=x_t[:, dc * 128:(dc + 1) * 128],
                                                                     identity=C.identb[:]), r=[bx, C.bidentb], w=[bpb], signal=(j == 7))
                                src = pbb[:, 0:1024].rearrange("p (j t) -> p j t", j=8)
                                dst = xnT[:, dcg * 8:(dcg + 1) * 8, tt * 128:(tt + 1) * 128]
                                if dcg % 2 == 0:
                                    S.op("act", lambda e: e.activation(out=dst, in_=src, func=AF.Copy), r=[bpb], w=[bxnT])
                                else:
                                    S.op("dve", lambda e: e.tensor_copy(out=dst, in_=src), r=[bpb], w=[bxnT])
                        wq_v = T["w_q"].rearrange("(c p) n -> p c n", p=128)
                        for qc in range(8):
                            (w_t, bw) = wq[qc % 2]
                            S.dma("pool", w_t[:], T["WqS"][qc], w=[bw])
                            for sub in range(2):
                                pb_, bpb = C.ps[nb % 8], C.bps[nb % 8]
                                nb += 1
                                for dc in range(32):
                                    S.op("pe", lambda e: e.matmul(pb_[:], w_t[:, dc, sub * 128:(sub + 1) * 128], xnT[:, dc, :],
                                                                  start=(dc == 0), stop=(dc == 31)), r=[bw, bxnT], w=[bpb], signal=(dc == 31))
                                S.op("act", lambda e: e.activation(out=qT[:, qc * 2 + sub, :], in_=pb_[:], func=AF.Copy), r=[bpb], w=[bqT])
                        if "Qd" in T:
                            S.dma("sp", T["Qd"].rearrange("(c p) t -> p c t", p=128), qT[:], r=[bqT], is_output=True)
                        S.barrier()
                    with contextlib.ExitStack() as b4:
                        raw4 = lambda name, shape, dt: b4.enter_context(nc.sbuf_tensor(_uname("sr_" + name), list(shape), dt))
                        bR = Buf("route")
                        ro = lambda k, fn, extra_r=(), extra_w=(): S.op(k, fn, r=[bR] + list(extra_r), w=[bR] + list(extra_w))
                        S1 = [raw4(f"S{a}sb", [128, 8, 128], F32) for a in range(2)]
                        Vv = [raw4(f"V{a}", [128, 8, 16], F32) for a in range(2)]
                        Iu = [raw4(f"I{a}u", [128, 8, 16], U32) for a in range(2)]
                        If_ = [raw4(f"I{a}f", [128, 8, 16], F32) for a in range(2)]
                        tmp16 = raw4("tmp16", [128, 16, 128], F32)
                        tmpc = raw4("tmpc", [128, 8, 256], F32)
                        bAH = [[Buf(f"ah{a}{h}") for h in range(8)] for a in range(2)]
                        bH = [Buf(f"h{h}") for h in range(8)]
                        cand = raw4("cand", [128, 8, 256], F32)
                        SC = raw4("SC", [128, 8, 16], F32)
                        CIu = raw4("CIu", [128, 8, 16], U32)
                        CIf = raw4("CIf", [128, 8, 16], F32)
                        ii = raw4("ii", [128, 8, 16], mybir.dt.int32)
                        irf = raw4("irf", [128, 8, 16], F32)
                        jrf = raw4("jrf", [128, 8, 16], F32)
                        ex = raw4("ex", [128, 8, 16], F32)
                        zz = raw4("zz", [128, 16], F32)
                        gate = raw4("gate", [128, 8, 16], F32)
                        e12 = [raw4(f"e{a}r", [128, 8, 16], F32) for a in range(2)]
                        tr3 = raw4("tr3", [128, 3, 128], F32)
                        At = [_tile(nc, b4, f"At{i}", [128, 128], BF16) for i in range(4)]
                        Bt = [_tile(nc, b4, f"Bt{i}", [128, 128], BF16) for i in range(4)]
                        GTt = [_tile(nc, b4, f"GTt{i}", [128, 128, 128], BF16) for i in range(1)]
                        io16 = C.iota128[:, 0:16]
                        for tt in range(4):
                            tsl = slice(tt * 128, (tt + 1) * 128)
                            for hh in range(8):
                                for a in range(2):
                                    bk = a * 2 + hh // 4
                                    S.op("pe", lambda e: e.matmul(C.ps[bk][:, (hh % 4) * 128:(hh % 4 + 1) * 128], qT[:, 2 * hh + a, tsl],
                                                                  C.kT[a][:], start=True, stop=True), r=[bqT, C.bkT], w=[C.bps[bk]])
                            for a in range(2):
                                for hb in range(2):
                                    ro("act", lambda e: e.activation(out=S1[a][:, hb * 4:(hb + 1) * 4, :],
                                                                     in_=C.ps[a * 2 + hb][:].rearrange("p (h n) -> p h n", h=4), func=AF.Copy),
                                       extra_r=[C.bps[a * 2 + hb]])
                            ah = [(a, hh) for a in range(2) for hh in range(8)]
                            for (a, hh) in ah:
                                S.op("dve", lambda e: e.max(out=Vv[a][:, hh, 0:8], in_=S1[a][:, hh, :]), r=[bR], w=[bAH[a][hh]])
                            for (a, hh) in ah:
                                S.op("dve", lambda e: e.max_index(out=Iu[a][:, hh, 0:8], in_max=Vv[a][:, hh, 0:8], in_values=S1[a][:, hh, :]),
                                     r=[bR, bAH[a][hh]], w=[bAH[a][hh]])
                            for (a, hh) in ah:
                                S.op("dve", lambda e: e.match_replace(out=tmp16[:, a * 8 + hh, :], in_to_replace=Vv[a][:, hh, 0:8],
                                                                      in_values=S1[a][:, hh, :], imm_value=-1e30), r=[bR, bAH[a][hh]], w=[bAH[a][hh]])
                            for (a, hh) in ah:
                                S.op("dve", lambda e: e.max(out=Vv[a][:, hh, 8:16], in_=tmp16[:, a * 8 + hh, :]), r=[bAH[a][hh]], w=[bAH[a][hh]])
                            for (a, hh) in ah:
                                S.op("dve", lambda e: e.max_index(out=Iu[a][:, hh, 8:16], in_max=Vv[a][:, hh, 8:16], in_values=tmp16[:, a * 8 + hh, :]),
                                     r=[bAH[a][hh]], w=[bAH[a][hh]])
                            for a in range(2):
                                ro("dve", lambda e: e.tensor_copy(out=If_[a][:], in_=Iu[a][:]), extra_r=bAH[a], extra_w=bAH[a])
                            for hh in range(8):
                                S.op("dve", lambda e: e.tensor_tensor(out=cand[:, hh, :].rearrange("p (i j) -> p i j", i=16),
                                                                      in0=Vv[0][:, hh, :].unsqueeze(2).broadcast_to([128, 16, 16]),
                                                                      in1=Vv[1][:, hh, :].unsqueeze(1).broadcast_to([128, 16, 16]), op=ALU.add),
                                     r=[bAH[0][hh], bAH[1][hh], bR], w=[bH[hh]])
                            for hh in range(8):
                                S.op("dve", lambda e: e.max(out=SC[:, hh, 0:8], in_=cand[:, hh, :]), r=[bH[hh]], w=[bH[hh]])
                            for hh in range(8):
                                S.op("dve", lambda e: e.max_index(out=CIu[:, hh, 0:8], in_max=SC[:, hh, 0:8], in_values=cand[:, hh, :]), r=[bH[hh]], w=[bH[hh]])
                            for hh in range(8):
                                S.op("dve", lambda e: e.match_replace(out=tmpc[:, hh, :], in_to_replace=SC[:, hh, 0:8], in_values=cand[:, hh, :],
                                                                      imm_value=-1e30), r=[bH[hh], bR], w=[bH[hh]])
                            for hh in range(8):
                                S.op("dve", lambda e: e.max(out=SC[:, hh, 8:16], in_=tmpc[:, hh, :]), r=[bH[hh]], w=[bH[hh]])
                            for hh in range(8):
                                S.op("dve", lambda e: e.max_index(out=CIu[:, hh, 8:16], in_max=SC[:, hh, 8:16], in_values=tmpc[:, hh, :]), r=[bH[hh]], w=[bH[hh]])
                            ro("dve", lambda e: e.tensor_copy(out=CIf[:], in_=CIu[:]), extra_r=bH, extra_w=bH)
                            ro("dve", lambda e: e.tensor_tensor(out=ex[:], in0=SC[:], in1=SC[:, :, 0:1].broadcast_to([128, 8, 16]), op=ALU.subtract))
                            ro("act", lambda e: e.activation(out=ex[:], in_=ex[:], func=AF.Exp))
                            ro("dve", lambda e: e.reduce_sum(out=zz[:, 0:8], in_=ex[:], axis=AX.X))
                            ro("dve", lambda e: e.reciprocal(out=zz[:, 8:16], in_=zz[:, 0:8]))
                            ro("dve", lambda e: e.tensor_tensor(out=gate[:], in0=ex[:], in1=zz[:, 8:16].unsqueeze(2).broadcast_to([128, 8, 16]),
                                                                op=ALU.mult))
                            ro("dve", lambda e: e.tensor_scalar(out=ii[:], in0=CIf[:], scalar1=1.0 / 16, scalar2=-0.46875, op0=ALU.mult, op1=ALU.add))
                            ro("dve", lambda e: e.tensor_copy(out=irf[:], in_=ii[:]))
                            ro("dve", lambda e: e.scalar_tensor_tensor(out=jrf[:], in0=irf[:], scalar=-16.0, in1=CIf[:], op0=ALU.mult, op1=ALU.add))
                            for a, sel in ((0, irf), (1, jrf)):
                                for hh in range(8):
                                    ro("dve", lambda e: e.tensor_tensor(out=tmpc[:, hh, :].rearrange("p (k i) -> p k i", k=16), in0=sel[:, hh, :].unsqueeze(2).broadcast_to([128, 16, 16]),
                                                                        in1=io16.unsqueeze(1).broadcast_to([128, 16, 16]), op=ALU.is_equal))
                                    ro("dve", lambda e: e.tensor_tensor(out=tmpc[:, hh, :].rearrange("p (k i) -> p k i", k=16), in0=tmpc[:, hh, :].rearrange("p (k i) -> p k i", k=16),
                                                                        in1=If_[a][:, hh, :].unsqueeze(1).broadcast_to([128, 16, 16]), op=ALU.mult))
                                ro("dve", lambda e: e.reduce_sum(out=e12[a][:].rearrange("p h k -> p (h k)"),
                                                                 in_=tmpc[:].rearrange("p h (k i) -> p (h k) i", k=16), axis=AX.X))
                            TRB, bTRB = C.ps[4], C.bps[4]
                            for n_, src in enumerate((e12[0], e12[1], gate)):
                                ro("pe", lambda e: e.transpose(out=TRB[:, n_ * 128:(n_ + 1) * 128], in_=src[:].rearrange("p h k -> p (h k)"),
                                                               identity=C.identf[:]), extra_r=[C.bidentf], extra_w=[bTRB])
                            ro("act", lambda e: e.activation(out=tr3[:].rearrange("p a t -> p (a t)"), in_=TRB[:, 0:384], func=AF.Copy), extra_r=[bTRB])
                            if "R3d" in T and tt == 0:
                                S.dma("sp", T["R3d"][:], tr3[:], r=[bR], is_output=True)
                                S.dma("sp", T["S1d"][:], S1[0][:], r=[bR], is_output=True)
                                S.dma("sp", T["V1d"][:], Vv[0][:], r=[bR], is_output=True)
                                S.dma("sp", T["SCd"][:], SC[:], r=[bR], is_output=True)
                                S.dma("sp", T["CId"][:], CIf[:], r=[bR], is_output=True)
                                S.dma("sp", T["I1d"][:], If_[0][:], r=[bR], is_output=True)
                            g_t, bg = GTt[0]
                            for t4 in range(32):
                                pb_, bpb = C.ps[5 + t4 % 3], C.bps[5 + t4 % 3]
                                for tk in range(4):
                                    t = t4 * 4 + tk
                                    (a_t, ba), (b_t, bb) = At[t % 4], Bt[t % 4]
                                    S.op("dve", lambda e: e.tensor_scalar(out=a_t[:], in0=C.iota128[:], scalar1=tr3[:, 0, t:t + 1],
                                                                          scalar2=tr3[:, 2, t:t + 1], op0=ALU.is_equal, op1=ALU.mult),
                                         r=[bR, C.biota], w=[ba])
                                    S.op("dve", lambda e: e.tensor_scalar(out=b_t[:], in0=C.iota128[:], scalar1=tr3[:, 1, t:t + 1],
                                                                          scalar2=None, op0=ALU.is_equal), r=[bR, C.biota], w=[bb])
                                    S.op("pe", lambda e: e.matmul(pb_[:, tk * 128:(tk + 1) * 128], b_t[:], a_t[:], start=True, stop=True),
                                         r=[ba, bb], w=[bpb])
                                S.op("act", lambda e: e.activation(out=g_t[:, :, t4 * 4:(t4 + 1) * 4].rearrange("p e t -> p t e"),
                                                                   in_=pb_[:].rearrange("p (t e) -> p t e", t=4), func=AF.Copy), r=[bpb], w=[bg])
                            S.dma("sp", T["GT"][st * 4 + tt], g_t[:], r=[bg])
                        S.barrier()
                if stop_after == "B4":
                    continue
                with contextlib.ExitStack() as b5:
                    tl = lambda name, shape, dt: _tile(nc, b5, name, shape, dt)
                    Dn = [tl(f"Dn{i}", [128, 32, 256], BF16) for i in range(2)]
                    Up = [tl(f"Up{i}", [128, 8, 512], BF16) for i in range(2)]
                    act = [tl(f"act{i}", [128, 8, TS], BF16) for i in range(2)]
                    Gc = [tl(f"Gc{i}", [128, 8, TS], BF16) for i in range(2)]
                    gel = [tl(f"gel{i}", [128, TS], BF16) for i in range(2)]
                    dn_v = T["downT"].rearrange("(c p) e -> p c e", p=128)
                    up_v = T["up"].rearrange("(c p) d -> p c d", p=128)
                    nd = nu = ng = 0
                    first_pass = False
                    for cg in range(C.ncg):
                        (g_c, bgc), (a_c, bac) = Gc[cg % 2], act[cg % 2]
                        for tt in range(4):
                            S.dma("sp", g_c[:, :, tt * 128:(tt + 1) * 128], T["GT"][st * 4 + tt][:, cg * 8:(cg + 1) * 8, :], w=[bgc])
                        for cp in range(4):
                            (d_t, bd) = Dn[nd % 2]
                            nd += 1
                            e0 = (cg * 8 + cp * 2) * 128
                            if first_pass:
                                S.dma("pool", d_t[:], dn_v[:, :, e0:e0 + 256], w=[bd])
                                S.dma("sp", T["DnS"][cg * 4 + cp], d_t[:], r=[bd])
                            else:
                                S.dma("sp", d_t[:], T["DnS"][cg * 4 + cp], w=[bd])
                            for ck in range(2):
                                ci = cp * 2 + ck
                                pb_, bpb = C.ps[nb % 8], C.bps[nb % 8]
                                nb += 1
                                for dc in range(32):
                                    S.op("pe", lambda e: e.matmul(pb_[:], d_t[:, dc, ck * 128:(ck + 1) * 128], xnT[:, dc, :],
                                                                  start=(dc == 0), stop=(dc == 31)), r=[bd, bxnT], w=[bpb], signal=(dc == 31))
                                (ge, bge) = gel[ng % 2]
                                ng += 1
                                S.op("act", lambda e: e.activation(out=ge[:], in_=pb_[:], func=GELU), r=[bpb], w=[bge])
                                S.op("dve", lambda e: e.tensor_tensor(out=a_c[:, ci, :], in0=ge[:], in1=g_c[:, ci, :], op=ALU.mult),
                                     r=[bge, bgc], w=[bac])
                        for ds in range(8):
                            (u_t, bu) = Up[nu % 2]
                            nu += 1
                            if first_pass:
                                S.dma("pool", u_t[:], up_v[:, cg * 8:(cg + 1) * 8, ds * 512:(ds + 1) * 512], w=[bu])
                                S.dma("sp", T["UpS"][cg * 8 + ds], u_t[:], r=[bu])
                            else:
                                S.dma("pool", u_t[:], T["UpS"][cg * 8 + ds], w=[bu])
                            for tt in range(4):
                                pb_, bpb = C.ps[nb % 8], C.bps[nb % 8]
                                nb += 1
                                for ci in range(8):
                                    S.op("pe", lambda e: e.matmul(pb_[:], a_c[:, ci, tt * 128:(tt + 1) * 128], u_t[:, ci, :],
                                                                  start=(ci == 0), stop=(ci == 7)), r=[bac, bu], w=[bpb], signal=(ci == 7))
                                hs = hT[tt][0][:, ds * 512:(ds + 1) * 512]
                                S.op("dve", lambda e: e.tensor_tensor(out=hs, in0=pb_[:], in1=hs, op=ALU.add), r=[bpb, hT[tt][1]], w=[hT[tt][1]])
                    S.barrier()
            if "Hd" in T:
                for tt in range(4):
                    S.dma("sp", T["Hd"][tt * 128:(tt + 1) * 128, :], hT[tt][0][:], r=[hT[tt][1]], is_output=True)
            with contextlib.ExitStack() as b6:
                tl = lambda name, shape, dt: _tile(nc, b6, name, shape, dt)
                gfin, bgfin = tl("gfin", [128, D], F32)
                S.dma("sp", gfin[:], T["gfin_r"][:], w=[bgfin])
                junk, bjunk = tl("junkC", [128, D], BF16)
                ot = [tl(f"ot{i}", [128, D], F32) for i in range(2)]
                s8 = [tl(f"s8c{i}", [128, 8], F32) for i in range(2)]
                for tt in range(4):
                    (o_t, bo), (s_, bs_) = ot[tt % 2], s8[tt % 2]
                    h_t, bh = hT[tt]
                    S.op("dve", lambda e: e.memset(s_[:], 0.0), w=[bs_])
                    S.op("act", lambda e: e.activation(out=junk[:], in_=h_t[:], func=AF.Square, accum_out=s_[:, 0:1]), r=[bh], w=[bjunk, bs_])
                    rstd_ops(S, s_[:, 0:1], s_[:, 1:2], bs_, s_[:, 2:4], 1, 1.0 / D, [])
                    S.op("dve", lambda e: e.scalar_tensor_tensor(out=o_t[:], in0=h_t[:], scalar=s_[:, 1:2], in1=gfin[:],
                                                                 op0=ALU.mult, op1=ALU.mult), r=[bh, bs_, bgfin], w=[bo])
                    S.dma("sp", T["out"][t0 + tt * 128:t0 + (tt + 1) * 128, :], o_t[:], r=[bo], is_output=True)
                S.barrier()
        S.barrier()


def build(dbg=None):
    nc = bass.Bass("TRN2", target_bir_lowering=False)
    T = {}

    def din(name, shape, dt=F32):
        T[name] = nc.dram_tensor(name, list(shape), dt, kind="ExternalInput").ap()

    def dscr(name, shape, dt, out=False):
        T[name] = nc.dram_tensor(name, list(shape), dt, kind="ExternalOutput" if out else "Internal").ap()

    din("x_own", [NTOK, D])
    din("x_prev", [NTOK, D])
    din("gmix_r", [128, D])
    din("w_in", [D, 6144])
    din("ident", [128, 128])
    for n in ("a_re_q", "a_im_q", "ldt_q"):
        din(n, [128, 128])
    for n in ("a_re_b", "a_im_b", "ldt_b", "b_re_b", "b_im_b"):
        din(n, [128, 1024])
    din("cmix1", [128, 2048])
    din("cmix2", [128, 2048])
    din("d_col", [128, 16])
    din("sgn1", [128, 1])
    din("rowmask", [128, 8])
    din("colmask", [128, 8, 128])
    din("iota512", [128, 512])
    din("swapI", [128, 128])
    din("riota512", [128, 512])
    dscr("YG", [2048, NTOK], BF16, out=(dbg == "S5"))
    din("w_glu", [2048, 2048])
    if dbg == "B2":
        dscr("YTd", [4096, TS], BF16, out=True)
        dscr("RSd", [128, 8], F32, out=True)
    din("w_out", [D, D])
    din("wmT", [128, 8, 128])
    din("bs_rep", [128, 8, 128])
    din("gm_cols", [128, 4, 16])
    din("gffn_r", [128, D])
    din("gfin_r", [128, D])
    din("w_q", [D, 2048])
    din("k1T", [128, 128])
    din("k2T", [128, 128])
    din("downT", [D, 16384])
    din("up", [16384, D])
    din("iota128", [128, 128])
    dscr("GT", [16, 128, 128, 128], BF16)
    dscr("DnS", [64, 128, 32, 256], BF16)
    dscr("WinS", [24, 128, 32, 256], BF16)
    dscr("WoS", [8, 128, 32, 512], BF16)
    dscr("WqS", [8, 128, 32, 256], BF16)
    dscr("UpS", [128, 128, 8, 512], BF16)
    if dbg == "B6x":
        dscr("Qd", [2048, TS], BF16, out=True)
        dscr("R3d", [128, 3, 128], F32, out=True)
        dscr("S1d", [128, 8, 128], F32, out=True)
        dscr("V1d", [128, 8, 16], F32, out=True)
        dscr("SCd", [128, 8, 16], F32, out=True)
        dscr("CId", [128, 8, 16], F32, out=True)
        dscr("I1d", [128, 8, 16], F32, out=True)
        dscr("Hd", [TS, D], F32, out=True)
    dscr("ZT", [2048, 4096], BF16, out=(dbg == "A"))
    dscr("UT", [2048, NTOK], BF16, out=(dbg == "A"))
    dscr("VN", [NTOK, 2048], BF16, out=(dbg == "A"))
    dscr("out", [NTOK, D], F32, out=True)

    with contextlib.ExitStack() as st:
        C = Ctx()
        C.nc, C.T = nc, T
        C.S = S = Sched(nc, st)
        C.ps = [st.enter_context(nc.psum_tensor(f"ps{i}", [128, 512], F32)) for i in range(8)]
        C.bps = [Buf(f"ps{i}") for i in range(8)]
        C.bZT, C.bUT, C.bVN, C.bYG, C.bGT = Buf("ZT"), Buf("UT"), Buf("VN"), Buf("YG"), Buf("GT")
        C.identb, C.bidentb = _tile(nc, st, "identb", [128, 128], BF16)
        C.identf, C.bidentf = _tile(nc, st, "identf", [128, 128], F32)
        S.dma("pool", C.identb[:], T["ident"][:], w=[C.bidentb])
        S.dma("sp", C.identf[:], T["ident"][:], w=[C.bidentf])
        C.iota128, C.biota = _tile(nc, st, "iota128", [128, 128], F32)
        S.dma("sp", C.iota128[:], T["iota128"][:], w=[C.biota])
        C.bkT = Buf("kT")
        C.kT = [st.enter_context(nc.sbuf_tensor(f"sb_k{a}T", [128, 128], BF16)) for a in range(2)]
        S.dma("pool", C.kT[0][:], T["k1T"][:], w=[C.bkT])
        S.dma("pool", C.kT[1][:], T["k2T"][:], w=[C.bkT])
        C.ncg = 16
        C.win_cached, C.win_fresh = set(), set()
        if dbg == "A":
            phase_a(C, [0, 4])
        else:
            phase_a(C, list(range(8)))
        if dbg == "S5":
            phase_s5(C, [0, 5])
        elif dbg != "TA":
            phase_s5(C, list(range(16)))
        if dbg == "B2":
            phase_b(C, [0], stop_after="B2")
        elif dbg == "B6":
            phase_b(C, [0])
        elif dbg == "B7":
            phase_b(C, [0, 1])
        elif dbg in ("TA", "TS"):
            pass
        elif dbg == "TB2":
            phase_b(C, [0], stop_after="B2")
        elif dbg == "TB4":
            phase_b(C, [0], stop_after="B4")
        elif dbg is None:
            phase_b(C, list(range(4)))
        S.finish()
        print("instructions:", S.nins, "sems:", S.nsem)
    return nc


def host_layout(inputs):
    f = lambda k: np.ascontiguousarray(np.asarray(inputs[k], dtype=np.float32))
    x = f("x")
    shared = {}
    shared["gmix_r"] = np.ascontiguousarray(np.broadcast_to(f("norm_mix_g")[0], (128, D)))
    shared["w_in"] = f("w_in")[0]
    shared["ident"] = np.eye(128, dtype=np.float32)
    a_re, a_im, ldt = f("s5_a_re")[0], f("s5_a_im")[0], f("s5_log_dt")[0]
    b_re, b_im = f("s5_b_re")[0], f("s5_b_im")[0]
    c_re, c_im = f("s5_c_re")[0], f("s5_c_im")[0]
    cp = np.ascontiguousarray
    shared["a_re_q"] = cp(np.concatenate([a_re.T, a_re.T], axis=0))
    shared["a_im_q"] = cp(np.concatenate([a_im.T, a_im.T], axis=0))
    shared["ldt_q"] = cp(np.broadcast_to(ldt[None, :], (128, 128)))
    def blay(a_gp):
        t = a_gp.reshape(16, 8, 64)
        t = np.broadcast_to(t[:, :, None, :], (16, 8, 16, 64))
        return cp(t.transpose(1, 2, 0, 3).reshape(128, 1024))
    shared["a_re_b"] = blay(a_re)
    shared["a_im_b"] = blay(a_im)
    shared["ldt_b"] = blay(np.broadcast_to(ldt[:, None], (128, 64)))
    bl = lambda b: cp(b.reshape(16, 8, 64, 16).transpose(1, 3, 0, 2).reshape(128, 1024))
    shared["b_re_b"] = bl(b_re)
    shared["b_im_b"] = bl(b_im)
    cl = lambda c: c.transpose(2, 0, 1).reshape(64, 2048)
    shared["cmix1"] = cp(np.concatenate([cl(c_re), cl(c_im)], axis=0))
    shared["cmix2"] = cp(np.concatenate([cl(c_im), cl(c_re)], axis=0))
    shared["d_col"] = cp(f("s5_d")[0].reshape(16, 128).T)
    shared["sgn1"] = np.concatenate([np.ones((64, 1), np.float32), -np.ones((64, 1), np.float32)], axis=0)
    rm = np.zeros((128, 8), np.float32)
    cmk = np.zeros((128, 8, 128), np.float32)
    for g8 in range(8):
        rm[g8 * 16:(g8 + 1) * 16, g8] = 1.0
        cmk[:, g8, g8 * 16:(g8 + 1) * 16] = 1.0
    shared["rowmask"] = rm
    shared["colmask"] = cmk
    shared["w_glu"] = f("s5_w_glu")[0]
    shared["w_out"] = f("w_out")[0]
    shared["wmT"] = cp(f("gm_w_s")[0].transpose(2, 0, 1))
    shared["bs_rep"] = cp(np.broadcast_to(f("gm_b_s")[0][None], (128, 8, 128)))
    colv = lambda v: v.reshape(16, 128).T
    shared["gm_cols"] = cp(np.stack([colv(f("gm_ln_g")[0]), colv(f("gm_ln_b")[0]), colv(f("norm_s5_out_g")[0]),
                                     colv(f("norm_gm_out_g")[0])], axis=1))
    shared["gffn_r"] = cp(np.broadcast_to(f("norm_ffn_g")[0], (128, D)))
    shared["gfin_r"] = cp(np.broadcast_to(f("norm_final_g"), (128, D)))
    shared["w_q"] = f("peer_w_q")[0]
    shared["k1T"] = cp(f("peer_keys_1")[0].T)
    shared["k2T"] = cp(f("peer_keys_2")[0].T)
    shared["downT"] = cp(f("peer_down")[0].T)
    shared["up"] = f("peer_up")[0]
    shared["iota128"] = cp(np.broadcast_to(np.arange(128, dtype=np.float32)[None, :], (128, 128)))
    shared["swapI"] = cp(np.roll(np.eye(128, dtype=np.float32), 64, axis=1))
    shared["riota512"] = cp(np.broadcast_to(np.arange(511, -1, -1, dtype=np.float32)[None, :], (128, 512)))
    shared["iota512"] = cp(np.broadcast_to(np.arange(512, dtype=np.float32)[None, :], (128, 512)))
    maps = []
    for c in range(8):
        b, s = c // 2, c % 2
        m = dict(shared)
        m["x_own"] = np.ascontiguousarray(x[b, s * NTOK:(s + 1) * NTOK])
        m["x_prev"] = np.ascontiguousarray(x[b, 0:NTOK]) if s == 1 else np.zeros((NTOK, D), np.float32)
        maps.append(m)
    return maps


def kernel(**inputs):
    maps = host_layout(inputs)
    nc = build()
    res = run_bass_kernel_spmd(nc, maps, core_ids=list(range(8)))
    out = np.zeros((4, 4096, D), np.float32)
    for c in range(8):
        b, s = c // 2, c % 2
        out[b, s * NTOK:(s + 1) * NTOK] = res.results[c]["out"]
    return out
```

```python
import contextlib
import math

import ml_dtypes
import numpy as np

import concourse.bass as bass
import concourse.mybir as mybir
from concourse.bass_utils import run_bass_kernel_spmd

F32 = mybir.dt.float32
BF16 = mybir.dt.bfloat16
U32 = mybir.dt.uint32
ALU = mybir.AluOpType
AF = mybir.ActivationFunctionType
AX = mybir.AxisListType

D = 4096
NTOK = 2048
TS = 512
NST = NTOK // TS
EPS = 1e-6
TWO_PI = 2.0 * math.pi
GELU = AF.Gelu_apprx_tanh
CONV_INTERLEAVE = True


class Buf:
    __slots__ = ("name", "w", "r")

    def __init__(self, name=""):
        self.name = name
        self.w = None
        self.r = []


class Sched:
    EPOCH = 3500

    def __init__(self, nc, stack):
        self.nc = nc
        self.stack = stack
        self.eng = {"pe": nc.tensor, "dve": nc.vector, "act": nc.scalar, "pool": nc.gpsimd, "sp": nc.sync}
        self.sem = {}
        self.cnt = {}
        self.pending = {}
        self.seen = {k: {} for k in self.eng}
        self.nsem = 0
        self.last_old = {}
        for k in self.eng:
            self._new_epoch(k)
        self.dpool = {}
        for k, n in (("sp", 24), ("pool", 24), ("act", 8)):
            self.dpool[k] = [[self._mksem(f"d{k}{i}"), 0] for i in range(n)]
        self.dnext = {k: 0 for k in self.dpool}
        self.out_events = []
        self.nins = 0

    def _mksem(self, name):
        self.nsem += 1
        return self.stack.enter_context(self.nc.semaphore(f"{name}_{self.nsem}"))

    def _new_epoch(self, k):
        if k in self.sem and self.cnt[k] > 0:
            self.last_old[k] = (self.sem[k], self.cnt[k])
        self.sem[k] = self._mksem(f"e{k}")
        self.cnt[k] = 0
        self.pending[k] = False

    def _wait(self, k, evs):
        need = {}
        for (s, v) in evs:
            if self.seen[k].get(id(s), (None, 0))[1] >= v:
                continue
            if id(s) not in need or need[id(s)][1] < v:
                need[id(s)] = (s, v)
        for s, v in need.values():
            self.eng[k].wait_ge(s, v)
            self.seen[k][id(s)] = (s, v)

    def _deps(self, k, r, w):
        evs = []
        for b in r:
            if b.w is not None:
                evs.append(b.w)
        for b in w:
            if b.w is not None:
                evs.append(b.w)
            evs.extend(b.r)
        if k == "pe":
            evs = [e for e in evs if e[0] is not self.sem["pe"]]
        return evs

    def _mark(self, ev, r, w):
        for b in r:
            b.r.append(ev)
            if len(b.r) > 48:
                best = {}
                for (s, v) in b.r:
                    if id(s) not in best or best[id(s)][1] < v:
                        best[id(s)] = (s, v)
                b.r = list(best.values())
        for b in w:
            b.w = ev
            b.r = []

    def op(self, k, fn, r=(), w=(), signal=True):
        self._wait(k, self._deps(k, r, w))
        ins = fn(self.eng[k])
        self.nins += 1
        if signal:
            ins.then_inc(self.sem[k], 1)
            self.cnt[k] += 1
            self.pending[k] = False
            ev = (self.sem[k], self.cnt[k])
        else:
            self.pending[k] = True
            ev = (self.sem[k], self.cnt[k] + 1)
        self._mark(ev, r, w)
        if signal and self.cnt[k] >= self.EPOCH:
            self._new_epoch(k)
        return ev

    def dma(self, k, out, in_, r=(), w=(), is_output=False, **kw):
        pool = self.dpool[k]
        i = self.dnext[k]
        self.dnext[k] = (i + 1) % len(pool)
        slot = pool[i]
        evs = self._deps(k, r, w)
        if slot[1] > 0:
            evs.append((slot[0], slot[1]))
        self._wait(k, evs)
        ins = self.eng[k].dma_start(out=out, in_=in_, **kw)
        self.nins += 1
        slot[1] += 16
        ins.then_inc(slot[0], 16)
        ev = (slot[0], slot[1])
        self._mark(ev, r, w)
        if is_output:
            self.out_events.append(ev)
        return ev

    def barrier(self):
        evs = []
        for k in self.eng:
            assert not self.pending[k], k
            if self.cnt[k] > 0:
                evs.append((self.sem[k], self.cnt[k]))
            elif k in self.last_old:
                evs.append(self.last_old[k])
        for k in self.dpool:
            for s, v in self.dpool[k]:
                if v > 0:
                    evs.append((s, v))
        for k in self.eng:
            self._wait(k, evs)

    def barrier_dma(self):
        evs = []
        for k in self.dpool:
            for s_, v in self.dpool[k]:
                if v > 0:
                    evs.append((s_, v))
        for k in ("pool", "sp"):
            self._wait(k, evs)

    def finish(self):
        self._wait("sp", self.out_events)
        self.barrier()


class Ctx:
    pass


_UID = [0]


def _uname(name):
    _UID[0] += 1
    return f"{name}_{_UID[0]}"


def _tile(nc, stack, name, shape, dt):
    return stack.enter_context(nc.sbuf_tensor(_uname("sb_" + name), list(shape), dt)), Buf(name)


def phase_a(C, st_list):
    nc, S, T = C.nc, C.S, C.T
    with contextlib.ExitStack() as ph:
        tl = lambda name, shape, dt: _tile(nc, ph, name, shape, dt)
        gmix, bgmix = tl("gmix", [128, D], F32)
        S.dma("sp", gmix[:], T["gmix_r"][:], w=[bgmix])
        xt = [tl(f"xt{i}", [128, D], F32) for i in range(2)]
        abf = [tl(f"abf{i}", [128, D], BF16) for i in range(4)]
        aT, baT = tl("aT", [128, 32, TS], BF16)
        wb = [tl(f"wb{i}", [128, 32, 256], BF16) for i in range(2)]
        zs = [tl(f"zs{i}", [128, TS], BF16) for i in range(3)]
        V = [tl(f"V{i}", [128, 2048], F32) for i in range(4)]
        vn = [tl(f"vn{i}", [128, 2048], BF16) for i in range(2)]
        junk, bjunk = tl("junkA", [128, 2048], BF16)
        st8 = [tl(f"st8{i}", [128, 8], F32) for i in range(2)]
        w_in_v = T["w_in"].rearrange("(c p) n -> p c n", p=128)
        nx = 0
        nw = 0
        nz = 0
        nb = 0
        def norm_part(st):
            own = st >= NST
            xsrc = T["x_own"] if own else T["x_prev"]
            t0 = (st - NST if own else st) * TS
            for tt in range(4):
                (x_t, bx), (a_t, ba), (s8, bs8) = xt[tt % 2], abf[tt], st8[tt % 2]
                S.dma("sp", x_t[:], xsrc[t0 + tt * 128:t0 + (tt + 1) * 128, :], w=[bx])
                S.op("dve", lambda e: e.memset(s8[:], 0.0), w=[bs8])
                S.op("act", lambda e: e.activation(out=a_t[:], in_=x_t[:], func=AF.Square, accum_out=s8[:, 0:1]),
                     r=[bx], w=[ba, bs8])
                S.op("dve", lambda e: e.tensor_scalar(out=s8[:, 1:2], in0=s8[:, 0:1], scalar1=1.0 / D, scalar2=EPS,
                                                      op0=ALU.mult, op1=ALU.add), r=[bs8], w=[bs8])
                S.op("act", lambda e: e.activation(out=s8[:, 3:4], in_=s8[:, 1:2], func=AF.Sqrt), r=[bs8], w=[bs8])
                S.op("dve", lambda e: e.reciprocal(out=s8[:, 2:3], in_=s8[:, 3:4]), r=[bs8], w=[bs8])
                S.op("dve", lambda e: e.scalar_tensor_tensor(out=a_t[:], in0=x_t[:], scalar=s8[:, 2:3], in1=gmix[:],
                                                             op0=ALU.mult, op1=ALU.mult), r=[bx, bs8, bgmix], w=[ba])

        def transpose_part():
            nonlocal_nb = cntA["nb"]
            for tt in range(4):
                (a_t, ba) = abf[tt]
                for dcg in range(4):
                    pb, bpb = C.ps[nonlocal_nb % 8], C.bps[nonlocal_nb % 8]
                    nonlocal_nb += 1
                    pbb = pb[:].bitcast(BF16)
                    for j in range(8):
                        dc = dcg * 8 + j
                        S.op("pe", lambda e: e.transpose(out=pbb[:, j * 128:(j + 1) * 128],
                                                         in_=a_t[:, dc * 128:(dc + 1) * 128], identity=C.identb[:]),
                             r=[ba, C.bidentb], w=[bpb], signal=(j == 7))
                    src = pbb[:, 0:1024].rearrange("p (j t) -> p j t", j=8)
                    dst = aT[:, dcg * 8:(dcg + 1) * 8, tt * 128:(tt + 1) * 128]
                    if dcg % 2 == 0:
                        S.op("act", lambda e: e.activation(out=dst, in_=src, func=AF.Copy), r=[bpb], w=[baT])
                    else:
                        S.op("dve", lambda e: e.tensor_copy(out=dst, in_=src), r=[bpb], w=[baT])
            cntA["nb"] = nonlocal_nb

        cntA = {"nb": 0}
        norm_part(st_list[0])
        for si, st in enumerate(st_list):
            own = st >= NST
            t0 = (st - NST if own else st) * TS
            transpose_part()
            if si + 1 < len(st_list):
                norm_part(st_list[si + 1])
            nb = cntA["nb"]
            ncol = 24 if own else 8
            for ct in range(ncol):
                (w_t, bw) = wb[nw % 2]
                nw += 1
                if ct not in C.win_cached:
                    S.dma("pool", w_t[:], w_in_v[:, :, ct * 256:(ct + 1) * 256], w=[bw])
                    S.dma("sp", T["WinS"][ct], w_t[:], r=[bw])
                    C.win_cached.add(ct)
                    C.win_fresh.add(ct)
                else:
                    if ct in C.win_fresh:
                        S.barrier_dma()
                        C.win_fresh.clear()
                    S.dma("pool", w_t[:], T["WinS"][ct], w=[bw])
                if ct < 16:
                    for sub in range(2):
                        pb, bpb = C.ps[nb % 8], C.bps[nb % 8]
                        nb += 1
                        for dc in range(32):
                            S.op("pe", lambda e: e.matmul(pb[:], w_t[:, dc, sub * 128:(sub + 1) * 128], aT[:, dc, :],
                                                          start=(dc == 0), stop=(dc == 31)),
                                 r=[bw, baT], w=[bpb], signal=(dc == 31))
                        (z_t, bz) = zs[nz % 3]
                        nz += 1
                        if ct < 8:
                            S.op("act", lambda e: e.activation(out=z_t[:], in_=pb[:], func=AF.Copy), r=[bpb], w=[bz])
                            row = (ct * 2 + sub) * 128
                            S.dma("sp", T["ZT"][row:row + 128, st * TS:(st + 1) * TS], z_t[:], r=[bz])
                        else:
                            S.op("act", lambda e: e.activation(out=z_t[:], in_=pb[:], func=GELU), r=[bpb], w=[bz])
                            row = ((ct - 8) * 2 + sub) * 128
                            S.dma("sp", T["UT"][row:row + 128, t0:t0 + TS], z_t[:], r=[bz])
                else:
                    vc = ct - 16
                    for tt in range(4):
                        pb, bpb = C.ps[nb % 8], C.bps[nb % 8]
                        nb += 1
                        for dc in range(32):
                            S.op("pe", lambda e: e.matmul(pb[:, 0:256], aT[:, dc, tt * 128:(tt + 1) * 128], w_t[:, dc, :],
                                                          start=(dc == 0), stop=(dc == 31)),
                                 r=[bw, baT], w=[bpb], signal=(dc == 31))
                        S.op("act", lambda e: e.activation(out=V[tt][0][:, vc * 256:(vc + 1) * 256], in_=pb[:, 0:256],
                                                           func=GELU), r=[bpb], w=[V[tt][1]])
            cntA["nb"] = nb
            if own:
                for tt in range(4):
                    v_t, bv = V[tt]
                    (s8, bs8) = st8[nx % 2]
                    (vn_t, bvn) = vn[nx % 2]
                    nx += 1
                    S.op("dve", lambda e: e.memset(s8[:], 0.0), w=[bs8])
                    S.op("act", lambda e: e.activation(out=vn_t[:], in_=v_t[:], func=AF.Square,
                                                       accum_out=s8[:, 0:1]), r=[bv], w=[bvn, bs8])
                    S.op("dve", lambda e: e.tensor_scalar(out=junk[:, 0:2048], in0=v_t[:], scalar1=1.0, scalar2=0.0,
                                                          op0=ALU.mult, op1=ALU.add, accum_out=s8[:, 1:2]),
                         r=[bv], w=[bjunk, bs8])
                    S.op("dve", lambda e: e.tensor_scalar(out=s8[:, 2:3], in0=s8[:, 1:2], scalar1=1.0 / 2048, scalar2=None,
                                                          op0=ALU.mult), r=[bs8], w=[bs8])
                    S.op("dve", lambda e: e.tensor_tensor(out=s8[:, 3:4], in0=s8[:, 2:3], in1=s8[:, 2:3], op=ALU.mult),
                         r=[bs8], w=[bs8])
                    S.op("dve", lambda e: e.scalar_tensor_tensor(out=s8[:, 4:5], in0=s8[:, 0:1], scalar=1.0 / 2048,
                                                                 in1=s8[:, 3:4], op0=ALU.mult, op1=ALU.subtract),
                         r=[bs8], w=[bs8])
                    S.op("dve", lambda e: e.tensor_scalar(out=s8[:, 6:7], in0=s8[:, 4:5], scalar1=EPS, scalar2=None,
                                                          op0=ALU.add), r=[bs8], w=[bs8])
                    S.op("act", lambda e: e.activation(out=s8[:, 7:8], in_=s8[:, 6:7], func=AF.Sqrt), r=[bs8], w=[bs8])
                    S.op("dve", lambda e: e.reciprocal(out=s8[:, 5:6], in_=s8[:, 7:8]), r=[bs8], w=[bs8])
                    S.op("dve", lambda e: e.tensor_scalar(out=vn_t[:], in0=v_t[:], scalar1=s8[:, 2:3], scalar2=s8[:, 5:6],
                                                          op0=ALU.subtract, op1=ALU.mult), r=[bv, bs8], w=[bvn])
                    S.dma("sp", T["VN"][t0 + tt * 128:t0 + (tt + 1) * 128, :], vn_t[:], r=[bvn])
        S.barrier()


def phase_s5(C, gb_list):
    nc, S, T = C.nc, C.S, C.T
    PI = math.pi
    with contextlib.ExitStack() as ph:
        tl = lambda name, shape, dt: _tile(nc, ph, name, shape, dt)
        bset = Buf("s5setup")
        raw = lambda name, shape, dt: ph.enter_context(nc.sbuf_tensor(_uname("sr_" + name), list(shape), dt))
        so = lambda k, fn: S.op(k, fn, r=[bset], w=[bset])
        aq, iq, lq = raw("aq", [128, 128], F32), raw("iq", [128, 128], F32), raw("lq", [128, 128], F32)
        theta, mag, tq = raw("theta", [128, 128], F32), raw("mag", [128, 128], F32), raw("tq", [128, 128], F32)
        offs = raw("offs", [128, 8, 128], F32)
        S.dma("sp", aq[:], T["a_re_q"][:], w=[bset])
        S.dma("sp", iq[:], T["a_im_q"][:], w=[bset])
        S.dma("sp", lq[:], T["ldt_q"][:], w=[bset])
        so("act", lambda e: e.activation(out=lq[:], in_=lq[:], func=AF.Exp))
        so("dve", lambda e: e.tensor_tensor(out=tq[:], in0=aq[:], in1=lq[:], op=ALU.mult))
        so("act", lambda e: e.activation(out=mag[:], in_=tq[:], func=AF.Exp))
        so("dve", lambda e: e.tensor_tensor(out=theta[:], in0=iq[:], in1=lq[:], op=ALU.mult))
        kq = raw("kq", [128, 128], mybir.dt.int32)
        so("dve", lambda e: e.tensor_scalar(out=tq[:], in0=theta[:], scalar1=1.0 / TWO_PI, scalar2=None, op0=ALU.mult))
        so("dve", lambda e: e.tensor_copy(out=kq[:], in_=tq[:]))
        so("dve", lambda e: e.tensor_tensor(out=theta[:], in0=tq[:], in1=kq[:], op=ALU.subtract))
        for seg in range(8):
            so("dve", lambda e: e.tensor_scalar(out=tq[:], in0=theta[:], scalar1=float(512 * seg), scalar2=None, op0=ALU.mult))
            so("dve", lambda e: e.tensor_copy(out=kq[:], in_=tq[:]))
            so("dve", lambda e: e.tensor_tensor(out=offs[:, seg, :], in0=tq[:], in1=kq[:], op=ALU.subtract))
        hp = raw("hp", [128, 1], F32)
        BST1, BST2 = raw("BST1", [128, 16, 128], BF16), raw("BST2", [128, 16, 128], BF16)
        sg = raw("sg", [128, 1], F32)
        CST1, CST2 = raw("CST1", [128, 16, 128], BF16), raw("CST2", [128, 16, 128], BF16)
        dcol, DST = raw("dcol", [128, 16], F32), raw("DST", [128, 16, 128], BF16)
        rmask, cmask = raw("rmask", [128, 8], F32), raw("cmask", [128, 8, 128], BF16)
        iota = raw("iota512", [128, 512], F32)
        riota = raw("riota512", [128, 512], F32)
        c512, s512 = raw("c512", [128, 128], F32), raw("s512", [128, 128], F32)
        swapI = raw("swapI", [128, 128], F32)
        eq_, mag512 = raw("eq", [128, 128], F32), raw("mag512", [128, 128], F32)
        tmpst = contextlib.ExitStack()
        rawt = lambda name, shape, dt: tmpst.enter_context(nc.sbuf_tensor(_uname("st_" + name), list(shape), dt))
        names = ["arb", "aib", "ldb", "brb", "bib", "magb", "ang", "nsin", "ncos", "nr", "ni", "u1", "u2", "cr", "ci"]
        B = {n: rawt("s5" + n, [128, 1024], F32) for n in names}
        for n, src in (("arb", "a_re_b"), ("aib", "a_im_b"), ("ldb", "ldt_b"), ("brb", "b_re_b"), ("bib", "b_im_b")):
            S.dma("sp", B[n][:], T[src][:], w=[bset])
        tt_ = lambda o, a, b, op: so("dve", lambda e: e.tensor_tensor(out=B[o][:], in0=B[a][:], in1=B[b][:], op=op))
        so("act", lambda e: e.activation(out=B["ldb"][:], in_=B["ldb"][:], func=AF.Exp))
        tt_("u1", "arb", "ldb", ALU.mult)
        so("act", lambda e: e.activation(out=B["magb"][:], in_=B["u1"][:], func=AF.Exp))
        tt_("ang", "aib", "ldb", ALU.mult)
        kb = rawt("kb", [128, 1024], mybir.dt.int32)
        so("dve", lambda e: e.memset(hp[:], PI / 2))
        so("dve", lambda e: e.tensor_scalar(out=B["u1"][:], in0=B["ang"][:], scalar1=1.0 / TWO_PI, scalar2=None, op0=ALU.mult))
        so("dve", lambda e: e.tensor_copy(out=kb[:], in_=B["u1"][:]))
        so("dve", lambda e: e.tensor_tensor(out=B["u1"][:], in0=B["u1"][:], in1=kb[:], op=ALU.subtract))
        so("dve", lambda e: e.scalar_tensor_tensor(out=B["u2"][:], in0=B["u1"][:], scalar=-1.0, in1=B["u1"][:],
                                                   op0=ALU.mult, op1=ALU.max))
        so("act", lambda e: e.activation(out=B["nsin"][:], in_=B["u1"][:], func=AF.Sin, scale=TWO_PI))
        so("act", lambda e: e.activation(out=B["ncos"][:], in_=B["u2"][:], func=AF.Sin, scale=-TWO_PI, bias=hp[:, 0:1]))
        tt_("u1", "magb", "ncos", ALU.mult)
        so("dve", lambda e: e.tensor_scalar(out=B["nr"][:], in0=B["u1"][:], scalar1=-1.0, scalar2=None, op0=ALU.add))
        tt_("ni", "magb", "nsin", ALU.mult)
        tt_("u1", "arb", "arb", ALU.mult)
        tt_("u2", "aib", "aib", ALU.mult)
        tt_("u1", "u1", "u2", ALU.add)
        so("dve", lambda e: e.reciprocal(out=B["u2"][:], in_=B["u1"][:]))
        tt_("cr", "nr", "arb", ALU.mult)
        tt_("u1", "ni", "aib", ALU.mult)
        tt_("cr", "cr", "u1", ALU.add)
        tt_("cr", "cr", "u2", ALU.mult)
        tt_("ci", "ni", "arb", ALU.mult)
        tt_("u1", "nr", "aib", ALU.mult)
        tt_("ci", "ci", "u1", ALU.subtract)
        tt_("ci", "ci", "u2", ALU.mult)
        tt_("nr", "cr", "brb", ALU.mult)
        tt_("u1", "ci", "bib", ALU.mult)
        tt_("nr", "nr", "u1", ALU.subtract)
        tt_("ni", "cr", "bib", ALU.mult)
        tt_("u1", "ci", "brb", ALU.mult)
        tt_("ni", "ni", "u1", ALU.add)
        v3 = lambda n: B[n][:].rearrange("p (g q) -> p g q", g=16)
        so("dve", lambda e: e.tensor_copy(out=BST1[:, :, 0:64], in_=v3("nr")))
        so("dve", lambda e: e.tensor_copy(out=BST1[:, :, 64:128], in_=v3("ni")))
        so("dve", lambda e: e.tensor_copy(out=BST2[:, :, 0:64], in_=v3("ni")))
        so("dve", lambda e: e.tensor_scalar(out=BST2[:, :, 64:128], in0=v3("nr"), scalar1=-1.0, scalar2=None, op0=ALU.mult))
        cm1 = B["arb"]
        cmA = rawt("cmA", [128, 2048], F32)
        S.dma("sp", cmA[:], T["cmix1"][:], w=[bset])
        S.dma("sp", sg[:], T["sgn1"][:], w=[bset])
        cmB = rawt("cmB", [128, 2048], F32)
        S.dma("sp", cmB[:], T["cmix2"][:], w=[bset])
        so("dve", lambda e: e.tensor_scalar(out=CST2[:].rearrange("p g q -> p (g q)"), in0=cmB[:], scalar1=-1.0,
                                            scalar2=None, op0=ALU.mult))
        so("dve", lambda e: e.tensor_scalar(out=CST1[:].rearrange("p g q -> p (g q)"), in0=cmA[:], scalar1=sg[:, 0:1],
                                            scalar2=None, op0=ALU.mult))
        S.dma("sp", dcol[:], T["d_col"][:], w=[bset])
        for gb in range(16):
            so("dve", lambda e: e.tensor_scalar(out=DST[:, gb, :], in0=C.identf[:], scalar1=dcol[:, gb:gb + 1],
                                                scalar2=None, op0=ALU.mult))
        S.dma("sp", rmask[:], T["rowmask"][:], w=[bset])
        S.dma("pool", cmask[:], T["colmask"][:], w=[bset])
        S.dma("sp", iota[:], T["iota512"][:], w=[bset])

        S.dma("sp", swapI[:], T["swapI"][:], w=[bset])
        S.dma("sp", riota[:], T["riota512"][:], w=[bset])
        so("dve", lambda e: e.tensor_tensor(out=eq_[:], in0=aq[:], in1=lq[:], op=ALU.mult))
        so("act", lambda e: e.activation(out=mag512[:], in_=eq_[:], func=AF.Exp, scale=512.0))
        so("dve", lambda e: e.tensor_scalar(out=tq[:], in0=theta[:], scalar1=512.0, scalar2=None, op0=ALU.mult))
        so("dve", lambda e: e.tensor_copy(out=kq[:], in_=tq[:]))
        so("dve", lambda e: e.tensor_tensor(out=tq[:], in0=tq[:], in1=kq[:], op=ALU.subtract))
        so("dve", lambda e: e.scalar_tensor_tensor(out=c512[:], in0=tq[:], scalar=-1.0, in1=tq[:], op0=ALU.mult, op1=ALU.max))
        so("act", lambda e: e.activation(out=s512[:], in_=tq[:], func=AF.Sin, scale=TWO_PI))
        so("act", lambda e: e.activation(out=c512[:], in_=c512[:], func=AF.Sin, scale=-TWO_PI, bias=hp[:, 0:1]))
        so("dve", lambda e: e.tensor_scalar(out=s512[:], in0=s512[:], scalar1=sg[:, 0:1], scalar2=None, op0=ALU.mult))

        S.barrier()
        tmpst.close()
        NSET = 4
        zt = [tl(f"zt{i}", [128, 4096], BF16) for i in range(2)]
        bm = [[tl(f"bm{k}{i}", [128, 128], BF16) for i in range(4)] for k in range(NSET)]
        ROT = [tl(f"ROT{k}", [128, 128], F32) for k in range(NSET)]
        rtm = [tl(f"rtm{k}", [128, 128], F32) for k in range(2)]
        SNt = [tl(f"SN{k}", [128, 512], F32) for k in range(NSET)]
        CSt = [tl(f"CS{k}", [128, 512], F32) for k in range(NSET)]
        kph = [tl(f"kph{k}", [128, 512], mybir.dt.int32) for k in range(2)]
        t1 = [tl(f"t1{k}", [128, 512], F32) for k in range(4)]
        t2 = [tl(f"t2{k}", [128, 512], F32) for k in range(4)]
        vv = [tl(f"vv{k}", [128, 512], F32) for k in range(4)]
        q1 = [tl(f"q1{k}", [128, 512], BF16) for k in range(4)]
        q2 = [tl(f"q2{k}", [128, 512], BF16) for k in range(4)]
        ini = [tl(f"ini{k}", [128, 1], F32) for k in range(4)]
        accs = [tl(f"accs{k}", [128, 4], F32) for k in range(4)]
        CSb = [tl(f"CSb{k}", [128, 512], BF16) for k in range(NSET)]
        SNb = [tl(f"SNb{k}", [128, 512], BF16) for k in range(NSET)]
        vb = [tl(f"vb{k}", [128, 512], BF16) for k in range(4)]
        junkS = [tl(f"junkS{k}", [128, 512], F32) for k in range(2)]
        DEC = [tl(f"DEC{k}", [128, 512], F32) for k in range(2)]
        MCS = [tl(f"MCS{k}", [128, 512], F32) for k in range(NSET)]
        MSN = [tl(f"MSN{k}", [128, 512], F32) for k in range(NSET)]
        conv_jobs = []
        wout_v = T["w_out"].rearrange("(c p) n -> p c n", p=128)
        wq_v = T["w_q"].rearrange("(c p) n -> p c n", p=128)
        dn_v = T["downT"].rearrange("(c p) e -> p c e", p=128)
        up_v = T["up"].rearrange("(c p) d -> p c d", p=128)
        for ds in range(8):
            conv_jobs.append((T["WoS"][ds], wout_v[:, :, ds * 512:(ds + 1) * 512]))
        for qc in range(8):
            conv_jobs.append((T["WqS"][qc], wq_v[:, :, qc * 256:(qc + 1) * 256]))
        for cg in range(16):
            for cp in range(4):
                i_ = cg * 4 + cp
                conv_jobs.append((T["DnS"][i_], dn_v[:, :, i_ * 256:(i_ + 1) * 256]))
            for ds in range(8):
                conv_jobs.append((T["UpS"][cg * 8 + ds], up_v[:, cg * 8:(cg + 1) * 8, ds * 512:(ds + 1) * 512]))
        ysb = [tl(f"ysb{k}", [128, 512], BF16) for k in range(4)]
        P1, bP1, P2, bP2, RB, bRB = C.ps[0], C.bps[0], C.ps[1], C.bps[1], C.ps[2], C.bps[2]
        cnt = {"prep": 0, "it": 0, "y": 0}

        def prep_group(g):
            k = g % NSET
            gb, g8 = divmod(g, 8)
            S.op("act", lambda e: e.activation(out=bm[k][0][0][:], in_=BST1[:, gb, :], func=AF.Copy, scale=rmask[:, g8:g8 + 1]),
                 r=[bset], w=[bm[k][0][1]])
            S.op("act", lambda e: e.activation(out=bm[k][1][0][:], in_=BST2[:, gb, :], func=AF.Copy, scale=rmask[:, g8:g8 + 1]),
                 r=[bset], w=[bm[k][1][1]])
            S.op("dve", lambda e: e.tensor_tensor(out=bm[k][2][0][:], in0=CST1[:, gb, :], in1=cmask[:, g8, :],
                                                  op=ALU.mult), r=[bset], w=[bm[k][2][1]])
            S.op("dve", lambda e: e.tensor_tensor(out=bm[k][3][0][:], in0=CST2[:, gb, :], in1=cmask[:, g8, :],
                                                  op=ALU.mult), r=[bset], w=[bm[k][3][1]])
            (rt, brt), (rm_, brm) = ROT[k], rtm[cnt["prep"] % 2]
            S.op("act", lambda e: e.activation(out=rt[:], in_=C.identf[:], func=AF.Copy, scale=c512[:, g:g + 1]),
                 r=[bset, C.bidentf], w=[brt])
            S.op("act", lambda e: e.activation(out=rm_[:], in_=swapI[:], func=AF.Copy, scale=s512[:, g:g + 1]), r=[bset], w=[brm])
            S.op("dve", lambda e: e.tensor_tensor(out=rt[:], in0=rt[:], in1=rm_[:], op=ALU.add), r=[brt, brm], w=[brt])
            (sn, bsn), (cs, bcs), (ki_, bki) = SNt[k], CSt[k], kph[cnt["prep"] % 2]
            cnt["prep"] += 1
            S.op("act", lambda e: e.activation(out=sn[:], in_=iota[:], func=AF.Copy, scale=theta[:, g:g + 1]), r=[bset], w=[bsn])
            S.op("dve", lambda e: e.tensor_copy(out=ki_[:], in_=sn[:]), r=[bsn], w=[bki])
            S.op("dve", lambda e: e.tensor_tensor(out=sn[:], in0=sn[:], in1=ki_[:], op=ALU.subtract), r=[bki, bsn], w=[bsn])
            S.op("dve", lambda e: e.scalar_tensor_tensor(out=cs[:], in0=sn[:], scalar=-1.0, in1=sn[:], op0=ALU.mult, op1=ALU.max),
                 r=[bsn], w=[bcs])
            S.op("act", lambda e: e.activation(out=sn[:], in_=sn[:], func=AF.Sin, scale=TWO_PI), r=[bsn], w=[bsn])
            S.op("act", lambda e: e.activation(out=cs[:], in_=cs[:], func=AF.Sin, scale=-TWO_PI, bias=hp[:, 0:1]), r=[bcs, bset], w=[bcs])
            S.op("act", lambda e: e.activation(out=CSb[k][0][:], in_=cs[:], func=AF.Copy), r=[bcs], w=[CSb[k][1]])
            S.op("act", lambda e: e.activation(out=SNb[k][0][:], in_=sn[:], func=AF.Copy), r=[bsn], w=[SNb[k][1]])
            (dc_, bdc) = DEC[cnt["prep"] % 2]
            S.op("act", lambda e: e.activation(out=dc_[:], in_=riota[:], func=AF.Exp, scale=eq_[:, g:g + 1]), r=[bset], w=[bdc])
            S.op("dve", lambda e: e.tensor_tensor(out=MCS[k][0][:], in0=dc_[:], in1=cs[:], op=ALU.mult), r=[bdc, bcs], w=[MCS[k][1]])
            S.op("dve", lambda e: e.tensor_tensor(out=MSN[k][0][:], in0=dc_[:], in1=sn[:], op=ALU.mult), r=[bdc, bsn], w=[MSN[k][1]])

        def stage_a(itn, g, seg, z_t, bz):
            k, ix = g % NSET, itn % 4
            zseg = z_t[:, seg * 512:(seg + 1) * 512]
            S.op("pe", lambda e: e.matmul(P1[:], bm[k][0][0][:], zseg, start=True, stop=True), r=[bm[k][0][1], bz], w=[bP1])
            S.op("pe", lambda e: e.matmul(P2[:], bm[k][1][0][:], zseg, start=True, stop=True), r=[bm[k][1][1], bz], w=[bP2])
            if seg < 4:
                (ac, bac), (jk, bjk) = accs[ix], junkS[itn % 2]
                S.op("dve", lambda e: e.memset(ac[:, 0:2], 0.0), w=[bac])
                S.op("dve", lambda e: e.scalar_tensor_tensor(out=jk[:], in0=P1[:], scalar=1.0, in1=MCS[k][0][:], op0=ALU.mult, op1=ALU.mult,
                                                             accum_out=ac[:, 0:1]), r=[bP1, MCS[k][1]], w=[bjk, bac])
                S.op("dve", lambda e: e.scalar_tensor_tensor(out=jk[:], in0=P2[:], scalar=1.0, in1=MSN[k][0][:], op0=ALU.mult, op1=ALU.mult,
                                                             accum_out=ac[:, 1:2]), r=[bP2, MSN[k][1]], w=[bjk, bac])
                return
            (a1, ba1), (a2, ba2) = t1[ix], t2[ix]
            S.op("dve", lambda e: e.tensor_tensor(out=a1[:], in0=P1[:], in1=CSt[k][0][:], op=ALU.mult), r=[bP1, CSt[k][1]], w=[ba1])
            S.op("dve", lambda e: e.tensor_tensor(out=a2[:], in0=P2[:], in1=SNt[k][0][:], op=ALU.mult), r=[bP2, SNt[k][1]], w=[ba2])
            S.op("dve", lambda e: e.tensor_tensor(out=a1[:], in0=a1[:], in1=a2[:], op=ALU.add), r=[ba1, ba2], w=[ba1])

        def stage_b(itn, g, seg, g8):
            k, ix = g % NSET, itn % 4
            if seg < 4:
                (ac, bac) = accs[ix]
                S.op("dve", lambda e: e.tensor_tensor(out=ac[:, 2:3], in0=ac[:, 0:1], in1=ac[:, 1:2], op=ALU.add), r=[bac], w=[bac])
                if seg > 0:
                    S.op("dve", lambda e: e.scalar_tensor_tensor(out=ac[:, 3:4], in0=ini[ix][0][:, 0:1], scalar=mag512[:, g:g + 1],
                                                                 in1=ac[:, 2:3], op0=ALU.mult, op1=ALU.add), r=[bac, ini[ix][1], bset], w=[bac])
                    vend = ac[:, 3:4]
                else:
                    vend = ac[:, 2:3]
                col = itn % 8
                S.op("pe", lambda e: e.matmul(RB[:, col:col + 1], ROT[k][0][:], vend, start=True, stop=True), r=[ROT[k][1], bac], w=[bRB])
                nx_ = ini[(itn + 2) % 4]
                S.op("act", lambda e: e.activation(out=nx_[0][:], in_=RB[:, col:col + 1], func=AF.Copy), r=[bRB], w=[nx_[1]])
                return
            (a1, ba1), (v_t, bv) = t1[ix], vv[ix]
            if seg == 0:
                init, rr = 0.0, [ba1, bset]
            else:
                init, rr = ini[ix][0][:, 0:1], [ba1, bset, ini[ix][1]]
            S.op("dve", lambda e: e.tensor_tensor_scan(out=v_t[:], data0=mag[:, g:g + 1].broadcast_to([128, 512]), data1=a1[:],
                                                       initial=init, op0=ALU.mult, op1=ALU.add), r=rr, w=[bv])
            if seg < 7:
                col = itn % 8
                S.op("pe", lambda e: e.matmul(RB[:, col:col + 1], ROT[k][0][:], v_t[:, 511:512], start=True, stop=True),
                     r=[ROT[k][1], bv], w=[bRB])
                nx_ = ini[(itn + 2) % 4]
                S.op("act", lambda e: e.activation(out=nx_[0][:], in_=RB[:, col:col + 1], func=AF.Copy), r=[bRB], w=[nx_[1]])
            if seg >= 4:
                (x1, bx1), (x2, bx2) = q1[ix], q2[ix]
                (vb_, bvb) = vb[ix]
                S.op("act", lambda e: e.activation(out=vb_[:], in_=v_t[:], func=AF.Copy), r=[bv], w=[bvb])
                S.op("dve", lambda e: e.tensor_tensor(out=x1[:], in0=vb_[:], in1=CSb[k][0][:], op=ALU.mult), r=[bvb, CSb[k][1]], w=[bx1])
                S.op("dve", lambda e: e.tensor_tensor(out=x2[:], in0=vb_[:], in1=SNb[k][0][:], op=ALU.mult), r=[bvb, SNb[k][1]], w=[bx2])

        def stage_c(itn, g, seg, g8):
            k, ix = g % NSET, itn % 4
            if seg >= 4:
                (x1, bx1), (x2, bx2) = q1[ix], q2[ix]
                Y, bY = C.ps[4 + seg - 4], C.bps[4 + seg - 4]
                S.op("pe", lambda e: e.matmul(Y[:], bm[k][2][0][:], x1[:], start=False, stop=False), r=[bm[k][2][1], bx1], w=[bY])
                S.op("pe", lambda e: e.matmul(Y[:], bm[k][3][0][:], x2[:], start=False, stop=(g8 == 7)), r=[bm[k][3][1], bx2], w=[bY])

        prep_group(gb_list[0] * 8)
        prep_group(gb_list[0] * 8 + 1)
        S.dma("sp", zt[0][0][:], T["ZT"][gb_list[0] * 128:(gb_list[0] + 1) * 128, :], w=[zt[0][1]])
        for gi, gb in enumerate(gb_list):
            z_t, bz = zt[gi % 2]
            if gi + 1 < len(gb_list):
                nz_t, nbz = zt[(gi + 1) % 2]
                ngb = gb_list[gi + 1]
                S.dma("sp", nz_t[:], T["ZT"][ngb * 128:(ngb + 1) * 128, :], w=[nbz])
            for so_ in range(4):
                S.op("pe", lambda e: e.matmul(C.ps[4 + so_][:], DST[:, gb, :], z_t[:, 2048 + so_ * 512:2048 + (so_ + 1) * 512],
                                              start=True, stop=False), r=[bset, bz], w=[C.bps[4 + so_]])
            prev = None
            prev2 = None
            for pr in range(4):
                pair = (gb * 8 + pr * 2, gb * 8 + pr * 2 + 1)
                for seg in range(8):
                    for g in pair:
                        itn = cnt["it"]
                        cnt["it"] += 1
                        stage_a(itn, g, seg, z_t, bz)
                        if prev is not None:
                            stage_b(*prev)
                        if prev2 is not None:
                            stage_c(*prev2)
                        prev2 = prev
                        prev = (itn, g, seg, g % 8)
                    if seg == 3:
                        if pr < 3:
                            nxt = (pair[0] + 2, pair[1] + 2)
                        elif gi + 1 < len(gb_list):
                            nxt = (gb_list[gi + 1] * 8, gb_list[gi + 1] * 8 + 1)
                        else:
                            nxt = ()
                        for g_ in nxt:
                            prep_group(g_)
                    if CONV_INTERLEAVE and seg in (1, 3, 5, 7) and conv_jobs:
                        o_ap, i_ap = conv_jobs.pop(0)
                        S.dma("pool", o_ap, i_ap)
            stage_b(*prev)
            stage_c(*prev2)
            stage_c(*prev)
            for so_ in range(4):
                y_t, by = ysb[cnt["y"] % 4]
                cnt["y"] += 1
                S.op("act", lambda e: e.activation(out=y_t[:], in_=C.ps[4 + so_][:], func=GELU), r=[C.bps[4 + so_]], w=[by])
                S.dma("sp", T["YG"][gb * 128:(gb + 1) * 128, so_ * 512:(so_ + 1) * 512], y_t[:], r=[by])
        while conv_jobs:
            o_ap, i_ap = conv_jobs.pop(0)
            S.dma("pool", o_ap, i_ap)
        S.barrier()


def rstd_ops(S, src_ap, dst, bdst, tmp, n, scale, rsrc):
    S.op("dve", lambda e: e.tensor_scalar(out=tmp[:, 0:n], in0=src_ap, scalar1=scale, scalar2=EPS, op0=ALU.mult, op1=ALU.add),
         r=list(rsrc) + [bdst], w=[bdst])
    S.op("act", lambda e: e.activation(out=tmp[:, n:2 * n], in_=tmp[:, 0:n], func=AF.Sqrt), r=[bdst], w=[bdst])
    S.op("dve", lambda e: e.reciprocal(out=dst, in_=tmp[:, n:2 * n]), r=[bdst], w=[bdst])


def phase_b(C, st_list, stop_after=None):
    nc, S, T = C.nc, C.S, C.T
    with contextlib.ExitStack() as pbs:
        raw = lambda name, shape, dt: pbs.enter_context(nc.sbuf_tensor(_uname("sr_" + name), list(shape), dt))
        bK = Buf("bconst")
        so = lambda k, fn: S.op(k, fn, r=[bK], w=[bK])
        wmT = raw("wmT", [128, 8, 128], BF16)
        bsr = raw("bsr", [128, 8, 128], F32)
        BIAS = raw("BIAS", [128, 16, 128], F32)
        cols = raw("cols", [128, 4, 16], F32)
        ones = raw("ones", [128, 128], BF16)
        S.dma("pool", wmT[:], T["wmT"][:], w=[bK])
        S.dma("sp", bsr[:], T["bs_rep"][:], w=[bK])
        S.dma("sp", cols[:], T["gm_cols"][:], w=[bK])
        so("dve", lambda e: e.memset(ones[:], 1.0))
        so("dve", lambda e: e.memset(wmT[64:128, :, 0:64], 0.0))
        for hh in range(8):
            bank = C.ps[hh // 4]
            S.op("pe", lambda e: e.matmul(bank[:, (hh % 4) * 128:(hh % 4 + 1) * 128], ones[:], wmT[:, hh, :], start=True, stop=True),
                 r=[bK], w=[C.bps[hh // 4]])
        for ct in range(16):
            hh = ct // 2
            bank = C.ps[hh // 4]
            S.op("dve", lambda e: e.scalar_tensor_tensor(out=BIAS[:, ct, :], in0=bank[:, (hh % 4) * 128:(hh % 4 + 1) * 128],
                                                         scalar=cols[:, 1, ct:ct + 1], in1=bsr[:, hh, :], op0=ALU.mult, op1=ALU.add),
                 r=[bK, C.bps[hh // 4]], w=[bK])
        hT = [_tile(nc, pbs, f"h{i}", [128, D], F32) for i in range(4)]
        rstd, brstd = _tile(nc, pbs, "rstdAB", [128, 8], F32)
        rtmp = raw("rtmp", [128, 16], F32)
        rtmp2 = raw("rtmp2", [128, 16], F32)
        wglu_v = T["w_glu"].rearrange("(c p) n -> p c n", p=128)
        wout_v = T["w_out"].rearrange("(c p) n -> p c n", p=128)
        nb = 0
        for st in st_list:
            t0 = st * TS
            with contextlib.ExitStack() as b1:
                tl = lambda name, shape, dt: _tile(nc, b1, name, shape, dt)
                yT, byT = _tile(nc, b1, "yT", [128, 32, TS], BF16)
                with contextlib.ExitStack() as b1a:
                    tla = lambda name, shape, dt: _tile(nc, b1a, name, shape, dt)
                    ygT, bygT = tla("ygT", [128, 16, TS], BF16)
                    uT, buT = tla("uT", [128, 16, TS], BF16)
                    wg = [tla(f"wg{i}", [128, 16, 256], BF16) for i in range(2)]
                    sig = [tla(f"sig{i}", [128, TS], F32) for i in range(3)]
                    ypre = [tla(f"ypre{i}", [128, TS], BF16) for i in range(3)]
                    ysq = [tla(f"ysq{i}", [128, TS], BF16) for i in range(3)]
                    vnt = [tla(f"vnt{i}", [128, 2048], BF16) for i in range(2)]
                    S.dma("sp", ygT[:], T["YG"].rearrange("(c p) t -> p c t", p=128)[:, :, t0:t0 + TS], w=[bygT])
                    S.dma("sp", uT[:], T["UT"].rearrange("(c p) t -> p c t", p=128)[:, :, t0:t0 + TS], w=[buT])
                    SSQ, bSSQ = C.ps[7], C.bps[7]
                    n2 = 0
                    deferred = []
                    for oc2 in range(8):
                        (w_t, bw) = wg[oc2 % 2]
                        S.dma("pool", w_t[:], wglu_v[:, :, oc2 * 256:(oc2 + 1) * 256], w=[bw])
                        for sub in range(2):
                            oc = oc2 * 2 + sub
                            pb_, bpb = C.ps[nb % 6], C.bps[nb % 6]
                            nb += 1
                            for ci in range(16):
                                S.op("pe", lambda e: e.matmul(pb_[:], w_t[:, ci, sub * 128:(sub + 1) * 128], ygT[:, ci, :],
                                                              start=(ci == 0), stop=(ci == 15)), r=[bw, bygT], w=[bpb], signal=(ci == 15))
                            while len(deferred) > 1:
                                deferred.pop(0)()
                            (sg_, bsg), (yp, byp), (yq, byq) = sig[n2 % 3], ypre[n2 % 3], ysq[n2 % 3]
                            n2 += 1
                            S.op("act", lambda e: e.activation(out=sg_[:], in_=pb_[:], func=AF.Sigmoid), r=[bpb], w=[bsg])
                            S.op("dve", lambda e: e.tensor_tensor(out=yp[:], in0=ygT[:, oc, :], in1=sg_[:], op=ALU.mult), r=[bygT, bsg], w=[byp])
                            S.op("act", lambda e: e.activation(out=yq[:], in_=yp[:], func=AF.Square), r=[byp], w=[byq])
                            S.op("dve", lambda e: e.tensor_scalar(out=yT[:, oc, :], in0=yp[:], scalar1=cols[:, 2, oc:oc + 1], scalar2=None,
                                                                  op0=ALU.mult), r=[byp, bK], w=[byT])
                            def ssq_a(oc=oc, yq=yq, byq=byq):
                                for tt in range(4):
                                    S.op("pe", lambda e: e.matmul(SSQ[:, oc * 4 + tt:oc * 4 + tt + 1], yq[:, tt * 128:(tt + 1) * 128],
                                                                  ones[:, 0:1], start=True, stop=True), r=[byq, bK], w=[bSSQ])
                            deferred.append(ssq_a)
                    for tt in range(4):
                        (v_t, bv) = vnt[tt % 2]
                        S.dma("sp", v_t[:], T["VN"][t0 + tt * 128:t0 + (tt + 1) * 128, :], w=[bv])
                        for cq in range(4):
                            pb_, bpb = C.ps[nb % 6], C.bps[nb % 6]
                            nb += 1
                            for j in range(4):
                                ct = cq * 4 + j
                                S.op("pe", lambda e: e.matmul(pb_[:, j * 128:(j + 1) * 128], v_t[:, ct * 128:(ct + 1) * 128], wmT[:, ct // 2, :],
                                                              start=True, stop=True), r=[bv, bK], w=[bpb], signal=(j == 3))
                            while len(deferred) > 1:
                                deferred.pop(0)()
                            (sg_, bsg), (yp, byp), (yq, byq) = sig[n2 % 3], ypre[n2 % 3], ysq[n2 % 3]
                            n2 += 1
                            for j in range(4):
                                ct = cq * 4 + j
                                S.op("dve", lambda e: e.scalar_tensor_tensor(out=sg_[:, j * 128:(j + 1) * 128], in0=pb_[:, j * 128:(j + 1) * 128],
                                                                             scalar=cols[:, 0, ct:ct + 1], in1=BIAS[:, ct, :],
                                                                             op0=ALU.mult, op1=ALU.add), r=[bpb, bK], w=[bsg])
                            S.op("dve", lambda e: e.tensor_tensor(out=yp[:].rearrange("p (j t) -> p j t", j=4),
                                                                  in0=sg_[:].rearrange("p (j t) -> p j t", j=4),
                                                                  in1=uT[:, cq * 4:(cq + 1) * 4, tt * 128:(tt + 1) * 128], op=ALU.mult),
                                 r=[bsg, buT], w=[byp])
                            S.op("act", lambda e: e.activation(out=yq[:], in_=yp[:], func=AF.Square), r=[byp], w=[byq])
                            for j in range(4):
                                ct = cq * 4 + j
                                S.op("act", lambda e: e.activation(out=yT[:, 16 + ct, tt * 128:(tt + 1) * 128], in_=yp[:, j * 128:(j + 1) * 128],
                                                                   func=AF.Copy, scale=cols[:, 3, ct:ct + 1]), r=[byp, bK], w=[byT])

                            def ssq_b(cq=cq, tt=tt, yq=yq, byq=byq):
                                for j in range(4):
                                    ct = cq * 4 + j
                                    S.op("pe", lambda e: e.matmul(SSQ[:, 64 + ct * 4 + tt:64 + ct * 4 + tt + 1], yq[:, j * 128:(j + 1) * 128],
                                                                  ones[:, 0:1], start=True, stop=True), r=[byq, bK], w=[bSSQ])
                            deferred.append(ssq_b)
                    while deferred:
                        deferred.pop(0)()
                    S.op("dve", lambda e: e.reduce_sum(out=rtmp[:, 8:16].rearrange("p (a t) -> p a t", a=2),
                                                       in_=SSQ[:, 0:128].rearrange("p (a o t) -> p a t o", a=2, t=4),
                                                       axis=AX.X), r=[bSSQ, brstd], w=[brstd])
                    rstd_ops(S, rtmp[:, 8:16], rstd[:], brstd, rtmp2, 8, 1.0 / 2048, [])
                    if "YTd" in T:
                        S.dma("sp", T["YTd"].rearrange("(c p) t -> p c t", p=128), yT[:], r=[byT], is_output=True)
                        S.dma("sp", T["RSd"][:], rstd[:], r=[brstd], is_output=True)
                    S.barrier()
                with contextlib.ExitStack() as b2:
                    tlb = lambda name, shape, dt: _tile(nc, b2, name, shape, dt)
                    wo = [tlb(f"wo{i}", [128, 32, 512], BF16) for i in range(2)]
                    xs = [tlb(f"xs{i}", [128, 512], F32) for i in range(2)]
                    tm = [tlb(f"tm{i}", [128, 512], F32) for i in range(2)]
                    n3 = 0
                    for ds in range(8):
                        (w_t, bw) = wo[ds % 2]
                        S.dma("pool", w_t[:], T["WoS"][ds], w=[bw])
                        for tt in range(4):
                            PA, bPA = C.ps[nb % 8], C.bps[nb % 8]
                            PB, bPB = C.ps[(nb + 1) % 8], C.bps[(nb + 1) % 8]
                            nb += 2
                            for ci in range(16):
                                S.op("pe", lambda e: e.matmul(PA[:], yT[:, ci, tt * 128:(tt + 1) * 128], w_t[:, ci, :],
                                                              start=(ci == 0), stop=(ci == 15)), r=[byT, bw], w=[bPA], signal=(ci == 15))
                            for ci in range(16, 32):
                                S.op("pe", lambda e: e.matmul(PB[:], yT[:, ci, tt * 128:(tt + 1) * 128], w_t[:, ci, :],
                                                              start=(ci == 16), stop=(ci == 31)), r=[byT, bw], w=[bPB], signal=(ci == 31))
                            (x_t, bx), (t_t, bt) = xs[n3 % 2], tm[n3 % 2]
                            n3 += 1
                            S.dma("sp", x_t[:], T["x_own"][t0 + tt * 128:t0 + (tt + 1) * 128, ds * 512:(ds + 1) * 512], w=[bx])
                            S.op("dve", lambda e: e.scalar_tensor_tensor(out=t_t[:], in0=PA[:], scalar=rstd[:, tt:tt + 1], in1=x_t[:],
                                                                         op0=ALU.mult, op1=ALU.add), r=[bPA, brstd, bx], w=[bt])
                            S.op("dve", lambda e: e.scalar_tensor_tensor(out=hT[tt][0][:, ds * 512:(ds + 1) * 512], in0=PB[:],
                                                                         scalar=rstd[:, 4 + tt:5 + tt], in1=t_t[:],
                                                                         op0=ALU.mult, op1=ALU.add), r=[bPB, brstd, bt], w=[hT[tt][1]])
                    S.barrier()
            if stop_after == "B2":
                for tt in range(4):
                    S.dma("sp", T["out"][t0 + tt * 128:t0 + (tt + 1) * 128, :], hT[tt][0][:], r=[hT[tt][1]], is_output=True)
                S.barrier()
                continue
            with contextlib.ExitStack() as pk:
                xnT, bxnT = _tile(nc, pk, "xnT", [128, 32, TS], BF16)
                with contextlib.ExitStack() as b34:
                    qT, bqT = _tile(nc, b34, "qT", [128, 16, TS], BF16)
                    with contextlib.ExitStack() as b3:
                        tl = lambda name, shape, dt: _tile(nc, b3, name, shape, dt)
                        gffn, bgffn = tl("gffn", [128, D], F32)
                        S.dma("sp", gffn[:], T["gffn_r"][:], w=[bgffn])
                        xn = [tl(f"xn{i}", [128, D], BF16) for i in range(2)]
                        junk, bjunk = tl("junkB", [128, D], BF16)
                        s8 = [tl(f"s8b{i}", [128, 8], F32) for i in range(2)]
                        wq = [tl(f"wq{i}", [128, 32, 256], BF16) for i in range(2)]
                        for tt in range(4):
                            (x_t, bx), (s_, bs_) = xn[tt % 2], s8[tt % 2]
                            h_t, bh = hT[tt]
                            S.op("dve", lambda e: e.memset(s_[:], 0.0), w=[bs_])
                            S.op("act", lambda e: e.activation(out=junk[:], in_=h_t[:], func=AF.Square, accum_out=s_[:, 0:1]),
                                 r=[bh], w=[bjunk, bs_])
                            rstd_ops(S, s_[:, 0:1], s_[:, 1:2], bs_, s_[:, 2:4], 1, 1.0 / D, [])
                            S.op("dve", lambda e: e.scalar_tensor_tensor(out=x_t[:], in0=h_t[:], scalar=s_[:, 1:2], in1=gffn[:],
                                                                         op0=ALU.mult, op1=ALU.mult), r=[bh, bs_, bgffn], w=[bx])
                            for dcg in range(4):
                                pb_, bpb = C.ps[nb % 8], C.bps[nb % 8]
                                nb += 1
                                pbb = pb_[:].bitcast(BF16)
                                for j in range(8):
                                    dc = dcg * 8 + j
                                    S.op("pe", lambda e: e.transpose(out=pbb[:, j * 128:(j + 1) * 128], in_=x_t[:, dc * 128:(dc + 1) * 128],
                                                                     identity=C.identb[:]), r=[bx, C.bidentb], w=[bpb], signal=(j == 7))
                                src = pbb[:, 0:1024].rearrange("p (j t) -> p j t", j=8)
                                dst = xnT[:, dcg * 8:(dcg + 1) * 8, tt * 128:(tt + 1) * 128]
                                if dcg % 2 == 0:
                                    S.op("act", lambda e: e.activation(out=dst, in_=src, func=AF.Copy), r=[bpb], w=[bxnT])
                                else:
                                    S.op("dve", lambda e: e.tensor_copy(out=dst, in_=src), r=[bpb], w=[bxnT])
                        wq_v = T["w_q"].rearrange("(c p) n -> p c n", p=128)
                        for qc in range(8):
                            (w_t, bw) = wq[qc % 2]
                            S.dma("pool", w_t[:], T["WqS"][qc], w=[bw])
                            for sub in range(2):
                                pb_, bpb = C.ps[nb % 8], C.bps[nb % 8]
                                nb += 1
                                for dc in range(32):
                                    S.op("pe", lambda e: e.matmul(pb_[:], w_t[:, dc, sub * 128:(sub + 1) * 128], xnT[:, dc, :],
                                                                  start=(dc == 0), stop=(dc == 31)), r=[bw, bxnT], w=[bpb], signal=(dc == 31))
                                S.op("act", lambda e: e.activation(out=qT[:, qc * 2 + sub, :], in_=pb_[:], func=AF.Copy), r=[bpb], w=[bqT])
                        if "Qd" in T:
                            S.dma("sp", T["Qd"].rearrange("(c p) t -> p c t", p=128), qT[:], r=[bqT], is_output=True)
                        S.barrier()
                    with contextlib.ExitStack() as b4:
                        raw4 = lambda name, shape, dt: b4.enter_context(nc.sbuf_tensor(_uname("sr_" + name), list(shape), dt))
                        bR = Buf("route")
                        ro = lambda k, fn, extra_r=(), extra_w=(): S.op(k, fn, r=[bR] + list(extra_r), w=[bR] + list(extra_w))
                        S1 = [raw4(f"S{a}sb", [128, 8, 128], F32) for a in range(2)]
                        Vv = [raw4(f"V{a}", [128, 8, 16], F32) for a in range(2)]
                        Iu = [raw4(f"I{a}u", [128, 8, 16], U32) for a in range(2)]
                        If_ = [raw4(f"I{a}f", [128, 8, 16], F32) for a in range(2)]
                        tmp16 = raw4("tmp16", [128, 16, 128], F32)
                        tmpc = raw4("tmpc", [128, 8, 256], F32)
                        bAH = [[Buf(f"ah{a}{h}") for h in range(8)] for a in range(2)]
                        bH = [Buf(f"h{h}") for h in range(8)]
                        cand = raw4("cand", [128, 8, 256], F32)
                        SC = raw4("SC", [128, 8, 16], F32)
                        CIu = raw4("CIu", [128, 8, 16], U32)
                        CIf = raw4("CIf", [128, 8, 16], F32)
                        ii = raw4("ii", [128, 8, 16], mybir.dt.int32)
                        irf = raw4("irf", [128, 8, 16], F32)
                        jrf = raw4("jrf", [128, 8, 16], F32)
                        ex = raw4("ex", [128, 8, 16], F32)
                        zz = raw4("zz", [128, 16], F32)
                        gate = raw4("gate", [128, 8, 16], F32)
                        e12 = [raw4(f"e{a}r", [128, 8, 16], F32) for a in range(2)]
                        tr3 = raw4("tr3", [128, 3, 128], F32)
                        At = [_tile(nc, b4, f"At{i}", [128, 128], BF16) for i in range(4)]
                        Bt = [_tile(nc, b4, f"Bt{i}", [128, 128], BF16) for i in range(4)]
                        GTt = [_tile(nc, b4, f"GTt{i}", [128, 128, 128], BF16) for i in range(1)]
                        io16 = C.iota128[:, 0:16]
                        for tt in range(4):
                            tsl = slice(tt * 128, (tt + 1) * 128)
                            for hh in range(8):
                                for a in range(2):
                                    bk = a * 2 + hh // 4
                                    S.op("pe", lambda e: e.matmul(C.ps[bk][:, (hh % 4) * 128:(hh % 4 + 1) * 128], qT[:, 2 * hh + a, tsl],
                                                                  C.kT[a][:], start=True, stop=True), r=[bqT, C.bkT], w=[C.bps[bk]])
                            for a in range(2):
                                for hb in range(2):
                                    ro("act", lambda e: e.activation(out=S1[a][:, hb * 4:(hb + 1) * 4, :],
                                                                     in_=C.ps[a * 2 + hb][:].rearrange("p (h n) -> p h n", h=4), func=AF.Copy),
                                       extra_r=[C.bps[a * 2 + hb]])
                            ah = [(a, hh) for a in range(2) for hh in range(8)]
                            for (a, hh) in ah:
                                S.op("dve", lambda e: e.max(out=Vv[a][:, hh, 0:8], in_=S1[a][:, hh, :]), r=[bR], w=[bAH[a][hh]])
                            for (a, hh) in ah:
                                S.op("dve", lambda e: e.max_index(out=Iu[a][:, hh, 0:8], in_max=Vv[a][:, hh, 0:8], in_values=S1[a][:, hh, :]),
                                     r=[bR, bAH[a][hh]], w=[bAH[a][hh]])
                            for (a, hh) in ah:
                                S.op("dve", lambda e: e.match_replace(out=tmp16[:, a * 8 + hh, :], in_to_replace=Vv[a][:, hh, 0:8],
                                                                      in_values=S1[a][:, hh, :], imm_value=-1e30), r=[bR, bAH[a][hh]], w=[bAH[a][hh]])
                            for (a, hh) in ah:
                                S.op("dve", lambda e: e.max(out=Vv[a][:, hh, 8:16], in_=tmp16[:, a * 8 + hh, :]), r=[bAH[a][hh]], w=[bAH[a][hh]])
                            for (a, hh) in ah:
                                S.op("dve", lambda e: e.max_index(out=Iu[a][:, hh, 8:16], in_max=Vv[a][:, hh, 8:16], in_values=tmp16[:, a * 8 + hh, :]),
                                     r=[bAH[a][hh]], w=[bAH[a][hh]])
                            for a in range(2):
                                ro("dve", lambda e: e.tensor_copy(out=If_[a][:], in_=Iu[a][:]), extra_r=bAH[a], extra_w=bAH[a])
                            for hh in range(8):
                                S.op("dve", lambda e: e.tensor_tensor(out=cand[:, hh, :].rearrange("p (i j) -> p i j", i=16),
                                                                      in0=Vv[0][:, hh, :].unsqueeze(2).broadcast_to([128, 16, 16]),
                                                                      in1=Vv[1][:, hh, :].unsqueeze(1).broadcast_to([128, 16, 16]), op=ALU.add),
                                     r=[bAH[0][hh], bAH[1][hh], bR], w=[bH[hh]])
                            for hh in range(8):
                                S.op("dve", lambda e: e.max(out=SC[:, hh, 0:8], in_=cand[:, hh, :]), r=[bH[hh]], w=[bH[hh]])
                            for hh in range(8):
                                S.op("dve", lambda e: e.max_index(out=CIu[:, hh, 0:8], in_max=SC[:, hh, 0:8], in_values=cand[:, hh, :]), r=[bH[hh]], w=[bH[hh]])
                            for hh in range(8):
                                S.op("dve", lambda e: e.match_replace(out=tmpc[:, hh, :], in_to_replace=SC[:, hh, 0:8], in_values=cand[:, hh, :],
                                                                      imm_value=-1e30), r=[bH[hh], bR], w=[bH[hh]])
                            for hh in range(8):
                                S.op("dve", lambda e: e.max(out=SC[:, hh, 8:16], in_=tmpc[:, hh, :]), r=[bH[hh]], w=[bH[hh]])
                            for hh in range(8):
                                S.op("dve", lambda e: e.max_index(out=CIu[:, hh, 8:16], in_max=SC[:, hh, 8:16], in_values=tmpc[:, hh, :]), r=[bH[hh]], w=[bH[hh]])
                            ro("dve", lambda e: e.tensor_copy(out=CIf[:], in_=CIu[:]), extra_r=bH, extra_w=bH)
                            ro("dve", lambda e: e.tensor_tensor(out=ex[:], in0=SC[:], in1=SC[:, :, 0:1].broadcast_to([128, 8, 16]), op=ALU.subtract))
                            ro("act", lambda e: e.activation(out=ex[:], in_=ex[:], func=AF.Exp))
                            ro("dve", lambda e: e.reduce_sum(out=zz[:, 0:8], in_=ex[:], axis=AX.X))
                            ro("dve", lambda e: e.reciprocal(out=zz[:, 8:16], in_=zz[:, 0:8]))
                            ro("dve", lambda e: e.tensor_tensor(out=gate[:], in0=ex[:], in1=zz[:, 8:16].unsqueeze(2).broadcast_to([128, 8, 16]),
                                                                op=ALU.mult))
                            ro("dve", lambda e: e.tensor_scalar(out=ii[:], in0=CIf[:], scalar1=1.0 / 16, scalar2=-0.46875, op0=ALU.mult, op1=ALU.add))
                            ro("dve", lambda e: e.tensor_copy(out=irf[:], in_=ii[:]))
                            ro("dve", lambda e: e.scalar_tensor_tensor(out=jrf[:], in0=irf[:], scalar=-16.0, in1=CIf[:], op0=ALU.mult, op1=ALU.add))
                            for a, sel in ((0, irf), (1, jrf)):
                                for hh in range(8):
                                    ro("dve", lambda e: e.tensor_tensor(out=tmpc[:, hh, :].rearrange("p (k i) -> p k i", k=16), in0=sel[:, hh, :].unsqueeze(2).broadcast_to([128, 16, 16]),
                                                                        in1=io16.unsqueeze(1).broadcast_to([128, 16, 16]), op=ALU.is_equal))
                                    ro("dve", lambda e: e.tensor_tensor(out=tmpc[:, hh, :].rearrange("p (k i) -> p k i", k=16), in0=tmpc[:, hh, :].rearrange("p (k i) -> p k i", k=16),
                                                                        in1=If_[a][:, hh, :].unsqueeze(1).broadcast_to([128, 16, 16]), op=ALU.mult))
                                ro("dve", lambda e: e.reduce_sum(out=e12[a][:].rearrange("p h k -> p (h k)"),
                                                                 in_=tmpc[:].rearrange("p h (k i) -> p (h k) i", k=16), axis=AX.X))
                            TRB, bTRB = C.ps[4], C.bps[4]
                            for n_, src in enumerate((e12[0], e12[1], gate)):
                                ro("pe", lambda e: e.transpose(out=TRB[:, n_ * 128:(n_ + 1) * 128], in_=src[:].rearrange("p h k -> p (h k)"),
                                                               identity=C.identf[:]), extra_r=[C.bidentf], extra_w=[bTRB])
                            ro("act", lambda e: e.activation(out=tr3[:].rearrange("p a t -> p (a t)"), in_=TRB[:, 0:384], func=AF.Copy), extra_r=[bTRB])
                            if "R3d" in T and tt == 0:
                                S.dma("sp", T["R3d"][:], tr3[:], r=[bR], is_output=True)
                                S.dma("sp", T["S1d"][:], S1[0][:], r=[bR], is_output=True)
                                S.dma("sp", T["V1d"][:], Vv[0][:], r=[bR], is_output=True)
                                S.dma("sp", T["SCd"][:], SC[:], r=[bR], is_output=True)
                                S.dma("sp", T["CId"][:], CIf[:], r=[bR], is_output=True)
                                S.dma("sp", T["I1d"][:], If_[0][:], r=[bR], is_output=True)
                            g_t, bg = GTt[0]
                            for t4 in range(32):
                                pb_, bpb = C.ps[5 + t4 % 3], C.bps[5 + t4 % 3]
                                for tk in range(4):
                                    t = t4 * 4 + tk
                                    (a_t, ba), (b_t, bb) = At[t % 4], Bt[t % 4]
                                    S.op("dve", lambda e: e.tensor_scalar(out=a_t[:], in0=C.iota128[:], scalar1=tr3[:, 0, t:t + 1],
                                                                          scalar2=tr3[:, 2, t:t + 1], op0=ALU.is_equal, op1=ALU.mult),
                                         r=[bR, C.biota], w=[ba])
                                    S.op("dve", lambda e: e.tensor_scalar(out=b_t[:], in0=C.iota128[:], scalar1=tr3[:, 1, t:t + 1],
                                                                          scalar2=None, op0=ALU.is_equal), r=[bR, C.biota], w=[bb])
                                    S.op("pe", lambda e: e.matmul(pb_[:, tk * 128:(tk + 1) * 128], b_t[:], a_t[:], start=True, stop=True),
                                         r=[ba, bb], w=[bpb])
                                S.op("act", lambda e: e.activation(out=g_t[:, :, t4 * 4:(t4 + 1) * 4].rearrange("p e t -> p t e"),
                                                                   in_=pb_[:].rearrange("p (t e) -> p t e", t=4), func=AF.Copy), r=[bpb], w=[bg])
                            S.dma("sp", T["GT"][st * 4 + tt], g_t[:], r=[bg])
                        S.barrier()
                if stop_after == "B4":
                    continue
                with contextlib.ExitStack() as b5:
                    tl = lambda name, shape, dt: _tile(nc, b5, name, shape, dt)
                    Dn = [tl(f"Dn{i}", [128, 32, 256], BF16) for i in range(2)]
                    Up = [tl(f"Up{i}", [128, 8, 512], BF16) for i in range(2)]
                    act = [tl(f"act{i}", [128, 8, TS], BF16) for i in range(2)]
                    Gc = [tl(f"Gc{i}", [128, 8, TS], BF16) for i in range(2)]
                    gel = [tl(f"gel{i}", [128, TS], BF16) for i in range(2)]
                    dn_v = T["downT"].rearrange("(c p) e -> p c e", p=128)
                    up_v = T["up"].rearrange("(c p) d -> p c d", p=128)
                    nd = nu = ng = 0
                    first_pass = False
                    for cg in range(C.ncg):
                        (g_c, bgc), (a_c, bac) = Gc[cg % 2], act[cg % 2]
                        for tt in range(4):
                            S.dma("sp", g_c[:, :, tt * 128:(tt + 1) * 128], T["GT"][st * 4 + tt][:, cg * 8:(cg + 1) * 8, :], w=[bgc])
                        for cp in range(4):
                            (d_t, bd) = Dn[nd % 2]
                            nd += 1
                            e0 = (cg * 8 + cp * 2) * 128
                            if first_pass:
                                S.dma("pool", d_t[:], dn_v[:, :, e0:e0 + 256], w=[bd])
                                S.dma("sp", T["DnS"][cg * 4 + cp], d_t[:], r=[bd])
                            else:
                                S.dma("sp", d_t[:], T["DnS"][cg * 4 + cp], w=[bd])
                            for ck in range(2):
                                ci = cp * 2 + ck
                                pb_, bpb = C.ps[nb % 8], C.bps[nb % 8]
                                nb += 1
                                for dc in range(32):
                                    S.op("pe", lambda e: e.matmul(pb_[:], d_t[:, dc, ck * 128:(ck + 1) * 128], xnT[:, dc, :],
                                                                  start=(dc == 0), stop=(dc == 31)), r=[bd, bxnT], w=[bpb], signal=(dc == 31))
                                (ge, bge) = gel[ng % 2]
                                ng += 1
                                S.op("act", lambda e: e.activation(out=ge[:], in_=pb_[:], func=GELU), r=[bpb], w=[bge])
                                S.op("dve", lambda e: e.tensor_tensor(out=a_c[:, ci, :], in0=ge[:], in1=g_c[:, ci, :], op=ALU.mult),
                                     r=[bge, bgc], w=[bac])
                        for ds in range(8):
                            (u_t, bu) = Up[nu % 2]
                            nu += 1
                            if first_pass:
                                S.dma("pool", u_t[:], up_v[:, cg * 8:(cg + 1) * 8, ds * 512:(ds + 1) * 512], w=[bu])
                                S.dma("sp", T["UpS"][cg * 8 + ds], u_t[:], r=[bu])
                            else:
                                S.dma("pool", u_t[:], T["UpS"][cg * 8 + ds], w=[bu])
                            for tt in range(4):
                                pb_, bpb = C.ps[nb % 8], C.bps[nb % 8]
                                nb += 1
                                for ci in range(8):
                                    S.op("pe", lambda e: e.matmul(pb_[:], a_c[:, ci, tt * 128:(tt + 1) * 128], u_t[:, ci, :],
                                                                  start=(ci == 0), stop=(ci == 7)), r=[bac, bu], w=[bpb], signal=(ci == 7))
                                hs = hT[tt][0][:, ds * 512:(ds + 1) * 512]
                                S.op("dve", lambda e: e.tensor_tensor(out=hs, in0=pb_[:], in1=hs, op=ALU.add), r=[bpb, hT[tt][1]], w=[hT[tt][1]])
                    S.barrier()
            if "Hd" in T:
                for tt in range(4):
                    S.dma("sp", T["Hd"][tt * 128:(tt + 1) * 128, :], hT[tt][0][:], r=[hT[tt][1]], is_output=True)
            with contextlib.ExitStack() as b6:
                tl = lambda name, shape, dt: _tile(nc, b6, name, shape, dt)
                gfin, bgfin = tl("gfin", [128, D], F32)
                S.dma("sp", gfin[:], T["gfin_r"][:], w=[bgfin])
                junk, bjunk = tl("junkC", [128, D], BF16)
                ot = [tl(f"ot{i}", [128, D], F32) for i in range(2)]
                s8 = [tl(f"s8c{i}", [128, 8], F32) for i in range(2)]
                for tt in range(4):
                    (o_t, bo), (s_, bs_) = ot[tt % 2], s8[tt % 2]
                    h_t, bh = hT[tt]
                    S.op("dve", lambda e: e.memset(s_[:], 0.0), w=[bs_])
                    S.op("act", lambda e: e.activation(out=junk[:], in_=h_t[:], func=AF.Square, accum_out=s_[:, 0:1]), r=[bh], w=[bjunk, bs_])
                    rstd_ops(S, s_[:, 0:1], s_[:, 1:2], bs_, s_[:, 2:4], 1, 1.0 / D, [])
                    S.op("dve", lambda e: e.scalar_tensor_tensor(out=o_t[:], in0=h_t[:], scalar=s_[:, 1:2], in1=gfin[:],
                                                                 op0=ALU.mult, op1=ALU.mult), r=[bh, bs_, bgfin], w=[bo])
                    S.dma("sp", T["out"][t0 + tt * 128:t0 + (tt + 1) * 128, :], o_t[:], r=[bo], is_output=True)
                S.barrier()
        S.barrier()


def build(dbg=None):
    nc = bass.Bass("TRN2", target_bir_lowering=False)
    T = {}

    def din(name, shape, dt=F32):
        T[name] = nc.dram_tensor(name, list(shape), dt, kind="ExternalInput").ap()

    def dscr(name, shape, dt, out=False):
        T[name] = nc.dram_tensor(name, list(shape), dt, kind="ExternalOutput" if out else "Internal").ap()

    din("x_own", [NTOK, D])
    din("x_prev", [NTOK, D])
    din("gmix_r", [128, D])
    din("w_in", [D, 6144])
    din("ident", [128, 128])
    for n in ("a_re_q", "a_im_q", "ldt_q"):
        din(n, [128, 128])
    for n in ("a_re_b", "a_im_b", "ldt_b", "b_re_b", "b_im_b"):
        din(n, [128, 1024])
    din("cmix1", [128, 2048])
    din("cmix2", [128, 2048])
    din("d_col", [128, 16])
    din("sgn1", [128, 1])
    din("rowmask", [128, 8])
    din("colmask", [128, 8, 128])
    din("iota512", [128, 512])
    din("swapI", [128, 128])
    din("riota512", [128, 512])
    dscr("YG", [2048, NTOK], BF16, out=(dbg == "S5"))
    din("w_glu", [2048, 2048])
    if dbg == "B2":
        dscr("YTd", [4096, TS], BF16, out=True)
        dscr("RSd", [128, 8], F32, out=True)
    din("w_out", [D, D])
    din("wmT", [128, 8, 128])
    din("bs_rep", [128, 8, 128])
    din("gm_cols", [128, 4, 16])
    din("gffn_r", [128, D])
    din("gfin_r", [128, D])
    din("w_q", [D, 2048])
    din("k1T", [128, 128])
    din("k2T", [128, 128])
    din("downT", [D, 16384])
    din("up", [16384, D])
    din("iota128", [128, 128])
    dscr("GT", [16, 128, 128, 128], BF16)
    dscr("DnS", [64, 128, 32, 256], BF16)
    dscr("WinS", [24, 128, 32, 256], BF16)
    dscr("WoS", [8, 128, 32, 512], BF16)
    dscr("WqS", [8, 128, 32, 256], BF16)
    dscr("UpS", [128, 128, 8, 512], BF16)
    if dbg == "B6x":
        dscr("Qd", [2048, TS], BF16, out=True)
        dscr("R3d", [128, 3, 128], F32, out=True)
        dscr("S1d", [128, 8, 128], F32, out=True)
        dscr("V1d", [128, 8, 16], F32, out=True)
        dscr("SCd", [128, 8, 16], F32, out=True)
        dscr("CId", [128, 8, 16], F32, out=True)
        dscr("I1d", [128, 8, 16], F32, out=True)
        dscr("Hd", [TS, D], F32, out=True)
    dscr("ZT", [2048, 4096], BF16, out=(dbg == "A"))
    dscr("UT", [2048, NTOK], BF16, out=(dbg == "A"))
    dscr("VN", [NTOK, 2048], BF16, out=(dbg == "A"))
    dscr("out", [NTOK, D], F32, out=True)

    with contextlib.ExitStack() as st:
        C = Ctx()
        C.nc, C.T = nc, T
        C.S = S = Sched(nc, st)
        C.ps = [st.enter_context(nc.psum_tensor(f"ps{i}", [128, 512], F32)) for i in range(8)]
        C.bps = [Buf(f"ps{i}") for i in range(8)]
        C.bZT, C.bUT, C.bVN, C.bYG, C.bGT = Buf("ZT"), Buf("UT"), Buf("VN"), Buf("YG"), Buf("GT")
        C.identb, C.bidentb = _tile(nc, st, "identb", [128, 128], BF16)
        C.identf, C.bidentf = _tile(nc, st, "identf", [128, 128], F32)
        S.dma("pool", C.identb[:], T["ident"][:], w=[C.bidentb])
        S.dma("sp", C.identf[:], T["ident"][:], w=[C.bidentf])
        C.iota128, C.biota = _tile(nc, st, "iota128", [128, 128], F32)
        S.dma("sp", C.iota128[:], T["iota128"][:], w=[C.biota])
        C.bkT = Buf("kT")
        C.kT = [st.enter_context(nc.sbuf_tensor(f"sb_k{a}T", [128, 128], BF16)) for a in range(2)]
        S.dma("pool", C.kT[0][:], T["k1T"][:], w=[C.bkT])
        S.dma("pool", C.kT[1][:], T["k2T"][:], w=[C.bkT])
        C.ncg = 16
        C.win_cached, C.win_fresh = set(), set()
        if dbg == "A":
            phase_a(C, [0, 4])
        else:
            phase_a(C, list(range(8)))
        if dbg == "S5":
            phase_s5(C, [0, 5])
        elif dbg != "TA":
            phase_s5(C, list(range(16)))
        if dbg == "B2":
            phase_b(C, [0], stop_after="B2")
        elif dbg == "B6":
            phase_b(C, [0])
        elif dbg == "B7":
            phase_b(C, [0, 1])
        elif dbg in ("TA", "TS"):
            pass
        elif dbg == "TB2":
            phase_b(C, [0], stop_after="B2")
        elif dbg == "TB4":
            phase_b(C, [0], stop_after="B4")
        elif dbg is None:
            phase_b(C, list(range(4)))
        S.finish()
        print("instructions:", S.nins, "sems:", S.nsem)
    return nc


def host_layout(inputs):
    f = lambda k: np.ascontiguousarray(np.asarray(inputs[k], dtype=np.float32))
    x = f("x")
    shared = {}
    shared["gmix_r"] = np.ascontiguousarray(np.broadcast_to(f("norm_mix_g")[0], (128, D)))
    shared["w_in"] = f("w_in")[0]
    shared["ident"] = np.eye(128, dtype=np.float32)
    a_re, a_im, ldt = f("s5_a_re")[0], f("s5_a_im")[0], f("s5_log_dt")[0]
    b_re, b_im = f("s5_b_re")[0], f("s5_b_im")[0]
    c_re, c_im = f("s5_c_re")[0], f("s5_c_im")[0]
    cp = np.ascontiguousarray
    shared["a_re_q"] = cp(np.concatenate([a_re.T, a_re.T], axis=0))
    shared["a_im_q"] = cp(np.concatenate([a_im.T, a_im.T], axis=0))
    shared["ldt_q"] = cp(np.broadcast_to(ldt[None, :], (128, 128)))
    def blay(a_gp):
        t = a_gp.reshape(16, 8, 64)
        t = np.broadcast_to(t[:, :, None, :], (16, 8, 16, 64))
        return cp(t.transpose(1, 2, 0, 3).reshape(128, 1024))
    shared["a_re_b"] = blay(a_re)
    shared["a_im_b"] = blay(a_im)
    shared["ldt_b"] = blay(np.broadcast_to(ldt[:, None], (128, 64)))
    bl = lambda b: cp(b.reshape(16, 8, 64, 16).transpose(1, 3, 0, 2).reshape(128, 1024))
    shared["b_re_b"] = bl(b_re)
    shared["b_im_b"] = bl(b_im)
    cl = lambda c: c.transpose(2, 0, 1).reshape(64, 2048)
    shared["cmix1"] = cp(np.concatenate([cl(c_re), cl(c_im)], axis=0))
    shared["cmix2"] = cp(np.concatenate([cl(c_im), cl(c_re)], axis=0))
    shared["d_col"] = cp(f("s5_d")[0].reshape(16, 128).T)
    shared["sgn1"] = np.concatenate([np.ones((64, 1), np.float32), -np.ones((64, 1), np.float32)], axis=0)
    rm = np.zeros((128, 8), np.float32)
    cmk = np.zeros((128, 8, 128), np.float32)
    for g8 in range(8):
        rm[g8 * 16:(g8 + 1) * 16, g8] = 1.0
        cmk[:, g8, g8 * 16:(g8 + 1) * 16] = 1.0
    shared["rowmask"] = rm
    shared["colmask"] = cmk
    shared["w_glu"] = f("s5_w_glu")[0]
    shared["w_out"] = f("w_out")[0]
    shared["wmT"] = cp(f("gm_w_s")[0].transpose(2, 0, 1))
    shared["bs_rep"] = cp(np.broadcast_to(f("gm_b_s")[0][None], (128, 8, 128)))
    colv = lambda v: v.reshape(16, 128).T
    shared["gm_cols"] = cp(np.stack([colv(f("gm_ln_g")[0]), colv(f("gm_ln_b")[0]), colv(f("norm_s5_out_g")[0]),
                                     colv(f("norm_gm_out_g")[0])], axis=1))
    shared["gffn_r"] = cp(np.broadcast_to(f("norm_ffn_g")[0], (128, D)))
    shared["gfin_r"] = cp(np.broadcast_to(f("norm_final_g"), (128, D)))
    shared["w_q"] = f("peer_w_q")[0]
    shared["k1T"] = cp(f("peer_keys_1")[0].T)
    shared["k2T"] = cp(f("peer_keys_2")[0].T)
    shared["downT"] = cp(f("peer_down")[0].T)
    shared["up"] = f("peer_up")[0]
    shared["iota128"] = cp(np.broadcast_to(np.arange(128, dtype=np.float32)[None, :], (128, 128)))
    shared["swapI"] = cp(np.roll(np.eye(128, dtype=np.float32), 64, axis=1))
    shared["riota512"] = cp(np.broadcast_to(np.arange(511, -1, -1, dtype=np.float32)[None, :], (128, 512)))
    shared["iota512"] = cp(np.broadcast_to(np.arange(512, dtype=np.float32)[None, :], (128, 512)))
    maps = []
    for c in range(8):
        b, s = c // 2, c % 2
        m = dict(shared)
        m["x_own"] = np.ascontiguousarray(x[b, s * NTOK:(s + 1) * NTOK])
        m["x_prev"] = np.ascontiguousarray(x[b, 0:NTOK]) if s == 1 else np.zeros((NTOK, D), np.float32)
        maps.append(m)
    return maps


def kernel(**inputs):
    maps = host_layout(inputs)
    nc = build()
    res = run_bass_kernel_spmd(nc, maps, core_ids=list(range(8)))
    out = np.zeros((4, 4096, D), np.float32)
    for c in range(8):
        b, s = c // 2, c % 2
        out[b, s * NTOK:(s + 1) * NTOK] = res.results[c]["out"]
    return out
```
